# Optimizing a Trainium2 kernel written in Bass

```python
import math
import jax, jax.numpy as jnp
from jax import lax
import numpy as np

D_MODEL = 1024
BATCH = 2
SEQ = 8192
DEPTH = 2

GRID_W = 64
CTX_LEN = 256
HEAD_DIM = 64
ROPE_THETA = 10000.0
RMS_EPS = 1e-6
NEG_INF = -1e30
BLOCK = 128
A_HQ = 8
A_HKV = 2
A_GROUP = A_HQ // A_HKV
WINDOW = 128
B_H = 4
B_HD = HEAD_DIM
B_VD = 2 * HEAD_DIM
C_H = 4
C_DK = 128
C_DV = 128
CHUNK = 64
N_BRANCH = 3
BRANCH_W = 512
A_Q_W = A_HQ * HEAD_DIM
A_KV_W = A_HKV * HEAD_DIM
B_QK_W = B_H * 2 * B_HD
B_V_W = B_H * B_VD
C_K_W = C_H * C_DK
C_V_W = C_H * C_DV
GATE_W = N_BRANCH * D_MODEL
SPLITS = (A_Q_W, A_KV_W, A_KV_W, B_QK_W, B_QK_W, B_V_W, C_K_W, C_V_W, C_K_W, C_K_W, C_V_W, GATE_W)
IN_W = sum(SPLITS)
SPLIT_IDX = tuple(int(v) for v in np.cumsum(SPLITS)[:-1])
N_EXPERTS = 32
TOP_K = 4
EXPERT_FF = D_MODEL
SWIGLU_LIMIT = 7.0
SWIGLU_ALPHA = 1.702
MOE_BLOCK = 128

kernel_name = "hybrid_diffusion_gqa_diff_hgrn2_moe"


def rms_norm(x, g):
    xf = x.astype(jnp.float32)
    y = xf * lax.rsqrt(jnp.mean(xf * xf, axis=-1, keepdims=True) + RMS_EPS)
    return (y * g.astype(jnp.float32)).astype(x.dtype)


def modulate(x, g, shift, scale):
    return rms_norm(x, g) * (1 + scale[:, None, :]) + shift[:, None, :]


def axial_rope_tables(rows, dtype):
    row = jnp.repeat(jnp.arange(rows, dtype=jnp.float32), GRID_W)
    col = jnp.tile(jnp.arange(GRID_W, dtype=jnp.float32), rows)
    n_freq = HEAD_DIM // 4
    inv = ROPE_THETA ** (-jnp.arange(n_freq, dtype=jnp.float32) / n_freq)
    ang_r = row[:, None] * inv
    ang_c = col[:, None] * inv
    return (jnp.cos(ang_r).astype(dtype), jnp.sin(ang_r).astype(dtype),
            jnp.cos(ang_c).astype(dtype), jnp.sin(ang_c).astype(dtype))


def apply_axial_rope(x, tabs):
    cos_r, sin_r, cos_c, sin_c = tabs
    n = HEAD_DIM // 4
    half = HEAD_DIM // 2
    shape = (1, x.shape[1]) + (1,) * (x.ndim - 3) + (n,)

    def rot(xa, cos, sin):
        cos = cos.reshape(shape)
        sin = sin.reshape(shape)
        x1, x2 = xa[..., :n], xa[..., n:]
        return jnp.concatenate([x1 * cos - x2 * sin, x2 * cos + x1 * sin], axis=-1)

    return jnp.concatenate([rot(x[..., :half], cos_r, sin_r), rot(x[..., half:], cos_c, sin_c)], axis=-1)


def window_gqa_latent(q, k, v, kc, vc, sink):
    B, S = q.shape[:2]
    L = kc.shape[1]
    nb = S // BLOCK
    scale = HEAD_DIM ** -0.5
    qb = q.reshape(B, nb, BLOCK, A_HKV, A_GROUP, HEAD_DIM)

    def band(t):
        tp = jnp.pad(t, ((0, 0), (BLOCK, BLOCK), (0, 0), (0, 0))).reshape(B, nb + 2, BLOCK, A_HKV, HEAD_DIM)
        return jnp.concatenate([tp[:, :-2], tp[:, 1:-1], tp[:, 2:]], axis=2)

    kw, vw = band(k), band(v)
    s_loc = jnp.einsum('bnqhgd,bnkhd->bnhgqk', qb, kw).astype(jnp.float32) * scale
    s_ctx = jnp.einsum('bnqhgd,blhd->bnhgql', qb, kc).astype(jnp.float32) * scale
    qi = jnp.arange(BLOCK)[:, None]
    kj = jnp.arange(3 * BLOCK)[None, :]
    rel = kj - BLOCK - qi
    kpos = jnp.arange(nb)[:, None, None] * BLOCK + kj[None] - BLOCK
    valid = (jnp.abs(rel) <= WINDOW)[None] & (kpos >= 0) & (kpos < S)
    s_loc = jnp.where(valid[None, :, None, None], s_loc, NEG_INF)
    s_sink = jnp.broadcast_to(sink.astype(jnp.float32).reshape(1, 1, A_HKV, A_GROUP, 1, 1), s_loc.shape[:-1] + (1,))
    p = jax.nn.softmax(jnp.concatenate([s_loc, s_ctx, s_sink], axis=-1), axis=-1).astype(v.dtype)
    o = (jnp.einsum('bnhgqk,bnkhd->bnqhgd', p[..., :3 * BLOCK], vw)
         + jnp.einsum('bnhgql,blhd->bnqhgd', p[..., 3 * BLOCK:3 * BLOCK + L], vc))
    return o.reshape(B, S, A_Q_W)


def sink_attn_context(qc, kc, vc, sink):
    B, L = qc.shape[:2]
    qg = qc.reshape(B, L, A_HKV, A_GROUP, HEAD_DIM)
    s = jnp.einsum('blhgd,bmhd->bhglm', qg, kc).astype(jnp.float32) * HEAD_DIM ** -0.5
    s_sink = jnp.broadcast_to(sink.astype(jnp.float32).reshape(1, A_HKV, A_GROUP, 1, 1), s.shape[:-1] + (1,))
    p = jax.nn.softmax(jnp.concatenate([s, s_sink], axis=-1), axis=-1)[..., :L].astype(vc.dtype)
    return jnp.einsum('bhglm,bmhd->blhgd', p, vc).reshape(B, L, A_Q_W)


def diff_attend(q, keys, vals, lam):
    s = jnp.einsum('bqhcd,bkhcd->bhcqk', q, keys).astype(jnp.float32) * B_HD ** -0.5
    p = jax.nn.softmax(s, axis=-1)
    a = (p[:, :, 0] - lam * p[:, :, 1]).astype(vals.dtype)
    return jnp.einsum('bhqk,bkhe->bqhe', a, vals)


def diff_attn_latent(q, k, v, kc, vc, lam):
    B, S = q.shape[:2]
    nb = S // BLOCK
    keys = jnp.concatenate([k, kc], axis=1)
    vals = jnp.concatenate([v, vc], axis=1)
    qb = jnp.moveaxis(q.reshape(B, nb, BLOCK, B_H, 2, B_HD), 1, 0)
    o = lax.map(lambda qq: diff_attend(qq, keys, vals, lam), qb)
    return jnp.moveaxis(o, 0, 1).reshape(B, S, B_H, B_VD)


def diff_head_out(o, w, lam_init):
    B, T = o.shape[:2]
    return (rms_norm(o, w) * (1 - lam_init)).reshape(B, T, B_V_W)


def hgrn2_log_forget(z, lb):
    lbf = lb.astype(jnp.float32)
    return jnp.logaddexp(jnp.log(lbf), jnp.log1p(-lbf) + jax.nn.log_sigmoid(z.astype(jnp.float32)))


def hgrn2_scan(q, k, logf, v, state):
    B, T = q.shape[:2]
    nc = T // CHUNK

    def chunks(t):
        return t.astype(jnp.float32).reshape(B, nc, CHUNK, C_H, t.shape[-1]).transpose(1, 0, 3, 2, 4)

    tri = jnp.tril(jnp.ones((CHUNK, CHUNK), dtype=bool))

    def step(S_prev, inp):
        qc, kc, gc, vc = inp
        b = jnp.cumsum(gc, axis=2)
        o_inter = jnp.einsum('bhtd,bhde->bhte', qc * jnp.exp(b), S_prev)
        diff = jnp.where(tri[:, :, None], b[:, :, :, None, :] - b[:, :, None, :, :], NEG_INF)
        att = jnp.einsum('bhtsd,bhsd->bhts', qc[:, :, :, None, :] * jnp.exp(diff), kc)
        o_intra = jnp.einsum('bhts,bhse->bhte', att, vc)
        b_last = b[:, :, -1:, :]
        S_new = (jnp.exp(b_last[:, :, 0, :])[..., None] * S_prev
                 + jnp.einsum('bhsd,bhse->bhde', kc * jnp.exp(b_last - b), vc))
        return S_new, o_inter + o_intra

    S_fin, o = lax.scan(step, state, (chunks(q), chunks(k), chunks(logf), chunks(v)))
    return S_fin, o.transpose(1, 0, 3, 2, 4).reshape(B, T, C_H, C_DV)


def hgrn2_prep(cq, ci, cff, cfb, lb):
    B, T, _ = cq.shape
    shp = (B, T, C_H, C_DK)
    q = jax.nn.silu(cq).reshape(shp)
    i = ci.reshape(B, T, C_H, C_DV)
    lf_f = hgrn2_log_forget(cff, lb[0]).reshape(shp)
    lf_b = hgrn2_log_forget(cfb, lb[1]).reshape(shp)
    return q, i, lf_f, lf_b


def hgrn2_bidir(q, i, lf_f, lf_b, s_f, s_b):
    k_f = -jnp.expm1(lf_f)
    k_b = -jnp.expm1(lf_b)
    sf, o_f = hgrn2_scan(q, k_f, lf_f, i, s_f)
    rev = lambda t: jnp.flip(t, axis=1)
    sb, o_b = hgrn2_scan(rev(q), rev(k_b), rev(lf_b), rev(i), s_b)
    return sf, sb, o_f + rev(o_b)


def hgrn2_out(o, g, w):
    B, T = o.shape[:2]
    y = rms_norm(o, w) * jax.nn.silu(g.reshape(o.shape).astype(jnp.float32))
    return y.reshape(B, T, C_V_W).astype(g.dtype)


def merge_branches(o_a, o_b, o_c, gates, w_br, w_o):
    g = gates.reshape(gates.shape[:-1] + (N_BRANCH, D_MODEL))
    y = jax.nn.sigmoid(g[..., 0, :]) * (o_a @ w_br[0])
    y = y + jax.nn.sigmoid(g[..., 1, :]) * (o_b @ w_br[1])
    y = y + jax.nn.sigmoid(g[..., 2, :]) * (o_c @ w_br[2])
    return y @ w_o


def hybrid_mixer(h, hc, tabs, lb, w_in, sink, lam_par, lam_init, diff_w, hgrn_w, w_br, w_o, with_ctx):
    B, S, _ = h.shape
    L = hc.shape[1]
    (aq, ak, av, bq, bk, bv, cq, ci, cff, cfb, cg, gt) = jnp.split(h @ w_in, SPLIT_IDX, axis=-1)
    (aqc, akc, avc, bqc, bkc, bvc, cqc, cic, cffc, cfbc, cgc, gtc) = jnp.split(hc @ w_in, SPLIT_IDX, axis=-1)

    ka_c = akc.reshape(B, L, A_HKV, HEAD_DIM)
    va_c = avc.reshape(B, L, A_HKV, HEAD_DIM)
    o_a = window_gqa_latent(apply_axial_rope(aq.reshape(B, S, A_HQ, HEAD_DIM), tabs),
                            apply_axial_rope(ak.reshape(B, S, A_HKV, HEAD_DIM), tabs),
                            av.reshape(B, S, A_HKV, HEAD_DIM), ka_c, va_c, sink)

    lp = lam_par.astype(jnp.float32)
    lam = jnp.exp(jnp.sum(lp[0] * lp[1])) - jnp.exp(jnp.sum(lp[2] * lp[3])) + lam_init
    kb_c = bkc.reshape(B, L, B_H, 2, B_HD)
    vb_c = bvc.reshape(B, L, B_H, B_VD)
    o_b = diff_attn_latent(apply_axial_rope(bq.reshape(B, S, B_H, 2, B_HD), tabs),
                           apply_axial_rope(bk.reshape(B, S, B_H, 2, B_HD), tabs),
                           bv.reshape(B, S, B_H, B_VD), kb_c, vb_c, lam)
    o_b = diff_head_out(o_b, diff_w, lam_init)

    zero = jnp.zeros((B, C_H, C_DK, C_DV), jnp.float32)
    qcc, icc, lfc_f, lfc_b = hgrn2_prep(cqc, cic, cffc, cfbc, lb)
    s_f, s_b, oc_c = hgrn2_bidir(qcc, icc, lfc_f, lfc_b, zero, zero)
    q_c, i_c, lf_f, lf_b = hgrn2_prep(cq, ci, cff, cfb, lb)
    _, _, o_c = hgrn2_bidir(q_c, i_c, lf_f, lf_b, s_f, s_b)
    o_c = hgrn2_out(o_c, cg, hgrn_w)

    y = merge_branches(o_a, o_b, o_c, gt, w_br, w_o)
    if not with_ctx:
        return y, None
    oa_c = sink_attn_context(aqc.reshape(B, L, A_HQ, HEAD_DIM), ka_c, va_c, sink)
    ob_c = diff_head_out(diff_attend(bqc.reshape(B, L, B_H, 2, B_HD), kb_c, vb_c, lam), diff_w, lam_init)
    oc_c = hgrn2_out(oc_c, cgc, hgrn_w)
    yc = merge_branches(oa_c, ob_c, oc_c, gtc, w_br, w_o)
    return y, yc


def moe_ffn(h, w_router, b_router, w_gu, b_gu, w_dn, b_dn):
    N, D = h.shape
    NK = N * TOP_K
    logits = (h @ w_router + b_router).astype(jnp.float32)
    top_v, top_e = lax.top_k(logits, TOP_K)
    gates = jax.nn.softmax(top_v, axis=-1)
    flat_e = top_e.reshape(-1).astype(jnp.int32)
    order = jnp.argsort(flat_e)
    sorted_e = flat_e[order]
    counts = jnp.bincount(flat_e, length=N_EXPERTS).astype(jnp.int32)
    padded = (counts + MOE_BLOCK - 1) // MOE_BLOCK * MOE_BLOCK
    pad_end = jnp.cumsum(padded)
    pad_start = pad_end - padded
    grp_start = jnp.cumsum(counts) - counts
    rank = jnp.arange(NK, dtype=jnp.int32) - grp_start[sorted_e]
    dest = jnp.zeros((NK,), jnp.int32).at[order].set(pad_start[sorted_e] + rank)
    n_blk = (NK + MOE_BLOCK - 1) // MOE_BLOCK + N_EXPERTS
    R = n_blk * MOE_BLOCK
    buf = jnp.zeros((R, D), h.dtype).at[dest].set(jnp.repeat(h, TOP_K, axis=0))
    blk_e = jnp.clip(jnp.searchsorted(pad_end, jnp.arange(n_blk, dtype=jnp.int32) * MOE_BLOCK, side='right'),
                     0, N_EXPERTS - 1)

    def expert_block(args):
        xb, e = args
        gu = xb @ w_gu[e] + b_gu[e]
        g, u = gu[:, :EXPERT_FF], gu[:, EXPERT_FF:]
        g = jnp.minimum(g, SWIGLU_LIMIT)
        u = jnp.clip(u, -SWIGLU_LIMIT, SWIGLU_LIMIT)
        return ((u + 1) * (g * jax.nn.sigmoid(SWIGLU_ALPHA * g))) @ w_dn[e] + b_dn[e]

    y = lax.map(expert_block, (buf.reshape(n_blk, MOE_BLOCK, D), blk_e)).reshape(R, D)
    return jnp.sum(y[dest].reshape(N, TOP_K, D) * gates[..., None].astype(y.dtype), axis=1)


def setup_inputs(seed: int = 0) -> dict:
    key = jax.random.key(seed)
    ks = jax.random.split(key, 21)
    D = D_MODEL

    def nrm(k, shape, scale):
        return jax.random.normal(k, shape, jnp.float32) * scale

    return {
        "x": nrm(ks[0], (BATCH, SEQ, D), 1.0),
        "c": nrm(ks[1], (BATCH, D), 1.0),
        "ctx": nrm(ks[2], (BATCH, CTX_LEN, D), 1.0),
        "c_ctx": nrm(ks[3], (D,), 1.0),
        "w_mod": nrm(ks[4], (DEPTH, D, 6 * D), 0.3 * D ** -0.5),
        "b_mod": nrm(ks[5], (DEPTH, 6 * D), 0.02),
        "norm_g": 1.0 + nrm(ks[6], (DEPTH, 4, D), 0.02),
        "w_in": nrm(ks[7], (DEPTH, D, IN_W), D ** -0.5),
        "attn_sink": nrm(ks[8], (DEPTH, A_HQ), 0.5),
        "diff_lambda": nrm(ks[9], (DEPTH, 4, B_HD), 0.1),
        "diff_norm_w": 1.0 + nrm(ks[10], (DEPTH, B_VD), 0.02),
        "hgrn_lb_logits": nrm(ks[11], (DEPTH, 2, C_H * C_DK), 0.5),
        "hgrn_norm_w": 1.0 + nrm(ks[12], (DEPTH, C_DV), 0.02),
        "w_branch": nrm(ks[13], (DEPTH, N_BRANCH, BRANCH_W, D), BRANCH_W ** -0.5),
        "w_out": nrm(ks[14], (DEPTH, D, D), D ** -0.5),
        "w_router": nrm(ks[15], (DEPTH, D, N_EXPERTS), D ** -0.5),
        "b_router": nrm(ks[16], (DEPTH, N_EXPERTS), 0.01),
        "w_gate_up": nrm(ks[17], (DEPTH, N_EXPERTS, D, 2 * EXPERT_FF), D ** -0.5),
        "b_gate_up": nrm(ks[18], (DEPTH, N_EXPERTS, 2 * EXPERT_FF), 0.01),
        "w_down": nrm(ks[19], (DEPTH, N_EXPERTS, EXPERT_FF, D), EXPERT_FF ** -0.5),
        "b_down": nrm(ks[20], (DEPTH, N_EXPERTS, D), 0.01),
    }


def reference(x, c, ctx, c_ctx, w_mod, b_mod, norm_g, w_in, attn_sink, diff_lambda, diff_norm_w,
              hgrn_lb_logits, hgrn_norm_w, w_branch, w_out, w_router, b_router, w_gate_up, b_gate_up,
              w_down, b_down):
    B, S, D = x.shape
    L = ctx.shape[1]
    ROWS = S // GRID_W
    tabs = axial_rope_tables(ROWS, x.dtype)
    lb_cum = jnp.cumsum(jax.nn.softmax(hgrn_lb_logits.astype(jnp.float32), axis=0), axis=0)
    lower_bounds = lb_cum - lb_cum[0:1]
    xc = ctx
    for l in range(DEPTH):
        last = l == DEPTH - 1
        lam_init = 0.8 - 0.6 * math.exp(-0.3 * l)
        mod = (jax.nn.silu(c) @ w_mod[l] + b_mod[l]).reshape(B, 6, D)
        mod_c = (jax.nn.silu(c_ctx)[None] @ w_mod[l] + b_mod[l]).reshape(1, 6, D)
        h = modulate(x, norm_g[l, 0], mod[:, 0], mod[:, 1])
        hc = modulate(xc, norm_g[l, 0], mod_c[:, 0], mod_c[:, 1])
        y, yc = hybrid_mixer(h, hc, tabs, lower_bounds[l], w_in[l], attn_sink[l], diff_lambda[l], lam_init,
                             diff_norm_w[l], hgrn_norm_w[l], w_branch[l], w_out[l], not last)
        x = x + mod[:, 2][:, None, :] * rms_norm(y, norm_g[l, 1])
        h = modulate(x, norm_g[l, 2], mod[:, 3], mod[:, 4])
        if last:
            f = moe_ffn(h.reshape(B * S, D), w_router[l], b_router[l], w_gate_up[l], b_gate_up[l],
                        w_down[l], b_down[l]).reshape(B, S, D)
        else:
            xc = xc + mod_c[:, 2][:, None, :] * rms_norm(yc, norm_g[l, 1])
            hc = modulate(xc, norm_g[l, 2], mod_c[:, 3], mod_c[:, 4])
            tok = jnp.concatenate([h.reshape(B * S, D), hc.reshape(B * L, D)], axis=0)
            ft = moe_ffn(tok, w_router[l], b_router[l], w_gate_up[l], b_gate_up[l], w_down[l], b_down[l])
            f = ft[:B * S].reshape(B, S, D)
            fc = ft[B * S:].reshape(B, L, D)
            xc = xc + mod_c[:, 5][:, None, :] * rms_norm(fc, norm_g[l, 3])
        x = x + mod[:, 5][:, None, :] * rms_norm(f, norm_g[l, 3])
    return x
```

```python
import contextlib
import numpy as np
import ml_dtypes
import concourse.bass as bass
import concourse.mybir as mybir
from concourse.bass_utils import run_bass_kernel_spmd

F32 = mybir.dt.float32
BF16 = mybir.dt.bfloat16
I32 = mybir.dt.int32
AF = mybir.ActivationFunctionType
ALU = mybir.AluOpType
AX = mybir.AxisListType

EPOCH = 30000
DEPOCH = 1800
COMPUTE = ("pe", "act", "dve", "pool")
ALLENG = ("pe", "act", "dve", "pool", "sp")

D = 1024
NE = 32
CAP = 3072
RMS_EPS = 1e-6


class DSem:
    __slots__ = ("name", "ep", "cnt")

    def __init__(self, name):
        self.name = name
        self.ep = 0
        self.cnt = 0


class Buf:
    __slots__ = ("name", "w", "r", "ds")

    def __init__(self, name, ds=None):
        self.name = name
        self.w = {}
        self.r = {}
        self.ds = ds


class Sched:
    def __init__(self, nc, stack):
        self.nc = nc
        self.stack = stack
        self.streams = {e: [] for e in ALLENG}
        self.cnt = {e: 0 for e in COMPUTE}
        self.waited = {e: {} for e in ALLENG}
        self.sems = {}
        self.nsem = 0

    def sem(self, key):
        s = self.sems.get(key)
        if s is None:
            s = self.stack.enter_context(self.nc.semaphore("s%d" % self.nsem))
            self.nsem += 1
            self.sems[key] = s
        return s

    def _wait(self, eng, key, val):
        if val <= 0:
            return
        if eng == "pe" and key[0] == "pe":
            return
        w = self.waited[eng]
        if w.get(key, 0) >= val:
            return
        w[key] = val
        self.streams[eng].append(("w", key, val))

    def _deps(self, eng, reads, writes, partial):
        for b in reads:
            for k, v in b.w.items():
                self._wait(eng, k, v)
        for b in writes:
            for k, v in b.w.items():
                self._wait(eng, k, v)
            for k, v in b.r.items():
                self._wait(eng, k, v)
        for b in partial:
            for k, v in b.r.items():
                self._wait(eng, k, v)

    def _record(self, tok, reads, writes, partial):
        k, v = tok
        for b in reads:
            if b.r.get(k, 0) < v:
                b.r[k] = v
        for b in writes:
            b.w = {k: v}
            b.r = {}
        for b in partial:
            if b.w.get(k, 0) < v:
                b.w[k] = v

    def op(self, eng, fn, reads=(), writes=(), partial=()):
        self._deps(eng, reads, writes, partial)
        n = self.cnt[eng]
        self.cnt[eng] = n + 1
        key = (eng, n // EPOCH)
        val = n % EPOCH + 1
        self.sem(key)
        self.streams[eng].append(("o", fn, key, 1))
        self._record((key, val), reads, writes, partial)

    def dma(self, eng, fn, ds, reads=(), writes=(), partial=()):
        self._deps(eng, reads, writes, partial)
        key = ("d", ds.name, ds.ep)
        self._wait(eng, key, ds.cnt)
        if ds.cnt >= 16 * DEPOCH:
            ds.ep += 1
            ds.cnt = 0
            key = ("d", ds.name, ds.ep)
        self.sem(key)
        ds.cnt += 16
        self.streams[eng].append(("o", fn, key, 16))
        self._record((key, ds.cnt), reads, writes, partial)

    def barrier(self, dsems):
        toks = []
        for eng in COMPUTE:
            c = self.cnt[eng]
            if c > 0:
                toks.append(((eng, (c - 1) // EPOCH), (c - 1) % EPOCH + 1))
        for ds in dsems:
            if ds.cnt > 0:
                toks.append((("d", ds.name, ds.ep), ds.cnt))
        for e in ALLENG:
            for k, v in toks:
                self._wait(e, k, v)

    def wait_all(self, eng, bufs):
        for b in bufs:
            for k, v in b.w.items():
                self._wait(eng, k, v)
            for k, v in b.r.items():
                self._wait(eng, k, v)

    def emit(self):
        nc = self.nc
        engmap = {"pe": "tensor", "act": "scalar", "dve": "vector", "pool": "gpsimd", "sp": "sync"}
        sems = self.sems
        streams = self.streams
        with nc.Block() as block:
            def mk(ename):
                def body(e):
                    for item in streams[ename]:
                        if item[0] == "w":
                            e.wait_ge(sems[item[1]], item[2])
                        else:
                            item[1](e).then_inc(sems[item[2]], item[3])
                return body
            for ename in ALLENG:
                if streams[ename]:
                    getattr(block, engmap[ename])(mk(ename))
        self.ninstr = getattr(self, "ninstr", 0) + sum(len(v) for v in streams.values())
        self.streams = {e: [] for e in ALLENG}


C_AQ, C_AK, C_AV, C_BQ, C_BK, C_BV, C_CQ, C_CI, C_FF, C_FB, C_CG, C_GT = (
    0, 512, 640, 768, 1280, 1792, 2304, 2816, 3328, 3840, 4352, 4864)
IN_W = 7936
P_AQ, P_AK, P_BQ, P_BK = 0, 512, 640, 1152
ROPE_W = 1664


def build(nc, S_, L_, dbg=(), nlayers=2):
    T = S_ + L_
    NT = T // 128
    NCT = L_ // 128
    NLT = S_ // 128
    tok_blocks = [(0, L_)] + [(L_ + 512 * i, 512) for i in range(S_ // 512)]
    NSLOT = NE * CAP

    def din(name, shape, dt=F32):
        return nc.dram_tensor(name, list(shape), dt, kind="ExternalInput").ap()

    def dscr(name, shape, dt):
        kind = "ExternalOutput" if name in dbg else "Internal"
        return nc.dram_tensor(name, list(shape), dt, kind=kind).ap()

    xin = din("xin", [S_, D]); ctxin = din("ctxin", [L_, D])
    c_col = din("c_col", [128, 8]); cc_col = din("cc_col", [128, 8])
    w_mod = din("w_mod", [2, D, 6 * D]); b_mod = din("b_mod", [2, 6 * D]); norm_g = din("norm_g", [2, 4 * D])
    w_in = din("w_in", [2, D, IN_W]); w_inp = din("w_inp", [2, D, ROPE_W])
    sink_in = din("sink", [2, 8]); dlam = din("dlam", [2, 256]); dnw = din("dnw", [2, 128])
    lbl = din("lbl", [2, 1024]); lbl_fm = din("lbl_fm", [2, 128, 8]); hnw = din("hnw", [2, 128])
    w_br = din("w_br", [2, 3, 512, D]); w_out = din("w_out", [2, D, D])
    w_router = din("w_router", [2, D, NE]); b_router = din("b_router", [2, NE])
    w_gu = din("w_gu", [2, NE, D, 2 * D]); b_gu_t = din("b_gu_t", [2, NE, 128, 16])
    w_dn = din("w_dn", [2, NE, D, D]); b_dn = din("b_dn", [2, NE, D])
    ropc = din("ropc", [128, T]); rops = din("rops", [128, T])
    cst_f = din("cst_f", [128, 8, 128])
    cst_b = din("cst_b", [128, 12, 128], BF16)
    rowmask_in = din("rowmask", [128, 4])
    out = nc.dram_tensor("out", [S_, D], F32, kind="ExternalOutput").ap()

    XR = dscr("XR", [T, D], F32)
    HT = dscr("HT", [8, 128, T], BF16)
    QA = dscr("QA", [8, 64, T], BF16); QAN = dscr("QAN", [8, T], F32)
    KA = dscr("KA", [2, 64, T], BF16); KAN = dscr("KAN", [2, T], F32)
    VA = dscr("VA", [T, 128], BF16)
    QB = dscr("QB", [8, 64, T], BF16); QBN = dscr("QBN", [8, T], F32)
    KB = dscr("KB", [8, 64, T], BF16); KBN = dscr("KBN", [8, T], F32)
    VB = dscr("VB", [T, 512], BF16)
    KMX = dscr("KMX", [10, 1], F32)
    HQ = dscr("HQ", [4, 128, T], BF16); HK = dscr("HK", [8, 128, T], BF16)
    HKt = dscr("HKt", [T, 1024], BF16); HG = dscr("HG", [T, 1024], F32)
    HV = dscr("HV", [T, 512], BF16); HGT = dscr("HGT", [T, 512], BF16)
    GT = dscr("GT", [T, 3072], BF16)
    OA = dscr("OA", [T, 512], BF16); OB = dscr("OB", [T, 512], BF16); OC = dscr("OC", [T, 512], BF16)
    OF = dscr("OF", [T, 512], F32)
    XBUF = dscr("XBUF", [NSLOT, D], BF16); YBUF = dscr("YBUF", [NSLOT, D], BF16)

    with contextlib.ExitStack() as st:
        S = Sched(nc, st)
        DS = {}
        uniq = [0]

        def SB(name, shape, dt, stack=None):
            uniq[0] += 1
            t = (stack or st).enter_context(nc.sbuf_tensor("%s_%d" % (name, uniq[0]), list(shape), dt))
            ds = DS.get(name)
            if ds is None:
                ds = DS[name] = DSem(name)
            return t, Buf(name, ds)

        class Pool:
            def __init__(self, name, shape, dt, n, stack=None):
                self.items = [SB("%s%d" % (name, i), shape, dt, stack) for i in range(n)]
                self.i = 0

            def next(self):
                it = self.items[self.i % len(self.items)]
                self.i += 1
                return it

        dB = {n: Buf(n) for n in ["XR", "HT", "QA", "QAN", "KA", "KAN", "VA", "QB", "QBN", "KB", "KBN", "VB", "KMX",
                                  "HQ", "HK", "HKt", "HG", "HV", "HGT", "GT", "OA", "OB", "OC", "OF", "XBUF", "YBUF", "out"]}

        def ld(eng, dst, src, dbuf, rd=()):
            S.dma(eng, lambda e: e.dma_start(out=dst, in_=src), dbuf.ds, reads=rd, writes=[dbuf])

        def stq(eng, dst, src, sbuf, dram):
            S.dma("act", lambda e: e.dma_start(out=dst, in_=src), sbuf.ds, reads=[sbuf], partial=[dram])

        def mm(o, lhsT, rhs, first, last, rd, pb):
            S.op("pe", lambda e: e.matmul(o, lhsT=lhsT, rhs=rhs, start=first, stop=last), reads=rd,
                 writes=[pb] if first else (), partial=() if first else [pb])

        def mmp(o, lhsT, rhs, first, last, rd, pb):
            S.op("pe", lambda e: e.matmul(o, lhsT=lhsT, rhs=rhs, start=first, stop=last), reads=rd, partial=[pb])

        def tr(o, i_, idt, rd, pb, first):
            S.op("pe", lambda e: e.transpose(out=o, in_=i_, identity=idt), reads=rd,
                 writes=[pb] if first else (), partial=() if first else [pb])

        def act(o, i_, func, rd, wr, eng="act", **kw):
            S.op(eng, lambda e: e.activation(out=o, in_=i_, func=func, **kw), reads=rd, writes=wr)

        def tt(eng, o, a, b, op, rd, wr, pr=()):
            S.op(eng, lambda e: e.tensor_tensor(out=o, in0=a, in1=b, op=op), reads=rd, writes=wr, partial=pr)

        def ts(eng, o, a, s1, s2, op0, op1, rd, wr, pr=(), **kw):
            if s2 is None:
                S.op(eng, lambda e: e.tensor_scalar(out=o, in0=a, scalar1=s1, scalar2=None, op0=op0, **kw), reads=rd, writes=wr, partial=pr)
            else:
                S.op(eng, lambda e: e.tensor_scalar(out=o, in0=a, scalar1=s1, scalar2=s2, op0=op0, op1=op1, **kw), reads=rd, writes=wr, partial=pr)

        def stt(o, a, s, b, op0, op1, rd, wr, pr=()):
            S.op("dve", lambda e: e.scalar_tensor_tensor(out=o, in0=a, scalar=s, in1=b, op0=op0, op1=op1), reads=rd, writes=wr, partial=pr)

        def cp(eng, o, i_, rd, wr, pr=()):
            if eng == "act":
                S.op("act", lambda e: e.copy(out=o, in_=i_), reads=rd, writes=wr, partial=pr)
            else:
                S.op(eng, lambda e: e.tensor_copy(out=o, in_=i_), reads=rd, writes=wr, partial=pr)

        def memset(eng, o, v, wr):
            S.op(eng, lambda e: e.memset(o, v), writes=wr)

        CF, bCF = SB("CF", [128, 8, 128], F32)
        CB, bCB = SB("CB", [128, 12, 128], BF16)
        RM, bRM = SB("RM", [128, 4], F32)
        ld("sp", CF[:], cst_f, bCF); ld("sp", CB[:], cst_b, bCB); ld("sp", RM[:], rowmask_in, bRM)
        UF_f, UB_f, DF_f, DB_f, UTS_f, ONES_f, ID_f, EBASE = [CF[:, i, :] for i in range(8)]
        ID_b, MPREV, MNEXT, UF_b, UB_b = [CB[:, i, :] for i in range(5)]
        BDC = CB[:, 5:9, :]
        BONES = CB[:, 9, 0:2]

        PS = [(st.enter_context(nc.psum_tensor("ps%d" % i, [128, 512], F32)), Buf("ps%d" % i)) for i in range(8)]

        MODL, bMODL = SB("MODL", [128, 6, D], F32)
        MODC, bMODC = SB("MODC", [128, 6, D], F32)
        eps_t, beps = SB("eps_t", [128, 1], F32)
        memset("pool", eps_t[:], RMS_EPS, [beps])

        WK4 = Pool("wk4", [128, D], F32, 4)
        WKB = Pool("wkb", [128, D], BF16, 4)
        SM = Pool("sm", [128, 16], F32, 8)
        phase = [None]

        def new_phase():
            if phase[0] is not None:
                S.barrier(list(DS.values()))
                S.emit()
                phase[0].close()
            phase[0] = contextlib.ExitStack()
            return phase[0]

        def rstd_from_ss(ss_ap, bss, n):
            t, b = SM.next()
            S.op("act", lambda e: e.activation(out=t[:, 0:1], in_=ss_ap, func=AF.Sqrt, bias=eps_t[:, 0:1], scale=1.0 / n),
                 reads=[bss, beps], writes=[b])
            t2, b2 = SM.next()
            S.op("dve", lambda e: e.reciprocal(out=t2[:, 0:1], in_=t[:, 0:1]), reads=[b], writes=[b2])
            return t2[:, 0:1], b2

        def norm_mod_T(xt, bx, ms, bms, gs, shs, t0, keep=None):
            junk, bj = WK4.next()
            ss, bss = SM.next()
            act(junk[:], xt[:], AF.Square, [bx], [bj, bss], accum_out=ss[:, 0:1])
            rs, brs = rstd_from_ss(ss[:, 0:1], bss, D)
            tmp, btmp = WK4.next()
            stt(tmp[:], xt[:], rs, ms[:, gs, :], ALU.mult, ALU.mult, [bx, brs, bms], [btmp])
            hb, bhb = WKB.next()
            tt("pool", hb[:], tmp[:], ms[:, shs, :], ALU.add, [btmp, bms], [bhb])
            if keep is not None:
                tt("dve", keep[0][:], tmp[:], ms[:, shs, :], ALU.add, [btmp, bms], [keep[1]])
            pt, bpt = PS[7]
            ptb = pt[:].bitcast(BF16)
            for kc in range(8):
                tr(ptb[:, kc * 128:(kc + 1) * 128], hb[:, kc * 128:(kc + 1) * 128], ID_b, [bhb, bCB], bpt, kc == 0)
            hT, bhT = WKB.next()
            cp("act", hT[:], ptb, [bpt], [bhT])
            stq("sp", HT[:, :, t0:t0 + 128].rearrange("k p t -> p k t"), hT[:].rearrange("p (k t) -> p k t", k=8), bhT, dB["HT"])
            return hb, bhb

        for l in range(nlayers):
            lam_init = 0.8 - 0.6 * float(np.exp(-0.3 * l))
            if phase[0] is not None:
                S.barrier(list(DS.values())); S.emit(); phase[0].close(); phase[0] = None
            lay = contextlib.ExitStack()
            LBT, bLBT = SB("LBT", [128, 1024], F32, lay)
            OMT, bOMT = SB("OMT", [128, 1024], F32, lay)
            LBF, bLBF = SB("LBF", [128, 8], F32, lay)
            OMF, bOMF = SB("OMF", [128, 8], F32, lay)
            DL, bDL = SB("DL", [128, 256], F32, lay)
            LV, bLV = SB("LV", [128, 8], F32, lay)
            DNW, bDNW = SB("DNW", [128, 128], F32, lay)
            HNW, bHNW = SB("HNW", [128, 128], F32, lay)
            SK, bSK = SB("SK", [128, 8], F32, lay)
            KMB, bKMB = SB("KMB", [128, 10], F32, lay)
            NKM, bNKM = SB("NKM", [128, 10], F32, lay)
            NKM8, bNKM8 = SB("NKM8", [128, 10], F32, lay)
            RS_, bRS = SB("RS", [128, NE], F32, lay)
            DEST, bDEST = SB("DEST", [128, NT + 1, 4], I32, lay)
            G4, bG4 = SB("G4", [128, NT, 4], F32, lay)
            ph = new_phase()
            W16a, bW16a = SB("W16a", [128, 8, 512], F32, ph)
            W16b, bW16b = SB("W16b", [128, 8, 512], F32, ph)
            cs, bcs = SB("cs", [128, 16], F32, ph)
            ld("sp", cs[:, 0:8], c_col, bcs); ld("sp", cs[:, 8:16], cc_col, bcs)
            act(cs[:], cs[:], AF.Silu, [bcs], [bcs])
            LH, bLH = SB("LH", [128, 16, 128], F32, ph)
            for j in range(16):
                cp("pool", LH[:, j, :], cs[:, j:j + 1].to_broadcast([128, 128]), [bcs], (), pr=[bLH])
            NG, bNG = W16b, bW16b
            for cb in range(12):
                ld("sp", W16a[:], w_mod[l, :, cb * 512:(cb + 1) * 512].rearrange("(k p) n -> p k n", p=128), bW16a)
                bm, bbm = WK4.next()
                ld("sp", bm[:, 0:512], b_mod[l, cb * 512:(cb + 1) * 512].partition_broadcast(128), bbm)
                for si, (ms, bms) in enumerate(((MODL, bMODL), (MODC, bMODC))):
                    pp, bpp = PS[si]
                    for kc in range(8):
                        mm(pp[:], LH[:, si * 8 + kc, :], W16a[:, kc, :], kc == 0, kc == 7, [bLH, bW16a], bpp)
                    tt("dve", ms[:, cb // 2, (cb % 2) * 512:(cb % 2) * 512 + 512], pp[:], bm[:, 0:512], ALU.add, [bpp, bbm], (), pr=[bms])
            ld("sp", NG[:].rearrange("p k n -> p (k n)"), norm_g[l].partition_broadcast(128), bNG)
            NGv = NG[:].rearrange("p k n -> p (k n)")
            for ms, bms in ((MODL, bMODL), (MODC, bMODC)):
                stt(ms[:, 1, :], ms[:, 1, :], 1.0, NGv[:, 0:D], ALU.add, ALU.mult, [bms, bNG], [bms])
                tt("dve", ms[:, 2, :], ms[:, 2, :], NGv[:, D:2 * D], ALU.mult, [bms, bNG], [bms])
                stt(ms[:, 4, :], ms[:, 4, :], 1.0, NGv[:, 2 * D:3 * D], ALU.add, ALU.mult, [bms, bNG], [bms])
                tt("dve", ms[:, 5, :], ms[:, 5, :], NGv[:, 3 * D:4 * D], ALU.mult, [bms, bNG], [bms])

            if l == 0:
                memset("pool", LBT[:], 0.0, [bLBT]); memset("pool", OMT[:], 1.0, [bOMT])
                memset("pool", LBF[:], 0.0, [bLBF]); memset("pool", OMF[:], 1.0, [bOMF])
            else:
                t1, b1 = WK4.next()
                ld("sp", LBT[:], lbl[1].partition_broadcast(128), bLBT)
                ld("sp", t1[:], lbl[0].partition_broadcast(128), b1)
                tt("dve", LBT[:], LBT[:], t1[:], ALU.subtract, [bLBT, b1], [bLBT])
                act(LBT[:], LBT[:], AF.Sigmoid, [bLBT], [bLBT])
                ts("dve", OMT[:], LBT[:], -1.0, 1.0, ALU.mult, ALU.add, [bLBT], [bOMT])
                t2, b2 = SM.next()
                ld("sp", LBF[:], lbl_fm[1], bLBF); ld("sp", t2[:, 0:8], lbl_fm[0], b2)
                tt("dve", LBF[:], LBF[:], t2[:, 0:8], ALU.subtract, [bLBF, b2], [bLBF])
                act(LBF[:], LBF[:], AF.Sigmoid, [bLBF], [bLBF])
                ts("dve", OMF[:], LBF[:], -1.0, 1.0, ALU.mult, ALU.add, [bLBF], [bOMF])
            ld("sp", DL[:], dlam[l].partition_broadcast(128), bDL)
            tt("dve", DL[:, 0:64], DL[:, 0:64], DL[:, 64:128], ALU.mult, [bDL], [bDL])
            tt("dve", DL[:, 128:192], DL[:, 128:192], DL[:, 192:256], ALU.mult, [bDL], [bDL])
            S.op("dve", lambda e: e.reduce_sum(out=LV[:, 0:1], in_=DL[:, 0:64], axis=AX.X), reads=[bDL], writes=[bLV])
            S.op("dve", lambda e: e.reduce_sum(out=LV[:, 1:2], in_=DL[:, 128:192], axis=AX.X), reads=[bDL], partial=[bLV])
            act(LV[:, 0:2], LV[:, 0:2], AF.Exp, [bLV], [bLV])
            tt("dve", LV[:, 2:3], LV[:, 1:2], LV[:, 0:1], ALU.subtract, [bLV], [bLV])
            ts("dve", LV[:, 3:4], LV[:, 2:3], -lam_init, None, ALU.add, None, [bLV], [bLV])
            NEGLAM = LV[:, 3:4]
            ld("sp", DNW[:], dnw[l].partition_broadcast(128), bDNW)
            ts("dve", DNW[:], DNW[:], 1.0 - lam_init, None, ALU.mult, None, [bDNW], [bDNW])
            ld("sp", HNW[:], hnw[l].partition_broadcast(128), bHNW)
            ld("sp", SK[:], sink_in[l].partition_broadcast(128), bSK)

            ph = new_phase()
            if l == 0:
                S.dma("sp", lambda e: e.dma_start(out=XR[0:L_, :], in_=ctxin), bCF.ds, partial=[dB["XR"]])
                for r0 in range(0, S_, 512):
                    S.dma("sp", lambda e, r0=r0: e.dma_start(out=XR[L_ + r0:L_ + r0 + 512, :], in_=xin[r0:r0 + 512, :]), bCB.ds, partial=[dB["XR"]])
            for i in range(NT):
                t0 = 128 * i
                ms, bms = (MODC, bMODC) if i < NCT else (MODL, bMODL)
                xt, bx = WK4.next()
                ld("sp", xt[:], XR[t0:t0 + 128, :], bx, [dB["XR"]])
                norm_mod_T(xt, bx, ms, bms, 1, 0, t0)

            ph = new_phase()
            WB8 = Pool("wb8", [128, 8, 512], BF16, 2, ph)
            HB = Pool("hb", [128, 8, 512], BF16, 3, ph)
            fm_jobs = []
            for i in range(4):
                fm_jobs.append((C_AQ + 128 * i, P_AQ + 128 * i, "rope", (QA, QAN, "QA", "QAN", 2 * i)))
            fm_jobs.append((C_AK, P_AK, "rope", (KA, KAN, "KA", "KAN", 0)))
            for i in range(4):
                fm_jobs.append((C_BQ + 128 * i, P_BQ + 128 * i, "rope", (QB, QBN, "QB", "QBN", 2 * i)))
            for i in range(4):
                fm_jobs.append((C_BK + 128 * i, P_BK + 128 * i, "rope", (KB, KBN, "KB", "KBN", 2 * i)))
            for i in range(4):
                fm_jobs.append((C_CQ + 128 * i, None, "silu", i))
            for i in range(4):
                fm_jobs.append((C_FF + 128 * i, None, "kfm", i))
            for i in range(4):
                fm_jobs.append((C_FB + 128 * i, None, "kfm", 4 + i))
            for (c0, p0, kind, info) in fm_jobs:
                wt, bwt = WB8.next()
                ld("pool", wt[:, :, 0:128], w_in[l, :, c0:c0 + 128].rearrange("(k p) n -> p k n", p=128), bwt)
                if kind == "rope":
                    ld("pool", wt[:, :, 128:256], w_inp[l, :, p0:p0 + 128].rearrange("(k p) n -> p k n", p=128), bwt)
                for (t0, n) in tok_blocks:
                    hb, bhb = HB.next()
                    ld("sp", hb[:, :, 0:n], HT[:, :, t0:t0 + n].rearrange("k p t -> p k t"), bhb, [dB["HT"]])
                    pa, bpa = PS[0]
                    for kc in range(8):
                        mm(pa[:, 0:n], wt[:, kc, 0:128], hb[:, kc, 0:n], kc == 0, kc == 7, [bwt, bhb], bpa)
                    if kind == "rope":
                        Qd, Nd, qn, nn, h0 = info
                        pb_, bpb = PS[1]
                        for kc in range(8):
                            mm(pb_[:, 0:n], wt[:, kc, 128:256], hb[:, kc, 0:n], kc == 0, kc == 7, [bwt, bhb], bpb)
                        rc, brc = WK4.next()
                        ld("sp", rc[:, 0:n], ropc[:, t0:t0 + n], brc); ld("sp", rc[:, 512:512 + n], rops[:, t0:t0 + n], brc)
                        t1, b1 = WK4.next()
                        tt("dve", t1[:, 0:n], pa[:, 0:n], rc[:, 0:n], ALU.mult, [bpa, brc], [b1])
                        tt("dve", t1[:, 512:512 + n], pb_[:, 0:n], rc[:, 512:512 + n], ALU.mult, [bpb, brc], (), pr=[b1])
                        qt, bqt = WKB.next()
                        tt("pool", qt[:, 0:n], t1[:, 0:n], t1[:, 512:512 + n], ALU.add, [b1], [bqt])
                        sq, bsq = WKB.next()
                        act(sq[:, 0:n], qt[:, 0:n], AF.Square, [bqt], [bsq])
                        pn, bpn = PS[2]
                        mm(pn[0:2, 0:n], BONES, sq[:, 0:n], True, True, [bCB, bsq], bpn)
                        nr, bnr = WK4.next()
                        act(nr[0:2, 0:n], pn[0:2, 0:n], AF.Sqrt, [bpn], [bnr])
                        stq("sp", Nd[h0:h0 + 2, t0:t0 + n], nr[0:2, 0:n], bnr, dB[nn])
                        stq("sp", Qd[h0, :, t0:t0 + n], qt[0:64, 0:n], bqt, dB[qn])
                        stq("sp", Qd[h0 + 1, :, t0:t0 + n], qt[64:128, 0:n], bqt, dB[qn])
                    elif kind == "silu":
                        qt, bqt = WKB.next()
                        act(qt[:, 0:n], pa[:, 0:n], AF.Silu, [bpa], [bqt])
                        stq("sp", HQ[info, :, t0:t0 + n], qt[:, 0:n], bqt, dB["HQ"])
                    else:
                        t1, b1 = WK4.next()
                        act(t1[:, 0:n], pa[:, 0:n], AF.Sigmoid, [bpa], [b1], scale=-1.0)
                        qt, bqt = WKB.next()
                        ts("dve", qt[:, 0:n], t1[:, 0:n], OMF[:, info:info + 1], None, ALU.mult, None, [b1, bOMF], [bqt])
                        stq("sp", HK[info, :, t0:t0 + n], qt[:, 0:n], bqt, dB["HK"])

            tm_jobs = [(C_AV, 128, "copy", (VA, "VA", 0)), (C_BV, 512, "copy", (VB, "VB", 0)), (C_CI, 512, "copy", (HV, "HV", 0)),
                       (C_CG, 512, "silu", (HGT, "HGT", 0)), (C_FF, 512, "logf", 0), (C_FB, 512, "logf", 1)]
            for j in range(6):
                tm_jobs.append((C_GT + 512 * j, 512, "sigm", (GT, "GT", 512 * j)))
            for (c0, w, kind, info) in tm_jobs:
                wt, bwt = WB8.next()
                ld("pool", wt[:, :, 0:w], w_in[l, :, c0:c0 + w].rearrange("(k p) n -> p k n", p=128), bwt)
                for (t0, n) in tok_blocks:
                    hb, bhb = HB.next()
                    ld("sp", hb[:, :, 0:n], HT[:, :, t0:t0 + n].rearrange("k p t -> p k t"), bhb, [dB["HT"]])
                    for s in range(n // 128):
                        tt0 = t0 + 128 * s
                        pa, bpa = PS[s % 2]
                        for kc in range(8):
                            mm(pa[:, 0:w], hb[:, kc, 128 * s:128 * s + 128], wt[:, kc, 0:w], kc == 0, kc == 7, [bwt, bhb], bpa)
                        if kind in ("copy", "silu", "sigm"):
                            dst, dn, co = info
                            ot, bot = WKB.next()
                            fn = {"copy": AF.Copy, "silu": AF.Silu, "sigm": AF.Sigmoid}[kind]
                            act(ot[:, 0:w], pa[:, 0:w], fn, [bpa], [bot])
                            stq("sp", dst[tt0:tt0 + 128, co:co + w], ot[:, 0:w], bot, dB[dn])
                        else:
                            d_ = info
                            t1, b1 = WK4.next()
                            act(t1[:, 0:512], pa[:, 0:512], AF.Sigmoid, [bpa], [b1])
                            tt("dve", t1[:, 0:512], t1[:, 0:512], OMT[:, d_ * 512:(d_ + 1) * 512], ALU.mult, [b1, bOMT], [b1])
                            tt("dve", t1[:, 0:512], t1[:, 0:512], LBT[:, d_ * 512:(d_ + 1) * 512], ALU.add, [b1, bLBT], [b1])
                            kt, bkt = WKB.next()
                            ts("dve", kt[:, 0:512], t1[:, 0:512], -1.0, 1.0, ALU.mult, ALU.add, [b1], [bkt])
                            stq("sp", HKt[tt0:tt0 + 128, d_ * 512:(d_ + 1) * 512], kt[:, 0:512], bkt, dB["HKt"])
                            act(t1[:, 512:1024], t1[:, 0:512], AF.Ln, [b1], [b1])
                            stq("sp", HG[tt0:tt0 + 128, d_ * 512:(d_ + 1) * 512], t1[:, 512:1024], b1, dB["HG"])

            ph = new_phase()
            kmrow, bkmrow = SB("kmrow", [1, T], F32, ph)
            kmv, bkmv = SB("kmv", [1, 16], F32, ph)
            for ku in range(10):
                src = KAN[ku:ku + 1, :] if ku < 2 else KBN[ku - 2:ku - 1, :]
                ld("sp", kmrow[:], src, bkmrow, [dB["KAN"], dB["KBN"]])
                S.op("dve", lambda e, ku=ku: e.reduce_max(out=kmv[:, ku:ku + 1], in_=kmrow[:], axis=AX.X), reads=[bkmrow], partial=[bkmv])
            stq("sp", KMX.rearrange("a b -> b a"), kmv[:, 0:10], bkmv, dB["KMX"])
            ld("sp", KMB[:], KMX.rearrange("a b -> (a b)").partition_broadcast(128), bKMB, [dB["KMX"]])
            ts("dve", NKM[:], KMB[:], -1.0, None, ALU.mult, None, [bKMB], [bNKM])
            ts("dve", NKM8[:], KMB[:], -0.125, None, ALU.mult, None, [bKMB], [bNKM8])

            ph = new_phase()
            KAUG = [SB("kaug%d" % i, [65, T], BF16, ph) for i in range(2)]
            VS, bVS = SB("vs", [128, NT, 132], BF16, ph)
            memset("pool", VS[:], 1.0, [bVS])
            QAUG = Pool("qaug", [65, 512], BF16, 2, ph)
            QNR = Pool("qnr", [65, 512], F32, 2, ph)
            PT = Pool("pt", [128, 512], BF16, 3, ph)
            OT = Pool("ot", [128, 512], F32, 2, ph)
            OT2 = Pool("ot2", [128, 512], F32, 2, ph)
            for i in range(2):
                memset("pool", KAUG[i][0][64:65, :], 1.0, [KAUG[i][1]])

            def load_k(slot, Kd, kname, ku):
                kt_, bk_ = KAUG[slot]
                S.dma("sp", lambda e: e.dma_start(out=kt_[0:64, :], in_=Kd[ku]), bk_.ds, reads=[dB[kname]], writes=[bk_])

            def load_v(Vd, vname, v0, dv):
                for j0 in range(0, NT, 11):
                    j1 = min(NT, j0 + 11)
                    S.dma("sp", lambda e, j0=j0, j1=j1: e.dma_start(out=VS[:, j0:j1, 0:dv], in_=Vd[128 * j0:128 * j1, v0:v0 + dv].rearrange("(j p) d -> p j d", p=128)),
                          bVS.ds, reads=[dB[vname]], writes=[bVS] if j0 == 0 else (), partial=() if j0 == 0 else [bVS])
                memset("pool", VS[:, :, dv:dv + 1], 1.0, [bVS])

            def attn_unit(Qd, Nd, qname, nname, u, kslot, kmcol, t0, n, keys, dv, accs):
                kt_, bk_ = KAUG[kslot]
                qa, bqa = QAUG.next()
                ld("sp", qa[0:64, 0:n], Qd[u, :, t0:t0 + n], bqa, [dB[qname]])
                qn, bqn = QNR.next()
                ld("sp", qn[64:65, 0:n], Nd[u:u + 1, t0:t0 + n], bqn, [dB[nname]])
                S.op("act", lambda e: e.activation(out=qa[64:65, 0:n], in_=qn[64:65, 0:n], func=AF.Copy, scale=NKM[64:65, kmcol:kmcol + 1]),
                     reads=[bqn, bNKM], partial=[bqa])
                nk = len(keys)
                for ki, (j, mask) in enumerate(keys):
                    pS, bpS = PS[ki % 2]
                    mm(pS[:, 0:n], kt_[:, 128 * j:128 * j + 128], qa[:, 0:n], True, True, [bk_, bqa], bpS)
                    pt_, bpt_ = PT.next()
                    act(pt_[:, 0:n], pS[:, 0:n], AF.Exp, [bpS], [bpt_], scale=0.125)
                    if mask is not None:
                        tt("pool", pt_[:, 0:n], pt_[:, 0:n], mask, ALU.mult, [bpt_, bCB], [bpt_])
                    for s in range(n // 128):
                        ao, ab = accs[s]
                        mmp(ao, pt_[:, 128 * s:128 * s + 128], VS[:, j, 0:dv + 1], ki == 0, ki == nk - 1, [bpt_, bVS], ab)

            def acc_views(dv, banks):
                res = []
                for s in range(4):
                    p, b = PS[banks[s // 2]]
                    res.append((p[:, (s % 2) * 256:(s % 2) * 256 + dv + 1], b))
                return res

            for ku in range(2):
                load_k(0, KA, "KA", ku)
                load_v(VA, "VA", 64 * ku, 64)
                for g in range(4):
                    hq = 4 * ku + g
                    for i in range(NT):
                        t0 = 128 * i
                        if i < NCT:
                            keys = [(j, None) for j in range(NCT)]
                        else:
                            keys = [(j, None) for j in range(NCT)]
                            if i > NCT:
                                keys.append((i - 1, MPREV))
                            keys.append((i, None))
                            if i < NT - 1:
                                keys.append((i + 1, MNEXT))
                        accs = acc_views(64, (2 + (i % 2) * 2, 3 + (i % 2) * 2))
                        pass
                        attn_unit(QA, QAN, "QA", "QAN", hq, 0, ku, t0, 128, keys, 64, accs)
                        qn, bqn = SM.next()
                        S.dma("sp", lambda e, qn=qn, t0=t0, hq=hq: e.dma_start(out=qn[:, 0:1], in_=QAN[hq, t0:t0 + 128].rearrange("(p o) -> p o", o=1)),
                              bqn.ds, reads=[dB["QAN"]], writes=[bqn])
                        es, bes = SM.next()
                        S.op("act", lambda e, es=es, qn=qn, ku=ku, hq=hq: e.activation(out=es[:, 0:1], in_=qn[:, 0:1], func=AF.Exp,
                                                                                        scale=NKM8[:, ku:ku + 1], bias=SK[:, hq:hq + 1]),
                             reads=[bqn, bNKM8, bSK], writes=[bes])
                        ao, ab = accs[0]
                        tt("dve", es[:, 1:2], es[:, 0:1], ao[:, 64:65], ALU.add, [bes, ab], [bes])
                        S.op("dve", lambda e, es=es: e.reciprocal(out=es[:, 2:3], in_=es[:, 1:2]), reads=[bes], writes=[bes])
                        o_, bo_ = WKB.next()
                        ts("dve", o_[:, 0:64], ao[:, 0:64], es[:, 2:3], None, ALU.mult, None, [ab, bes], [bo_])
                        stq("sp", OA[t0:t0 + 128, 64 * hq:64 * hq + 64], o_[:, 0:64], bo_, dB["OA"])

            for h in range(4):
                load_k(0, KB, "KB", 2 * h); load_k(1, KB, "KB", 2 * h + 1)
                load_v(VB, "VB", 128 * h, 128)
                for bi, (t0, n) in enumerate(tok_blocks):
                    keys = [(j, None) for j in range(NCT)] if t0 < L_ else [(j, None) for j in range(NT)]
                    O0, bO0 = OT.next()
                    for c in range(2):
                        accs = [(PS[2 + s_][0][:, 0:129], PS[2 + s_][1]) for s_ in range(4)]
                        attn_unit(QB, QBN, "QB", "QBN", 2 * h + c, c, 2 + 2 * h + c, t0, n, keys, 128, accs)
                        for s in range(n // 128):
                            a_, b_ = accs[s]
                            r, br = SM.next()
                            S.op("dve", lambda e, r=r, a_=a_: e.reciprocal(out=r[:, 0:1], in_=a_[:, 128:129]), reads=[b_], writes=[br])
                            if c == 0:
                                ts("dve", O0[:, 128 * s:128 * s + 128], a_[:, 0:128], r[:, 0:1], None, ALU.mult, None, [b_, br], (), pr=[bO0])
                            else:
                                tt("dve", r[:, 1:2], r[:, 0:1], NEGLAM, ALU.mult, [br, bLV], [br])
                                o0, bo0 = OT2.next()
                                stt(o0[:, 128:256], a_[:, 0:128], r[:, 1:2], O0[:, 128 * s:128 * s + 128], ALU.mult, ALU.add, [b_, br, bO0], [bo0])
                                ss, bss = SM.next()
                                act(o0[:, 256:384], o0[:, 128:256], AF.Square, [bo0], [bo0, bss], accum_out=ss[:, 0:1])
                                rs, brs = rstd_from_ss(ss[:, 0:1], bss, 128)
                                ob_, bob = WKB.next()
                                stt(ob_[:, 0:128], o0[:, 128:256], rs, DNW[:], ALU.mult, ALU.mult, [bo0, brs, bDNW], [bob])
                                tt0 = t0 + 128 * s
                                stq("sp", OB[tt0:tt0 + 128, 128 * h:128 * h + 128], ob_[:, 0:128], bob, dB["OB"])

            ph = new_phase()
            SF = [SB("sf%d" % h, [128, 128], F32, ph) for h in range(4)]
            SBF = [Pool("sbf%d_" % h, [128, 128], BF16, 2, ph) for h in range(4)]
            GP = Pool("gp", [128, 512], F32, 2, ph)
            QTp = Pool("qtp", [128, 512], BF16, 2, ph)
            KTp = Pool("ktp", [128, 512], BF16, 2, ph)
            KKp = Pool("kkp", [128, 512], BF16, 2, ph)
            VVp = Pool("vvp", [128, 512], BF16, 2, ph)
            E1p = Pool("e1p", [128, 512], F32, 2, ph)
            E2p = Pool("e2p", [128, 512], F32, 2, ph)
            QSp = Pool("qsp", [128, 512], BF16, 2, ph)
            KSp = Pool("ksp", [128, 512], BF16, 2, ph)
            KHp = Pool("khp", [128, 512], BF16, 2, ph)
            KHCp = Pool("khcp", [128, 4, 512], BF16, 2, ph)
            QSCp = Pool("qscp", [128, 4, 128], BF16, 4, ph)
            ATp = Pool("atp", [128, 128], BF16, 3, ph)
            OOp = Pool("oop", [128, 512], F32, 2, ph)
            for d_ in range(2):
                Um_f = UF_f if d_ == 0 else UB_f
                Dm_f = DF_f if d_ == 0 else DB_f
                Um_b = UF_b if d_ == 0 else UB_b
                if d_ == 0:
                    order = list(range(NT))
                else:
                    order = list(range(NCT - 1, -1, -1)) + list(range(NT - 1, NCT - 1, -1))
                corder = [0, 1, 2, 3] if d_ == 0 else [3, 2, 1, 0]
                cur = []
                for h in range(4):
                    memset("pool", SF[h][0][:], 0.0, [SF[h][1]])
                    sb_, bsb_ = SBF[h].next()
                    memset("pool", sb_[:], 0.0, [bsb_])
                    cur.append((sb_, bsb_))
                for i in order:
                    t0 = 128 * i
                    g, bg = GP.next(); ld("sp", g[:], HG[t0:t0 + 128, 512 * d_:512 * d_ + 512], bg, [dB["HG"]])
                    qT, bqT = QTp.next(); ld("sp", qT[:].rearrange("p (h t) -> p h t", h=4), HQ[:, :, t0:t0 + 128].rearrange("h p t -> p h t"), bqT, [dB["HQ"]])
                    kT, bkT = KTp.next(); ld("sp", kT[:].rearrange("p (h t) -> p h t", h=4), HK[4 * d_:4 * d_ + 4, :, t0:t0 + 128].rearrange("h p t -> p h t"), bkT, [dB["HK"]])
                    kk, bkk = KKp.next(); ld("sp", kk[:], HKt[t0:t0 + 128, 512 * d_:512 * d_ + 512], bkk, [dB["HKt"]])
                    vv, bvv = VVp.next(); ld("sp", vv[:], HV[t0:t0 + 128, :], bvv, [dB["HV"]])
                    pB, bpB = PS[0]
                    for h in range(4):
                        mmp(pB[:, 128 * h:128 * h + 128], g[:, 128 * h:128 * h + 128], Um_f, True, True, [bg, bCF], bpB) if h else \
                            mm(pB[:, 0:128], g[:, 0:128], Um_f, True, True, [bg, bCF], bpB)
                    e1, be1 = E1p.next(); act(e1[:], pB[:], AF.Exp, [bpB], [be1])
                    e2, be2 = E2p.next(); act(e2[:], pB[:], AF.Exp, [bpB], [be2], scale=-1.0)
                    qs, bqs = QSp.next(); tt("dve", qs[:], qT[:], e1[:], ALU.mult, [bqT, be1], [bqs])
                    ks, bks = KSp.next(); tt("dve", ks[:], kT[:], e2[:], ALU.mult, [bkT, be2], [bks])
                    pD, bpD = PS[1]
                    mm(pD[:], Dm_f, g[:], True, True, [bCF, bg], bpD)
                    e3, be3 = E2p.next(); act(e3[:], pD[:], AF.Exp, [bpD], [be3])
                    kh, bkh = KHp.next(); tt("dve", kh[:], kk[:], e3[:], ALU.mult, [bkk, be3], [bkh])
                    khc, bkhc = KHCp.next()
                    for c in range(4):
                        ts("dve", khc[:, c, :], kh[:], RM[:, c:c + 1], None, ALU.mult, None, [bkh, bRM], (), pr=[bkhc])
                    pO, bpO = PS[2 + (i % 2)]
                    for h in range(4):
                        hs = slice(128 * h, 128 * h + 128)
                        qsc, bqsc = QSCp.next()
                        for c in range(4):
                            tt("pool", qsc[:, c, :], qs[:, hs], BDC[:, c, :], ALU.mult, [bqs, bCB], (), pr=[bqsc])
                        pA, bpA = PS[4 + (h % 2)]
                        mm(pA[:, 0:128], ks[:, hs], qs[:, hs], True, True, [bks, bqs], bpA)
                        at_, bat = ATp.next()
                        tt("dve", at_[:], pA[:, 0:128], Um_b, ALU.mult, [bpA, bCB], [bat])
                        if h == 0:
                            mm(pO[:, hs], at_[:], vv[:, hs], True, False, [bat, bvv], bpO)
                        else:
                            mmp(pO[:, hs], at_[:], vv[:, hs], True, False, [bat, bvv], bpO)
                        sf, bsf = SF[h]
                        for ci, c in enumerate(corder):
                            sb_, bsb_ = cur[h]
                            mmp(pO[:, hs], qsc[:, c, :], sb_[:], False, ci == 3, [bqsc, bsb_], bpO)
                            pU, bpU = PS[6 + (ci % 2)]
                            mm(pU[:, 0:128], khc[:, c, hs], vv[:, hs], True, True, [bkhc, bvv], bpU)
                            tl = 32 * c + 31 if d_ == 0 else 32 * c
                            stt(sf[:], sf[:], e1[:, 128 * h + tl:128 * h + tl + 1], pU[:, 0:128], ALU.mult, ALU.add, [bsf, be1, bpU], [bsf])
                            nb_, bnb_ = SBF[h].next()
                            cp("act", nb_[:], sf[:], [bsf], [bnb_])
                            cur[h] = (nb_, bnb_)
                    oo, boo = OOp.next()
                    if d_ == 0:
                        cp("act", oo[:], pO[:], [bpO], [boo])
                        stq("sp", OF[t0:t0 + 128, :], oo[:], boo, dB["OF"])
                    else:
                        of_, bof = GP.next()
                        ld("sp", of_[:], OF[t0:t0 + 128, :], bof, [dB["OF"]])
                        tt("dve", oo[:], pO[:], of_[:], ALU.add, [bpO, bof], [boo])
                        gt_, bgt = WKB.next()
                        ld("sp", gt_[:, 0:512], HGT[t0:t0 + 128, :], bgt, [dB["HGT"]])
                        ss, bss = SM.next()
                        junk, bj = WK4.next()
                        for h in range(4):
                            hs = slice(128 * h, 128 * h + 128)
                            S.op("act", lambda e, junk=junk, oo=oo, ss=ss, hs=hs, h=h: e.activation(out=junk[:, hs], in_=oo[:, hs], func=AF.Square, accum_out=ss[:, h:h + 1]),
                                 reads=[boo], partial=[bj, bss])
                        t, b = SM.next()
                        S.op("act", lambda e, t=t, ss=ss: e.activation(out=t[:, 0:4], in_=ss[:, 0:4], func=AF.Sqrt, bias=eps_t[:, 0:1], scale=1.0 / 128),
                             reads=[bss, beps], writes=[b])
                        S.op("dve", lambda e, t=t: e.reciprocal(out=t[:, 4:8], in_=t[:, 0:4]), reads=[b], writes=[b])
                        oc, boc = WKB.next()
                        for h in range(4):
                            hs = slice(128 * h, 128 * h + 128)
                            stt(junk[:, hs], oo[:, hs], t[:, 4 + h:5 + h], HNW[:], ALU.mult, ALU.mult, [boo, b, bHNW], (), pr=[bj])
                        tt("dve", oc[:, 0:512], junk[:, 0:512], gt_[:, 0:512], ALU.mult, [bj, bgt], [boc])
                        stq("sp", OC[t0:t0 + 128, :], oc[:, 0:512], boc, dB["OC"])

            ph = new_phase()
            WBR, bWBR = SB("WBR", [128, 12, D], BF16, ph)
            ld("pool", WBR[:], w_br[l].rearrange("i (k p) n -> p (i k) n", p=128), bWBR)
            WO, bWO = SB("WO", [128, 8, D], BF16, ph)
            ld("pool", WO[:], w_out[l].rearrange("(k p) n -> p k n", p=128), bWO)
            WR, bWR = SB("WR", [128, 8, NE], F32, ph)
            ld("sp", WR[:], w_router[l].rearrange("(k p) n -> p k n", p=128), bWR)
            BR, bBR = SB("BR", [128, NE], F32, ph)
            ld("sp", BR[:], b_router[l].partition_broadcast(128), bBR)
            memset("pool", RS_[:], 0.0, [bRS])
            OTp = Pool("otp", [128, 12, 128], BF16, 2, ph)
            GTp = Pool("gtp", [128, 3072], BF16, 2, ph)
            H2Tp = Pool("h2t", [128, 8, 128], F32, 2, ph)
            SMR = Pool("smr", [128, 4, NE], F32, 3, ph)
            zt, bzt = WKB.next()
            memset("pool", zt[:], 0.0, [bzt])
            for r0 in range(0, NSLOT, 128):
                stq("sp", XBUF[r0:r0 + 128, :], zt[:], bzt, dB["XBUF"])
            for i in range(NT):
                t0 = 128 * i
                ms, bms = (MODC, bMODC) if i < NCT else (MODL, bMODL)
                ob3, bob3 = WKB.next(), None
                oin, boin = ob3
                oin2, boin2 = WKB.next()
                ld("sp", oin[:, 0:512], OA[t0:t0 + 128, :], boin, [dB["OA"]])
                ld("sp", oin[:, 512:1024], OB[t0:t0 + 128, :], boin, [dB["OB"]])
                ld("sp", oin2[:, 0:512], OC[t0:t0 + 128, :], boin2, [dB["OC"]])
                gts, bgts = GTp.next()
                ld("sp", gts[:], GT[t0:t0 + 128, :], bgts, [dB["GT"]])
                pt, bpt = PS[7]
                ptb = pt[:].bitcast(BF16)
                oT, boT = OTp.next()
                for kc in range(8):
                    tr(ptb[:, kc * 128:(kc + 1) * 128], oin[:, kc * 128:(kc + 1) * 128], ID_b, [boin, bCB], bpt, kc == 0)
                cp("act", oT[:, 0:8, :], ptb.rearrange("p (k t) -> p k t", k=8), [bpt], [boT])
                for kc in range(4):
                    tr(ptb[:, kc * 128:(kc + 1) * 128], oin2[:, kc * 128:(kc + 1) * 128], ID_b, [boin2, bCB], bpt, kc == 0)
                cp("act", oT[:, 8:12, :], ptb[:, 0:512].rearrange("p (k t) -> p k t", k=4), [bpt], (), pr=[boT])
                y, by = WK4.next()
                tmpy, btmpy = WK4.next()
                for br_ in range(3):
                    for cb in range(2):
                        pp, bpp = PS[cb]
                        for kc in range(4):
                            mm(pp[:], oT[:, 4 * br_ + kc, :], WBR[:, 4 * br_ + kc, 512 * cb:512 * cb + 512], kc == 0, kc == 3, [boT, bWBR], bpp)
                        gsl = gts[:, D * br_ + 512 * cb:D * br_ + 512 * cb + 512]
                        if br_ == 0:
                            tt("dve", y[:, 512 * cb:512 * cb + 512], pp[:], gsl, ALU.mult, [bpp, bgts], (), pr=[by])
                        else:
                            tt("dve", tmpy[:, 512 * cb:512 * cb + 512], pp[:], gsl, ALU.mult, [bpp, bgts], (), pr=[btmpy])
                            tt("pool", y[:, 512 * cb:512 * cb + 512], y[:, 512 * cb:512 * cb + 512], tmpy[:, 512 * cb:512 * cb + 512], ALU.add, [btmpy, by], [by])
                yb, byb = WKB.next()
                cp("act", yb[:], y[:], [by], [byb])
                for kc in range(8):
                    tr(ptb[:, kc * 128:(kc + 1) * 128], yb[:, kc * 128:(kc + 1) * 128], ID_b, [byb, bCB], bpt, kc == 0)
                yT, byT = WKB.next()
                cp("act", yT[:], ptb, [bpt], [byT])
                ss, bss = SM.next()
                junk, bj = WK4.next()
                pps = []
                for cb in range(2):
                    pp, bpp = PS[2 + cb]
                    for kc in range(8):
                        mm(pp[:], yT[:, kc * 128:(kc + 1) * 128], WO[:, kc, 512 * cb:512 * cb + 512], kc == 0, kc == 7, [byT, bWO], bpp)
                    S.op("act", lambda e, junk=junk, pp=pp, ss=ss, cb=cb: e.activation(out=junk[:, 512 * cb:512 * cb + 512], in_=pp[:], func=AF.Square, accum_out=ss[:, cb:cb + 1]),
                         reads=[bpp], partial=[bj, bss])
                    pps.append((pp, bpp))
                tt("dve", ss[:, 2:3], ss[:, 0:1], ss[:, 1:2], ALU.add, [bss], [bss])
                rs, brs = rstd_from_ss(ss[:, 2:3], bss, D)
                xt, bx = WK4.next()
                ld("sp", xt[:], XR[t0:t0 + 128, :], bx, [dB["XR"]])
                for cb in range(2):
                    pp, bpp = pps[cb]
                    stt(junk[:, 512 * cb:512 * cb + 512], pp[:], rs, ms[:, 2, 512 * cb:512 * cb + 512], ALU.mult, ALU.mult, [bpp, brs, bms], (), pr=[bj])
                tt("dve", xt[:], xt[:], junk[:], ALU.add, [bx, bj], [bx])
                stq("sp", XR[t0:t0 + 128, :], xt[:], bx, dB["XR"])
                h2f, bh2f = WK4.next()
                hb2, bhb2 = norm_mod_T(xt, bx, ms, bms, 4, 3, t0, keep=(h2f, bh2f))
                h2T, bh2T = H2Tp.next()
                for half in range(2):
                    pr0, bpr0 = PS[4 + half]
                    for kc in range(4):
                        k2 = 4 * half + kc
                        tr(pr0[:, kc * 128:(kc + 1) * 128], h2f[:, k2 * 128:(k2 + 1) * 128], ID_f, [bh2f, bCF], bpr0, kc == 0)
                    cp("act", h2T[:, 4 * half:4 * half + 4, :], pr0[:].rearrange("p (k t) -> p k t", k=4), [bpr0], (), pr=[bh2T])
                pl, bpl = PS[6]
                for kc in range(8):
                    mm(pl[:, 0:NE], h2T[:, kc, :], WR[:, kc, :], kc == 0, kc == 7, [bh2T, bWR], bpl)
                sm, bsm = SMR.next()
                Lg = sm[:, 0, :]; Mk = sm[:, 1, :]; Pb = sm[:, 2, :]; Oh = sm[:, 3, :]
                tt("dve", Lg, pl[:, 0:NE], BR[:], ALU.add, [bpl, bBR], [bsm])
                t8, bt8 = SM.next()
                S.op("dve", lambda e, t8=t8, Lg=Lg: e.max(out=t8[:, 0:8], in_=Lg), reads=[bsm], writes=[bt8])
                ts("dve", t8[:, 8:9], t8[:, 0:1], -1.0, None, ALU.mult, None, [bt8], [bt8])
                S.op("act", lambda e, t8=t8: e.activation(out=t8[:, 9:13], in_=t8[:, 0:4], func=AF.Exp, bias=t8[:, 8:9], accum_out=t8[:, 13:14]),
                     reads=[bt8], writes=[bt8])
                S.op("dve", lambda e, t8=t8: e.reciprocal(out=t8[:, 14:15], in_=t8[:, 13:14]), reads=[bt8], writes=[bt8])
                ts("dve", G4[:, i, :], t8[:, 9:13], t8[:, 14:15], None, ALU.mult, None, [bt8], (), pr=[bG4])
                ts("dve", Mk, Lg, t8[:, 3:4], None, ALU.is_ge, None, [bsm, bt8], [bsm])
                pq, bpq = PS[6]
                mm(pq[:, 64:64 + NE], UTS_f, Mk, True, False, [bCF, bsm], bpq)
                mm(pq[:, 64:64 + NE], ONES_f, RS_[:], False, True, [bCF, bRS], bpq)
                tt("dve", Pb, pq[:, 64:64 + NE], EBASE[:, 0:NE], ALU.add, [bpq, bCF], [bsm])
                tt("pool", RS_[:], RS_[:], Mk, ALU.add, [bRS, bsm], [bRS])
                df, bdf = SM.next()
                for k in range(4):
                    ts("dve", Oh, Lg, t8[:, k:k + 1], None, ALU.is_equal, None, [bsm, bt8], [bsm])
                    tt("dve", Oh, Oh, Pb, ALU.mult, [bsm], [bsm])
                    S.op("dve", lambda e, df=df, Oh=Oh, k=k: e.reduce_sum(out=df[:, k:k + 1], in_=Oh, axis=AX.X), reads=[bsm], partial=[bdf])
                ts("dve", df[:, 0:4], df[:, 0:4], float(NSLOT - 1), None, ALU.min, None, [bdf], [bdf])
                cp("dve", DEST[:, i, :], df[:, 0:4], [bdf], (), pr=[bDEST])
                for k in range(4):
                    S.dma("pool", lambda e, i=i, k=k, hb2=hb2: e.indirect_dma_start(
                        out=XBUF, out_offset=bass.IndirectOffsetOnAxis(ap=DEST[:, i, k:k + 1], axis=0),
                        in_=hb2[:, :], in_offset=None),
                        bhb2.ds, reads=[bhb2, bDEST], writes=[dB["XBUF"]] if (i == 0 and k == 0) else (), partial=() if (i == 0 and k == 0) else [dB["XBUF"]])

            ph = new_phase()
            WGU = Pool("wgu", [128, 8, 2 * D], BF16, 1, ph)
            WDN = Pool("wdn", [128, 8, D], BF16, 1, ph)
            BGU = Pool("bgu", [128, 16], F32, 2, ph)
            BDN = Pool("bdn", [128, D], F32, 1, ph)
            XSp = Pool("xsp", [128, D], BF16, 3, ph)
            XTp = Pool("xtp", [128, 8, 512], BF16, 2, ph)
            ATT = Pool("att", [128, 8, 512], BF16, 1, ph)
            GCp = Pool("gcp", [128, 512], F32, 2, ph)
            SGp = Pool("sgp", [128, 512], F32, 2, ph)
            UCp = Pool("ucp", [128, 512], F32, 2, ph)
            YTp = Pool("ytp", [128, D], BF16, 2, ph)
            for e_ in range(NE):
                wg, bwg = WGU.next(); wd, bwd = WDN.next(); bg_, bbg = BGU.next(); bd_, bbd = BDN.next()
                for kc in range(8):
                    S.dma("pool", lambda e, wg=wg, kc=kc, e_=e_: e.dma_start(out=wg[:, kc, :], in_=w_gu[l, e_, kc * 128:(kc + 1) * 128, :]),
                          bwg.ds, writes=[bwg] if kc == 0 else (), partial=() if kc == 0 else [bwg])
                ld("pool", wd[:], w_dn[l, e_].rearrange("(k p) n -> p k n", p=128), bwd)
                ld("sp", bg_[:], b_gu_t[l, e_], bbg)
                ld("sp", bd_[:], b_dn[l, e_].partition_broadcast(128), bbd)
                for blk in range(CAP // 512):
                    r0 = e_ * CAP + 512 * blk
                    xT, bxT = XTp.next()
                    for s in range(4):
                        xs, bxs = XSp.next()
                        ld("sp", xs[:], XBUF[r0 + 128 * s:r0 + 128 * s + 128, :], bxs, [dB["XBUF"]])
                        pt, bpt = PS[6 + (s % 2)]
                        ptb = pt[:].bitcast(BF16)
                        for kc in range(8):
                            tr(ptb[:, kc * 128:(kc + 1) * 128], xs[:, kc * 128:(kc + 1) * 128], ID_b, [bxs, bCB], bpt, kc == 0)
                        cp("act", xT[:, :, 128 * s:128 * s + 128], ptb.rearrange("p (k t) -> p k t", k=8), [bpt], (), pr=[bxT])
                    aT, baT = ATT.next()
                    for f in range(8):
                        pg, bpg = PS[0 + (f % 2) * 2]
                        pu, bpu = PS[1 + (f % 2) * 2]
                        for kc in range(8):
                            mm(pg[:], wg[:, kc, 128 * f:128 * f + 128], xT[:, kc, :], kc == 0, kc == 7, [bwg, bxT], bpg)
                        for kc in range(8):
                            mm(pu[:], wg[:, kc, D + 128 * f:D + 128 * f + 128], xT[:, kc, :], kc == 0, kc == 7, [bwg, bxT], bpu)
                        gc, bgc = GCp.next(); sg, bsg = SGp.next(); uc, buc = UCp.next()
                        ts("dve", gc[:], pg[:], bg_[:, f:f + 1], 7.0, ALU.add, ALU.min, [bpg, bbg], [bgc])
                        act(sg[:], gc[:], AF.Silu, [bgc], [bsg], scale=1.702)
                        ts("dve", uc[:], pu[:], bg_[:, 8 + f:9 + f], 7.0, ALU.add, ALU.min, [bpu, bbg], [buc])
                        ts("dve", uc[:], uc[:], -7.0, 1.0, ALU.max, ALU.add, [buc], [buc])
                        stt(aT[:, f, :], uc[:], 1.0 / 1.702, sg[:], ALU.mult, ALU.mult, [buc, bsg], (), pr=[baT])
                    for s in range(4):
                        yt, byt = YTp.next()
                        for cb in range(2):
                            pp, bpp = PS[4 + cb]
                            for f in range(8):
                                mm(pp[:], aT[:, f, 128 * s:128 * s + 128], wd[:, f, 512 * cb:512 * cb + 512], f == 0, f == 7, [baT, bwd], bpp)
                            tt("dve", yt[:, 512 * cb:512 * cb + 512], pp[:], bd_[:, 512 * cb:512 * cb + 512], ALU.add, [bpp, bbd], (), pr=[byt])
                        stq("sp", YBUF[r0 + 128 * s:r0 + 128 * s + 128, :], yt[:], byt, dB["YBUF"])

            ph = new_phase()
            YG = Pool("yg", [128, 4, D], BF16, 2, ph)
            for i in range(NT):
                t0 = 128 * i
                ms, bms = (MODC, bMODC) if i < NCT else (MODL, bMODL)
                yg, byg = YG.next()
                for k in range(4):
                    S.dma("pool", lambda e, yg=yg, i=i, k=k: e.indirect_dma_start(
                        out=yg[:, k, :], out_offset=None, in_=YBUF,
                        in_offset=bass.IndirectOffsetOnAxis(ap=DEST[:, i, k:k + 1], axis=0)),
                        byg.ds, reads=[dB["YBUF"], bDEST], writes=[byg] if k == 0 else (), partial=() if k == 0 else [byg])
                f_, bf_ = WK4.next()
                ts("dve", f_[:], yg[:, 0, :], G4[:, i, 0:1], None, ALU.mult, None, [byg, bG4], [bf_])
                for k in range(1, 4):
                    stt(f_[:], yg[:, k, :], G4[:, i, k:k + 1], f_[:], ALU.mult, ALU.add, [byg, bG4, bf_], [bf_])
                junk, bj = WK4.next()
                ss, bss = SM.next()
                act(junk[:], f_[:], AF.Square, [bf_], [bj, bss], accum_out=ss[:, 0:1])
                rs, brs = rstd_from_ss(ss[:, 0:1], bss, D)
                xt, bx = WK4.next()
                ld("sp", xt[:], XR[t0:t0 + 128, :], bx, [dB["XR"]])
                stt(junk[:], f_[:], rs, ms[:, 5, :], ALU.mult, ALU.mult, [bf_, brs, bms], [bj])
                tt("dve", xt[:], xt[:], junk[:], ALU.add, [bx, bj], [bx])
                if l == nlayers - 1 and i >= NCT:
                    stq("sp", out[t0 - L_:t0 - L_ + 128, :], xt[:], bx, dB["out"])
                else:
                    stq("sp", XR[t0:t0 + 128, :], xt[:], bx, dB["XR"])

            S.barrier(list(DS.values())); S.emit(); phase[0].close(); phase[0] = None
            lay.close()
        S.wait_all("sp", list(dB.values()))
        S.emit()
        print("sems used:", S.nsem, "instr:", S.ninstr)
    return nc


def make_consts(S_, L_):
    T = S_ + L_
    GRID_W = 64
    rows = S_ // GRID_W
    row = np.repeat(np.arange(rows, dtype=np.float32), GRID_W)
    col = np.tile(np.arange(GRID_W, dtype=np.float32), rows)
    inv = (10000.0 ** (-np.arange(16, dtype=np.float32) / 16)).astype(np.float32)
    ang_r = row[:, None] * inv
    ang_c = col[:, None] * inv
    cos64 = np.ones((64, T), np.float32)
    sin64 = np.zeros((64, T), np.float32)
    for d in range(64):
        half, j = d // 32, d % 32
        n = j % 16
        ang = (ang_r if half == 0 else ang_c)[:, n]
        cos64[d, L_:] = np.cos(ang)
        sin64[d, L_:] = (-np.sin(ang)) if j < 16 else np.sin(ang)
    ropc = np.concatenate([cos64, cos64], 0)
    rops = np.concatenate([sin64, sin64], 0)
    s = np.arange(128)[:, None]
    t = np.arange(128)[None, :]
    same = (s // 32) == (t // 32)
    UF = (same & (s <= t)).astype(np.float32)
    UB = (same & (s >= t)).astype(np.float32)
    BD = same.astype(np.float32)
    cst_f = np.zeros((128, 8, 128), np.float32)
    cst_f[:, 0] = UF; cst_f[:, 1] = UB; cst_f[:, 2] = BD - UF; cst_f[:, 3] = BD - UB
    cst_f[:, 4] = (s < t).astype(np.float32)
    cst_f[:, 5] = 1.0
    cst_f[:, 6] = np.eye(128, dtype=np.float32)
    cst_f[:, 7, 0:NE] = (np.arange(NE) * CAP).astype(np.float32)[None, :]
    cst_b = np.zeros((128, 12, 128), np.float32)
    cst_b[:, 0] = np.eye(128)
    cst_b[:, 1] = (s >= t)
    cst_b[:, 2] = (s <= t)
    cst_b[:, 3] = UF; cst_b[:, 4] = UB
    for c in range(4):
        cst_b[:, 5 + c] = ((t // 32) == c).astype(np.float32) * np.ones((128, 1), np.float32)
    cst_b[0:64, 9, 0] = 1.0
    cst_b[64:128, 9, 1] = 1.0
    rowmask = np.zeros((128, 4), np.float32)
    for c in range(4):
        rowmask[32 * c:32 * c + 32, c] = 1.0
    return dict(ropc=ropc, rops=rops, cst_f=cst_f, cst_b=cst_b.astype(ml_dtypes.bfloat16), rowmask=rowmask)


def rope_perm_cols():
    cols = []
    for (c0, w) in ((C_AQ, 512), (C_AK, 128), (C_BQ, 512), (C_BK, 512)):
        for m in range(w):
            blk, j = m // 32, m % 32
            cols.append(c0 + blk * 32 + (j + 16) % 32)
    return np.array(cols, dtype=np.int64)


def make_in_maps(inputs, S_, L_, n_cores=8):
    f = lambda a: np.ascontiguousarray(np.asarray(a, dtype=np.float32))
    x = f(inputs["x"]); c = f(inputs["c"]); ctx = f(inputs["ctx"]); c_ctx = f(inputs["c_ctx"])
    w_in = f(inputs["w_in"])
    consts = make_consts(S_, L_)
    shared = dict(
        cc_col=np.ascontiguousarray(c_ctx.reshape(8, 128).T),
        w_mod=f(inputs["w_mod"]), b_mod=f(inputs["b_mod"]), norm_g=f(inputs["norm_g"]).reshape(2, 4 * D),
        w_in=w_in, w_inp=np.ascontiguousarray(w_in[:, :, rope_perm_cols()]),
        sink=f(inputs["attn_sink"]), dlam=f(inputs["diff_lambda"]).reshape(2, 256), dnw=f(inputs["diff_norm_w"]),
        lbl=f(inputs["hgrn_lb_logits"]).reshape(2, 1024),
        lbl_fm=np.ascontiguousarray(f(inputs["hgrn_lb_logits"]).reshape(2, 8, 128).transpose(0, 2, 1)),
        hnw=f(inputs["hgrn_norm_w"]), w_br=f(inputs["w_branch"]), w_out=f(inputs["w_out"]),
        w_router=f(inputs["w_router"]), b_router=f(inputs["b_router"]),
        w_gu=f(inputs["w_gate_up"]),
        b_gu_t=np.ascontiguousarray(f(inputs["b_gate_up"]).reshape(2, NE, 16, 128).transpose(0, 1, 3, 2)),
        w_dn=f(inputs["w_down"]), b_dn=f(inputs["b_down"]),
    )
    shared.update(consts)
    maps = []
    B = x.shape[0]
    for core in range(n_cores):
        b = core * B // n_cores
        m = dict(shared)
        m["xin"] = x[b]
        m["ctxin"] = ctx[b]
        m["c_col"] = np.ascontiguousarray(c[b].reshape(8, 128).T)
        maps.append(m)
    return maps


def kernel(**inputs):
    x = np.asarray(inputs["x"])
    B, S_, _ = x.shape
    L_ = np.asarray(inputs["ctx"]).shape[1]
    nc = bass.Bass("TRN2", target_bir_lowering=False)
    build(nc, S_, L_)
    maps = make_in_maps(inputs, S_, L_)
    res = run_bass_kernel_spmd(nc, maps, core_ids=list(range(8)))
    outs = [np.asarray(res.results[(b * 8) // B]["out"]) for b in range(B)]
    return np.stack(outs, 0).astype(np.float32)
```

```python
import contextlib
import numpy as np
import ml_dtypes
import concourse.bass as bass
import concourse.mybir as mybir
from concourse.bass_utils import run_bass_kernel_spmd

F32 = mybir.dt.float32
BF16 = mybir.dt.bfloat16
I32 = mybir.dt.int32
AF = mybir.ActivationFunctionType
ALU = mybir.AluOpType
AX = mybir.AxisListType

EPOCH = 30000
DEPOCH = 1800
COMPUTE = ("pe", "act", "dve", "pool")
ALLENG = ("pe", "act", "dve", "pool", "sp")

D = 1024
NE = 32
CAP = 3072
RMS_EPS = 1e-6


class DSem:
    __slots__ = ("name", "ep", "cnt")

    def __init__(self, name):
        self.name = name
        self.ep = 0
        self.cnt = 0


class Buf:
    __slots__ = ("name", "w", "r", "ds")

    def __init__(self, name, ds=None):
        self.name = name
        self.w = {}
        self.r = {}
        self.ds = ds


class Sched:
    def __init__(self, nc, stack):
        self.nc = nc
        self.stack = stack
        self.streams = {e: [] for e in ALLENG}
        self.cnt = {e: 0 for e in COMPUTE}
        self.waited = {e: {} for e in ALLENG}
        self.sems = {}
        self.nsem = 0

    def sem(self, key):
        s = self.sems.get(key)
        if s is None:
            s = self.stack.enter_context(self.nc.semaphore("s%d" % self.nsem))
            self.nsem += 1
            self.sems[key] = s
        return s

    def _wait(self, eng, key, val):
        if val <= 0:
            return
        if eng == "pe" and key[0] == "pe":
            return
        w = self.waited[eng]
        if w.get(key, 0) >= val:
            return
        w[key] = val
        self.streams[eng].append(("w", key, val))

    def _deps(self, eng, reads, writes, partial):
        for b in reads:
            for k, v in b.w.items():
                self._wait(eng, k, v)
        for b in writes:
            for k, v in b.w.items():
                self._wait(eng, k, v)
            for k, v in b.r.items():
                self._wait(eng, k, v)
        for b in partial:
            for k, v in b.r.items():
                self._wait(eng, k, v)

    def _record(self, tok, reads, writes, partial):
        k, v = tok
        for b in reads:
            if b.r.get(k, 0) < v:
                b.r[k] = v
        for b in writes:
            b.w = {k: v}
            b.r = {}
        for b in partial:
            if b.w.get(k, 0) < v:
                b.w[k] = v

    def op(self, eng, fn, reads=(), writes=(), partial=()):
        self._deps(eng, reads, writes, partial)
        n = self.cnt[eng]
        self.cnt[eng] = n + 1
        key = (eng, n // EPOCH)
        val = n % EPOCH + 1
        self.sem(key)
        self.streams[eng].append(("o", fn, key, 1))
        self._record((key, val), reads, writes, partial)

    def dma(self, eng, fn, ds, reads=(), writes=(), partial=()):
        self._deps(eng, reads, writes, partial)
        key = ("d", ds.name, ds.ep)
        self._wait(eng, key, ds.cnt)
        if ds.cnt >= 16 * DEPOCH:
            ds.ep += 1
            ds.cnt = 0
            key = ("d", ds.name, ds.ep)
        self.sem(key)
        ds.cnt += 16
        self.streams[eng].append(("o", fn, key, 16))
        self._record((key, ds.cnt), reads, writes, partial)

    def barrier(self, dsems):
        toks = []
        for eng in COMPUTE:
            c = self.cnt[eng]
            if c > 0:
                toks.append(((eng, (c - 1) // EPOCH), (c - 1) % EPOCH + 1))
        for ds in dsems:
            if ds.cnt > 0:
                toks.append((("d", ds.name, ds.ep), ds.cnt))
        for e in ALLENG:
            for k, v in toks:
                self._wait(e, k, v)

    def wait_all(self, eng, bufs):
        for b in bufs:
            for k, v in b.w.items():
                self._wait(eng, k, v)
            for k, v in b.r.items():
                self._wait(eng, k, v)

    def emit(self):
        nc = self.nc
        engmap = {"pe": "tensor", "act": "scalar", "dve": "vector", "pool": "gpsimd", "sp": "sync"}
        sems = self.sems
        streams = self.streams
        with nc.Block() as block:
            def mk(ename):
                def body(e):
                    for item in streams[ename]:
                        if item[0] == "w":
                            e.wait_ge(sems[item[1]], item[2])
                        else:
                            item[1](e).then_inc(sems[item[2]], item[3])
                return body
            for ename in ALLENG:
                if streams[ename]:
                    getattr(block, engmap[ename])(mk(ename))
        self.ninstr = getattr(self, "ninstr", 0) + sum(len(v) for v in streams.values())
        self.streams = {e: [] for e in ALLENG}


C_AQ, C_AK, C_AV, C_BQ, C_BK, C_BV, C_CQ, C_CI, C_FF, C_FB, C_CG, C_GT = (
    0, 512, 640, 768, 1280, 1792, 2304, 2816, 3328, 3840, 4352, 4864)
IN_W = 7936
P_AQ, P_AK, P_BQ, P_BK = 0, 512, 640, 1152
ROPE_W = 1664


def build(nc, S_, L_, dbg=(), nlayers=2):
    T = S_ + L_
    NT = T // 128
    NCT = L_ // 128
    NLT = S_ // 128
    tok_blocks = [(0, L_)] + [(L_ + 512 * i, 512) for i in range(S_ // 512)]
    NSLOT = NE * CAP

    def din(name, shape, dt=F32):
        return nc.dram_tensor(name, list(shape), dt, kind="ExternalInput").ap()

    def dscr(name, shape, dt):
        kind = "ExternalOutput" if name in dbg else "Internal"
        return nc.dram_tensor(name, list(shape), dt, kind=kind).ap()

    xin = din("xin", [S_, D]); ctxin = din("ctxin", [L_, D])
    c_col = din("c_col", [128, 8]); cc_col = din("cc_col", [128, 8])
    w_mod = din("w_mod", [2, D, 6 * D]); b_mod = din("b_mod", [2, 6 * D]); norm_g = din("norm_g", [2, 4 * D])
    w_in = din("w_in", [2, D, IN_W]); w_inp = din("w_inp", [2, D, ROPE_W])
    sink_in = din("sink", [2, 8]); dlam = din("dlam", [2, 256]); dnw = din("dnw", [2, 128])
    lbl = din("lbl", [2, 1024]); lbl_fm = din("lbl_fm", [2, 128, 8]); hnw = din("hnw", [2, 128])
    w_br = din("w_br", [2, 3, 512, D]); w_out = din("w_out", [2, D, D])
    w_router = din("w_router", [2, D, NE]); b_router = din("b_router", [2, NE])
    w_gu = din("w_gu", [2, NE, D, 2 * D]); b_gu_t = din("b_gu_t", [2, NE, 128, 16])
    w_dn = din("w_dn", [2, NE, D, D]); b_dn = din("b_dn", [2, NE, D])
    ropc = din("ropc", [128, T]); rops = din("rops", [128, T])
    cst_f = din("cst_f", [128, 8, 128])
    cst_b = din("cst_b", [128, 12, 128], BF16)
    rowmask_in = din("rowmask", [128, 4])
    out = nc.dram_tensor("out", [S_, D], F32, kind="ExternalOutput").ap()

    XR = dscr("XR", [T, D], F32)
    HT = dscr("HT", [8, 128, T], BF16)
    QA = dscr("QA", [8, 64, T], BF16); QAN = dscr("QAN", [8, T], F32)
    KA = dscr("KA", [2, 64, T], BF16); KAN = dscr("KAN", [2, T], F32)
    VA = dscr("VA", [T, 128], BF16)
    QB = dscr("QB", [8, 64, T], BF16); QBN = dscr("QBN", [8, T], F32)
    KB = dscr("KB", [8, 64, T], BF16); KBN = dscr("KBN", [8, T], F32)
    VB = dscr("VB", [T, 512], BF16)
    KMX = dscr("KMX", [10, 1], F32)
    HQ = dscr("HQ", [4, 128, T], BF16); HK = dscr("HK", [8, 128, T], BF16)
    HKt = dscr("HKt", [T, 1024], BF16); HG = dscr("HG", [T, 1024], F32)
    HV = dscr("HV", [T, 512], BF16); HGT = dscr("HGT", [T, 512], BF16)
    GT = dscr("GT", [T, 3072], BF16)
    OA = dscr("OA", [T, 512], BF16); OB = dscr("OB", [T, 512], BF16); OC = dscr("OC", [T, 512], BF16)
    OF = dscr("OF", [T, 512], F32)
    XBUF = dscr("XBUF", [NSLOT, D], BF16); YBUF = dscr("YBUF", [NSLOT, D], BF16)

    with contextlib.ExitStack() as st:
        S = Sched(nc, st)
        DS = {}
        uniq = [0]

        def SB(name, shape, dt, stack=None):
            uniq[0] += 1
            t = (stack or st).enter_context(nc.sbuf_tensor("%s_%d" % (name, uniq[0]), list(shape), dt))
            ds = DS.get(name)
            if ds is None:
                ds = DS[name] = DSem(name)
            return t, Buf(name, ds)

        class Pool:
            def __init__(self, name, shape, dt, n, stack=None):
                self.items = [SB("%s%d" % (name, i), shape, dt, stack) for i in range(n)]
                self.i = 0

            def next(self):
                it = self.items[self.i % len(self.items)]
                self.i += 1
                return it

        dB = {n: Buf(n) for n in ["XR", "HT", "QA", "QAN", "KA", "KAN", "VA", "QB", "QBN", "KB", "KBN", "VB", "KMX",
                                  "HQ", "HK", "HKt", "HG", "HV", "HGT", "GT", "OA", "OB", "OC", "OF", "XBUF", "YBUF", "out"]}

        def ld(eng, dst, src, dbuf, rd=()):
            S.dma(eng, lambda e: e.dma_start(out=dst, in_=src), dbuf.ds, reads=rd, writes=[dbuf])

        def stq(eng, dst, src, sbuf, dram):
            S.dma("act", lambda e: e.dma_start(out=dst, in_=src), sbuf.ds, reads=[sbuf], partial=[dram])

        def mm(o, lhsT, rhs, first, last, rd, pb):
            S.op("pe", lambda e: e.matmul(o, lhsT=lhsT, rhs=rhs, start=first, stop=last), reads=rd,
                 writes=[pb] if first else (), partial=() if first else [pb])

        def mmp(o, lhsT, rhs, first, last, rd, pb):
            S.op("pe", lambda e: e.matmul(o, lhsT=lhsT, rhs=rhs, start=first, stop=last), reads=rd, partial=[pb])

        def tr(o, i_, idt, rd, pb, first):
            S.op("pe", lambda e: e.transpose(out=o, in_=i_, identity=idt), reads=rd,
                 writes=[pb] if first else (), partial=() if first else [pb])

        def act(o, i_, func, rd, wr, eng="act", **kw):
            S.op(eng, lambda e: e.activation(out=o, in_=i_, func=func, **kw), reads=rd, writes=wr)

        def tt(eng, o, a, b, op, rd, wr, pr=()):
            S.op(eng, lambda e: e.tensor_tensor(out=o, in0=a, in1=b, op=op), reads=rd, writes=wr, partial=pr)

        def ts(eng, o, a, s1, s2, op0, op1, rd, wr, pr=(), **kw):
            if s2 is None:
                S.op(eng, lambda e: e.tensor_scalar(out=o, in0=a, scalar1=s1, scalar2=None, op0=op0, **kw), reads=rd, writes=wr, partial=pr)
            else:
                S.op(eng, lambda e: e.tensor_scalar(out=o, in0=a, scalar1=s1, scalar2=s2, op0=op0, op1=op1, **kw), reads=rd, writes=wr, partial=pr)

        def stt(o, a, s, b, op0, op1, rd, wr, pr=()):
            S.op("dve", lambda e: e.scalar_tensor_tensor(out=o, in0=a, scalar=s, in1=b, op0=op0, op1=op1), reads=rd, writes=wr, partial=pr)

        def cp(eng, o, i_, rd, wr, pr=()):
            if eng == "act":
                S.op("act", lambda e: e.copy(out=o, in_=i_), reads=rd, writes=wr, partial=pr)
            else:
                S.op(eng, lambda e: e.tensor_copy(out=o, in_=i_), reads=rd, writes=wr, partial=pr)

        def memset(eng, o, v, wr):
            S.op(eng, lambda e: e.memset(o, v), writes=wr)

        CF, bCF = SB("CF", [128, 8, 128], F32)
        CB, bCB = SB("CB", [128, 12, 128], BF16)
        RM, bRM = SB("RM", [128, 4], F32)
        ld("sp", CF[:], cst_f, bCF); ld("sp", CB[:], cst_b, bCB); ld("sp", RM[:], rowmask_in, bRM)
        UF_f, UB_f, DF_f, DB_f, UTS_f, ONES_f, ID_f, EBASE = [CF[:, i, :] for i in range(8)]
        ID_b, MPREV, MNEXT, UF_b, UB_b = [CB[:, i, :] for i in range(5)]
        BDC = CB[:, 5:9, :]
        BONES = CB[:, 9, 0:2]

        PS = [(st.enter_context(nc.psum_tensor("ps%d" % i, [128, 512], F32)), Buf("ps%d" % i)) for i in range(8)]

        MODL, bMODL = SB("MODL", [128, 6, D], F32)
        MODC, bMODC = SB("MODC", [128, 6, D], F32)
        eps_t, beps = SB("eps_t", [128, 1], F32)
        memset("pool", eps_t[:], RMS_EPS, [beps])

        WK4 = Pool("wk4", [128, D], F32, 4)
        WKB = Pool("wkb", [128, D], BF16, 4)
        SM = Pool("sm", [128, 16], F32, 8)
        phase = [None]

        def new_phase():
            if phase[0] is not None:
                S.barrier(list(DS.values()))
                S.emit()
                phase[0].close()
            phase[0] = contextlib.ExitStack()
            return phase[0]

        def rstd_from_ss(ss_ap, bss, n):
            t, b = SM.next()
            S.op("act", lambda e: e.activation(out=t[:, 0:1], in_=ss_ap, func=AF.Sqrt, bias=eps_t[:, 0:1], scale=1.0 / n),
                 reads=[bss, beps], writes=[b])
            t2, b2 = SM.next()
            S.op("dve", lambda e: e.reciprocal(out=t2[:, 0:1], in_=t[:, 0:1]), reads=[b], writes=[b2])
            return t2[:, 0:1], b2

        def norm_mod_T(xt, bx, ms, bms, gs, shs, t0, keep=None):
            junk, bj = WK4.next()
            ss, bss = SM.next()
            act(junk[:], xt[:], AF.Square, [bx], [bj, bss], accum_out=ss[:, 0:1])
            rs, brs = rstd_from_ss(ss[:, 0:1], bss, D)
            tmp, btmp = WK4.next()
            stt(tmp[:], xt[:], rs, ms[:, gs, :], ALU.mult, ALU.mult, [bx, brs, bms], [btmp])
            hb, bhb = WKB.next()
            tt("pool", hb[:], tmp[:], ms[:, shs, :], ALU.add, [btmp, bms], [bhb])
            if keep is not None:
                tt("dve", keep[0][:], tmp[:], ms[:, shs, :], ALU.add, [btmp, bms], [keep[1]])
            pt, bpt = PS[7]
            ptb = pt[:].bitcast(BF16)
            for kc in range(8):
                tr(ptb[:, kc * 128:(kc + 1) * 128], hb[:, kc * 128:(kc + 1) * 128], ID_b, [bhb, bCB], bpt, kc == 0)
            hT, bhT = WKB.next()
            cp("act", hT[:], ptb, [bpt], [bhT])
            stq("sp", HT[:, :, t0:t0 + 128].rearrange("k p t -> p k t"), hT[:].rearrange("p (k t) -> p k t", k=8), bhT, dB["HT"])
            return hb, bhb

        for l in range(nlayers):
            lam_init = 0.8 - 0.6 * float(np.exp(-0.3 * l))
            if phase[0] is not None:
                S.barrier(list(DS.values())); S.emit(); phase[0].close(); phase[0] = None
            lay = contextlib.ExitStack()
            LBT, bLBT = SB("LBT", [128, 1024], F32, lay)
            OMT, bOMT = SB("OMT", [128, 1024], F32, lay)
            LBF, bLBF = SB("LBF", [128, 8], F32, lay)
            OMF, bOMF = SB("OMF", [128, 8], F32, lay)
            DL, bDL = SB("DL", [128, 256], F32, lay)
            LV, bLV = SB("LV", [128, 8], F32, lay)
            DNW, bDNW = SB("DNW", [128, 128], F32, lay)
            HNW, bHNW = SB("HNW", [128, 128], F32, lay)
            SK, bSK = SB("SK", [128, 8], F32, lay)
            KMB, bKMB = SB("KMB", [128, 10], F32, lay)
            NKM, bNKM = SB("NKM", [128, 10], F32, lay)
            NKM8, bNKM8 = SB("NKM8", [128, 10], F32, lay)
            RS_, bRS = SB("RS", [128, NE], F32, lay)
            DEST, bDEST = SB("DEST", [128, NT + 1, 4], I32, lay)
            G4, bG4 = SB("G4", [128, NT, 4], F32, lay)
            ph = new_phase()
            W16a, bW16a = SB("W16a", [128, 8, 512], F32, ph)
            W16b, bW16b = SB("W16b", [128, 8, 512], F32, ph)
            cs, bcs = SB("cs", [128, 16], F32, ph)
            ld("sp", cs[:, 0:8], c_col, bcs); ld("sp", cs[:, 8:16], cc_col, bcs)
            act(cs[:], cs[:], AF.Silu, [bcs], [bcs])
            LH, bLH = SB("LH", [128, 16, 128], F32, ph)
            for j in range(16):
                cp("pool", LH[:, j, :], cs[:, j:j + 1].to_broadcast([128, 128]), [bcs], (), pr=[bLH])
            NG, bNG = W16b, bW16b
            for cb in range(12):
                ld("sp", W16a[:], w_mod[l, :, cb * 512:(cb + 1) * 512].rearrange("(k p) n -> p k n", p=128), bW16a)
                bm, bbm = WK4.next()
                ld("sp", bm[:, 0:512], b_mod[l, cb * 512:(cb + 1) * 512].partition_broadcast(128), bbm)
                for si, (ms, bms) in enumerate(((MODL, bMODL), (MODC, bMODC))):
                    pp, bpp = PS[si]
                    for kc in range(8):
                        mm(pp[:], LH[:, si * 8 + kc, :], W16a[:, kc, :], kc == 0, kc == 7, [bLH, bW16a], bpp)
                    tt("dve", ms[:, cb // 2, (cb % 2) * 512:(cb % 2) * 512 + 512], pp[:], bm[:, 0:512], ALU.add, [bpp, bbm], (), pr=[bms])
            ld("sp", NG[:].rearrange("p k n -> p (k n)"), norm_g[l].partition_broadcast(128), bNG)
            NGv = NG[:].rearrange("p k n -> p (k n)")
            for ms, bms in ((MODL, bMODL), (MODC, bMODC)):
                stt(ms[:, 1, :], ms[:, 1, :], 1.0, NGv[:, 0:D], ALU.add, ALU.mult, [bms, bNG], [bms])
                tt("dve", ms[:, 2, :], ms[:, 2, :], NGv[:, D:2 * D], ALU.mult, [bms, bNG], [bms])
                stt(ms[:, 4, :], ms[:, 4, :], 1.0, NGv[:, 2 * D:3 * D], ALU.add, ALU.mult, [bms, bNG], [bms])
                tt("dve", ms[:, 5, :], ms[:, 5, :], NGv[:, 3 * D:4 * D], ALU.mult, [bms, bNG], [bms])

            if l == 0:
                memset("pool", LBT[:], 0.0, [bLBT]); memset("pool", OMT[:], 1.0, [bOMT])
                memset("pool", LBF[:], 0.0, [bLBF]); memset("pool", OMF[:], 1.0, [bOMF])
            else:
                t1, b1 = WK4.next()
                ld("sp", LBT[:], lbl[1].partition_broadcast(128), bLBT)
                ld("sp", t1[:], lbl[0].partition_broadcast(128), b1)
                tt("dve", LBT[:], LBT[:], t1[:], ALU.subtract, [bLBT, b1], [bLBT])
                act(LBT[:], LBT[:], AF.Sigmoid, [bLBT], [bLBT])
                ts("dve", OMT[:], LBT[:], -1.0, 1.0, ALU.mult, ALU.add, [bLBT], [bOMT])
                t2, b2 = SM.next()
                ld("sp", LBF[:], lbl_fm[1], bLBF); ld("sp", t2[:, 0:8], lbl_fm[0], b2)
                tt("dve", LBF[:], LBF[:], t2[:, 0:8], ALU.subtract, [bLBF, b2], [bLBF])
                act(LBF[:], LBF[:], AF.Sigmoid, [bLBF], [bLBF])
                ts("dve", OMF[:], LBF[:], -1.0, 1.0, ALU.mult, ALU.add, [bLBF], [bOMF])
            ld("sp", DL[:], dlam[l].partition_broadcast(128), bDL)
            tt("dve", DL[:, 0:64], DL[:, 0:64], DL[:, 64:128], ALU.mult, [bDL], [bDL])
            tt("dve", DL[:, 128:192], DL[:, 128:192], DL[:, 192:256], ALU.mult, [bDL], [bDL])
            S.op("dve", lambda e: e.reduce_sum(out=LV[:, 0:1], in_=DL[:, 0:64], axis=AX.X), reads=[bDL], writes=[bLV])
            S.op("dve", lambda e: e.reduce_sum(out=LV[:, 1:2], in_=DL[:, 128:192], axis=AX.X), reads=[bDL], partial=[bLV])
            act(LV[:, 0:2], LV[:, 0:2], AF.Exp, [bLV], [bLV])
            tt("dve", LV[:, 2:3], LV[:, 1:2], LV[:, 0:1], ALU.subtract, [bLV], [bLV])
            ts("dve", LV[:, 3:4], LV[:, 2:3], -lam_init, None, ALU.add, None, [bLV], [bLV])
            NEGLAM = LV[:, 3:4]
            ld("sp", DNW[:], dnw[l].partition_broadcast(128), bDNW)
            ts("dve", DNW[:], DNW[:], 1.0 - lam_init, None, ALU.mult, None, [bDNW], [bDNW])
            ld("sp", HNW[:], hnw[l].partition_broadcast(128), bHNW)
            ld("sp", SK[:], sink_in[l].partition_broadcast(128), bSK)

            ph = new_phase()
            if l == 0:
                S.dma("sp", lambda e: e.dma_start(out=XR[0:L_, :], in_=ctxin), bCF.ds, partial=[dB["XR"]])
                for r0 in range(0, S_, 512):
                    S.dma("sp", lambda e, r0=r0: e.dma_start(out=XR[L_ + r0:L_ + r0 + 512, :], in_=xin[r0:r0 + 512, :]), bCB.ds, partial=[dB["XR"]])
            for i in range(NT):
                t0 = 128 * i
                ms, bms = (MODC, bMODC) if i < NCT else (MODL, bMODL)
                xt, bx = WK4.next()
                ld("sp", xt[:], XR[t0:t0 + 128, :], bx, [dB["XR"]])
                norm_mod_T(xt, bx, ms, bms, 1, 0, t0)

            ph = new_phase()
            WB8 = Pool("wb8", [128, 8, 512], BF16, 2, ph)
            HB = Pool("hb", [128, 8, 512], BF16, 3, ph)
            fm_jobs = []
            for i in range(4):
                fm_jobs.append((C_AQ + 128 * i, P_AQ + 128 * i, "rope", (QA, QAN, "QA", "QAN", 2 * i)))
            fm_jobs.append((C_AK, P_AK, "rope", (KA, KAN, "KA", "KAN", 0)))
            for i in range(4):
                fm_jobs.append((C_BQ + 128 * i, P_BQ + 128 * i, "rope", (QB, QBN, "QB", "QBN", 2 * i)))
            for i in range(4):
                fm_jobs.append((C_BK + 128 * i, P_BK + 128 * i, "rope", (KB, KBN, "KB", "KBN", 2 * i)))
            for i in range(4):
                fm_jobs.append((C_CQ + 128 * i, None, "silu", i))
            for i in range(4):
                fm_jobs.append((C_FF + 128 * i, None, "kfm", i))
            for i in range(4):
                fm_jobs.append((C_FB + 128 * i, None, "kfm", 4 + i))
            for (c0, p0, kind, info) in fm_jobs:
                wt, bwt = WB8.next()
                ld("pool", wt[:, :, 0:128], w_in[l, :, c0:c0 + 128].rearrange("(k p) n -> p k n", p=128), bwt)
                if kind == "rope":
                    ld("pool", wt[:, :, 128:256], w_inp[l, :, p0:p0 + 128].rearrange("(k p) n -> p k n", p=128), bwt)
                for (t0, n) in tok_blocks:
                    hb, bhb = HB.next()
                    ld("sp", hb[:, :, 0:n], HT[:, :, t0:t0 + n].rearrange("k p t -> p k t"), bhb, [dB["HT"]])
                    pa, bpa = PS[0]
                    for kc in range(8):
                        mm(pa[:, 0:n], wt[:, kc, 0:128], hb[:, kc, 0:n], kc == 0, kc == 7, [bwt, bhb], bpa)
                    if kind == "rope":
                        Qd, Nd, qn, nn, h0 = info
                        pb_, bpb = PS[1]
                        for kc in range(8):
                            mm(pb_[:, 0:n], wt[:, kc, 128:256], hb[:, kc, 0:n], kc == 0, kc == 7, [bwt, bhb], bpb)
                        rc, brc = WK4.next()
                        ld("sp", rc[:, 0:n], ropc[:, t0:t0 + n], brc); ld("sp", rc[:, 512:512 + n], rops[:, t0:t0 + n], brc)
                        t1, b1 = WK4.next()
                        tt("dve", t1[:, 0:n], pa[:, 0:n], rc[:, 0:n], ALU.mult, [bpa, brc], [b1])
                        tt("dve", t1[:, 512:512 + n], pb_[:, 0:n], rc[:, 512:512 + n], ALU.mult, [bpb, brc], (), pr=[b1])
                        qt, bqt = WKB.next()
                        tt("pool", qt[:, 0:n], t1[:, 0:n], t1[:, 512:512 + n], ALU.add, [b1], [bqt])
                        sq, bsq = WKB.next()
                        act(sq[:, 0:n], qt[:, 0:n], AF.Square, [bqt], [bsq])
                        pn, bpn = PS[2]
                        mm(pn[0:2, 0:n], BONES, sq[:, 0:n], True, True, [bCB, bsq], bpn)
                        nr, bnr = WK4.next()
                        act(nr[0:2, 0:n], pn[0:2, 0:n], AF.Sqrt, [bpn], [bnr])
                        stq("sp", Nd[h0:h0 + 2, t0:t0 + n], nr[0:2, 0:n], bnr, dB[nn])
                        stq("sp", Qd[h0, :, t0:t0 + n], qt[0:64, 0:n], bqt, dB[qn])
                        stq("sp", Qd[h0 + 1, :, t0:t0 + n], qt[64:128, 0:n], bqt, dB[qn])
                    elif kind == "silu":
                        qt, bqt = WKB.next()
                        act(qt[:, 0:n], pa[:, 0:n], AF.Silu, [bpa], [bqt])
                        stq("sp", HQ[info, :, t0:t0 + n], qt[:, 0:n], bqt, dB["HQ"])
                    else:
                        t1, b1 = WK4.next()
                        act(t1[:, 0:n], pa[:, 0:n], AF.Sigmoid, [bpa], [b1], scale=-1.0)
                        qt, bqt = WKB.next()
                        ts("dve", qt[:, 0:n], t1[:, 0:n], OMF[:, info:info + 1], None, ALU.mult, None, [b1, bOMF], [bqt])
                        stq("sp", HK[info, :, t0:t0 + n], qt[:, 0:n], bqt, dB["HK"])

            tm_jobs = [(C_AV, 128, "copy", (VA, "VA", 0)), (C_BV, 512, "copy", (VB, "VB", 0)), (C_CI, 512, "copy", (HV, "HV", 0)),
                       (C_CG, 512, "silu", (HGT, "HGT", 0)), (C_FF, 512, "logf", 0), (C_FB, 512, "logf", 1)]
            for j in range(6):
                tm_jobs.append((C_GT + 512 * j, 512, "sigm", (GT, "GT", 512 * j)))
            for (c0, w, kind, info) in tm_jobs:
                wt, bwt = WB8.next()
                ld("pool", wt[:, :, 0:w], w_in[l, :, c0:c0 + w].rearrange("(k p) n -> p k n", p=128), bwt)
                for (t0, n) in tok_blocks:
                    hb, bhb = HB.next()
                    ld("sp", hb[:, :, 0:n], HT[:, :, t0:t0 + n].rearrange("k p t -> p k t"), bhb, [dB["HT"]])
                    for s in range(n // 128):
                        tt0 = t0 + 128 * s
                        pa, bpa = PS[s % 2]
                        for kc in range(8):
                            mm(pa[:, 0:w], hb[:, kc, 128 * s:128 * s + 128], wt[:, kc, 0:w], kc == 0, kc == 7, [bwt, bhb], bpa)
                        if kind in ("copy", "silu", "sigm"):
                            dst, dn, co = info
                            ot, bot = WKB.next()
                            fn = {"copy": AF.Copy, "silu": AF.Silu, "sigm": AF.Sigmoid}[kind]
                            act(ot[:, 0:w], pa[:, 0:w], fn, [bpa], [bot])
                            stq("sp", dst[tt0:tt0 + 128, co:co + w], ot[:, 0:w], bot, dB[dn])
                        else:
                            d_ = info
                            t1, b1 = WK4.next()
                            act(t1[:, 0:512], pa[:, 0:512], AF.Sigmoid, [bpa], [b1])
                            tt("dve", t1[:, 0:512], t1[:, 0:512], OMT[:, d_ * 512:(d_ + 1) * 512], ALU.mult, [b1, bOMT], [b1])
                            tt("dve", t1[:, 0:512], t1[:, 0:512], LBT[:, d_ * 512:(d_ + 1) * 512], ALU.add, [b1, bLBT], [b1])
                            kt, bkt = WKB.next()
                            ts("dve", kt[:, 0:512], t1[:, 0:512], -1.0, 1.0, ALU.mult, ALU.add, [b1], [bkt])
                            stq("sp", HKt[tt0:tt0 + 128, d_ * 512:(d_ + 1) * 512], kt[:, 0:512], bkt, dB["HKt"])
                            act(t1[:, 512:1024], t1[:, 0:512], AF.Ln, [b1], [b1])
                            stq("sp", HG[tt0:tt0 + 128, d_ * 512:(d_ + 1) * 512], t1[:, 512:1024], b1, dB["HG"])

            ph = new_phase()
            kmrow, bkmrow = SB("kmrow", [1, T], F32, ph)
            kmv, bkmv = SB("kmv", [1, 16], F32, ph)
            for ku in range(10):
                src = KAN[ku:ku + 1, :] if ku < 2 else KBN[ku - 2:ku - 1, :]
                ld("sp", kmrow[:], src, bkmrow, [dB["KAN"], dB["KBN"]])
                S.op("dve", lambda e, ku=ku: e.reduce_max(out=kmv[:, ku:ku + 1], in_=kmrow[:], axis=AX.X), reads=[bkmrow], partial=[bkmv])
            stq("sp", KMX.rearrange("a b -> b a"), kmv[:, 0:10], bkmv, dB["KMX"])
            ld("sp", KMB[:], KMX.rearrange("a b -> (a b)").partition_broadcast(128), bKMB, [dB["KMX"]])
            ts("dve", NKM[:], KMB[:], -1.0, None, ALU.mult, None, [bKMB], [bNKM])
            ts("dve", NKM8[:], KMB[:], -0.125, None, ALU.mult, None, [bKMB], [bNKM8])

            ph = new_phase()
            KAUG = [SB("kaug%d" % i, [65, T], BF16, ph) for i in range(2)]
            VS, bVS = SB("vs", [128, NT, 132], BF16, ph)
            memset("pool", VS[:], 1.0, [bVS])
            QAUG = Pool("qaug", [65, 512], BF16, 2, ph)
            QNR = Pool("qnr", [65, 512], F32, 2, ph)
            PT = Pool("pt", [128, 512], BF16, 4, ph)
            OT = Pool("ot", [128, 512], F32, 2, ph)
            OT2 = Pool("ot2", [128, 512], F32, 2, ph)
            for i in range(2):
                memset("pool", KAUG[i][0][64:65, :], 1.0, [KAUG[i][1]])

            def load_k(slot, Kd, kname, ku):
                kt_, bk_ = KAUG[slot]
                S.dma("sp", lambda e: e.dma_start(out=kt_[0:64, :], in_=Kd[ku]), bk_.ds, reads=[dB[kname]], writes=[bk_])

            def load_v(Vd, vname, v0, dv):
                for j0 in range(0, NT, 11):
                    j1 = min(NT, j0 + 11)
                    S.dma("sp", lambda e, j0=j0, j1=j1: e.dma_start(out=VS[:, j0:j1, 0:dv], in_=Vd[128 * j0:128 * j1, v0:v0 + dv].rearrange("(j p) d -> p j d", p=128)),
                          bVS.ds, reads=[dB[vname]], writes=[bVS] if j0 == 0 else (), partial=() if j0 == 0 else [bVS])
                memset("pool", VS[:, :, dv:dv + 1], 1.0, [bVS])

            def attn_unit(Qd, Nd, qname, nname, u, kslot, kmcol, t0, n, keys, dv, accs):
                kt_, bk_ = KAUG[kslot]
                qa, bqa = QAUG.next()
                ld("sp", qa[0:64, 0:n], Qd[u, :, t0:t0 + n], bqa, [dB[qname]])
                qn, bqn = QNR.next()
                ld("sp", qn[64:65, 0:n], Nd[u:u + 1, t0:t0 + n], bqn, [dB[nname]])
                S.op("act", lambda e: e.activation(out=qa[64:65, 0:n], in_=qn[64:65, 0:n], func=AF.Copy, scale=NKM[64:65, kmcol:kmcol + 1]),
                     reads=[bqn, bNKM], partial=[bqa])
                nk = len(keys)

                def issue_st(ki):
                    pS, bpS = PS[ki % 2]
                    j = keys[ki][0]
                    mm(pS[:, 0:n], kt_[:, 128 * j:128 * j + 128], qa[:, 0:n], True, True, [bk_, bqa], bpS)
                    return pS, bpS

                cur_st = issue_st(0)
                for ki, (j, mask) in enumerate(keys):
                    pS, bpS = cur_st
                    if ki + 1 < nk:
                        cur_st = issue_st(ki + 1)
                    pt_, bpt_ = PT.next()
                    act(pt_[:, 0:n], pS[:, 0:n], AF.Exp, [bpS], [bpt_], scale=0.125)
                    if mask is not None:
                        tt("pool", pt_[:, 0:n], pt_[:, 0:n], mask, ALU.mult, [bpt_, bCB], [bpt_])
                    for s in range(n // 128):
                        ao, ab = accs[s]
                        mmp(ao, pt_[:, 128 * s:128 * s + 128], VS[:, j, 0:dv + 1], ki == 0, ki == nk - 1, [bpt_, bVS], ab)

            def acc_views(dv, banks):
                res = []
                for s in range(4):
                    p, b = PS[banks[s // 2]]
                    res.append((p[:, (s % 2) * 256:(s % 2) * 256 + dv + 1], b))
                return res

            for ku in range(2):
                load_k(0, KA, "KA", ku)
                load_v(VA, "VA", 64 * ku, 64)
                for g in range(4):
                    hq = 4 * ku + g
                    for i in range(NT):
                        t0 = 128 * i
                        if i < NCT:
                            keys = [(j, None) for j in range(NCT)]
                        else:
                            keys = [(j, None) for j in range(NCT)]
                            if i > NCT:
                                keys.append((i - 1, MPREV))
                            keys.append((i, None))
                            if i < NT - 1:
                                keys.append((i + 1, MNEXT))
                        accs = acc_views(64, (2 + (i % 2) * 2, 3 + (i % 2) * 2))
                        pass
                        attn_unit(QA, QAN, "QA", "QAN", hq, 0, ku, t0, 128, keys, 64, accs)
                        qn, bqn = SM.next()
                        S.dma("sp", lambda e, qn=qn, t0=t0, hq=hq: e.dma_start(out=qn[:, 0:1], in_=QAN[hq, t0:t0 + 128].rearrange("(p o) -> p o", o=1)),
                              bqn.ds, reads=[dB["QAN"]], writes=[bqn])
                        es, bes = SM.next()
                        S.op("act", lambda e, es=es, qn=qn, ku=ku, hq=hq: e.activation(out=es[:, 0:1], in_=qn[:, 0:1], func=AF.Exp,
                                                                                        scale=NKM8[:, ku:ku + 1], bias=SK[:, hq:hq + 1]),
                             reads=[bqn, bNKM8, bSK], writes=[bes])
                        ao, ab = accs[0]
                        tt("dve", es[:, 1:2], es[:, 0:1], ao[:, 64:65], ALU.add, [bes, ab], [bes])
                        S.op("dve", lambda e, es=es: e.reciprocal(out=es[:, 2:3], in_=es[:, 1:2]), reads=[bes], writes=[bes])
                        o_, bo_ = WKB.next()
                        ts("dve", o_[:, 0:64], ao[:, 0:64], es[:, 2:3], None, ALU.mult, None, [ab, bes], [bo_])
                        stq("sp", OA[t0:t0 + 128, 64 * hq:64 * hq + 64], o_[:, 0:64], bo_, dB["OA"])

            for h in range(4):
                load_k(0, KB, "KB", 2 * h); load_k(1, KB, "KB", 2 * h + 1)
                load_v(VB, "VB", 128 * h, 128)
                for bi, (t0, n) in enumerate(tok_blocks):
                    keys = [(j, None) for j in range(NCT)] if t0 < L_ else [(j, None) for j in range(NT)]
                    O0, bO0 = OT.next()
                    for c in range(2):
                        accs = [(PS[2 + s_][0][:, 0:129], PS[2 + s_][1]) for s_ in range(4)]
                        attn_unit(QB, QBN, "QB", "QBN", 2 * h + c, c, 2 + 2 * h + c, t0, n, keys, 128, accs)
                        for s in range(n // 128):
                            a_, b_ = accs[s]
                            r, br = SM.next()
                            S.op("dve", lambda e, r=r, a_=a_: e.reciprocal(out=r[:, 0:1], in_=a_[:, 128:129]), reads=[b_], writes=[br])
                            if c == 0:
                                ts("dve", O0[:, 128 * s:128 * s + 128], a_[:, 0:128], r[:, 0:1], None, ALU.mult, None, [b_, br], (), pr=[bO0])
                            else:
                                tt("dve", r[:, 1:2], r[:, 0:1], NEGLAM, ALU.mult, [br, bLV], [br])
                                o0, bo0 = OT2.next()
                                stt(o0[:, 128:256], a_[:, 0:128], r[:, 1:2], O0[:, 128 * s:128 * s + 128], ALU.mult, ALU.add, [b_, br, bO0], [bo0])
                                ss, bss = SM.next()
                                act(o0[:, 256:384], o0[:, 128:256], AF.Square, [bo0], [bo0, bss], accum_out=ss[:, 0:1])
                                rs, brs = rstd_from_ss(ss[:, 0:1], bss, 128)
                                ob_, bob = WKB.next()
                                stt(ob_[:, 0:128], o0[:, 128:256], rs, DNW[:], ALU.mult, ALU.mult, [bo0, brs, bDNW], [bob])
                                tt0 = t0 + 128 * s
                                stq("sp", OB[tt0:tt0 + 128, 128 * h:128 * h + 128], ob_[:, 0:128], bob, dB["OB"])

            ph = new_phase()
            SF = [SB("sf%d" % h, [128, 128], F32, ph) for h in range(4)]
            SBF = [Pool("sbf%d_" % h, [128, 128], BF16, 2, ph) for h in range(4)]
            GP = Pool("gp", [128, 512], F32, 2, ph)
            QTp = Pool("qtp", [128, 512], BF16, 2, ph)
            KTp = Pool("ktp", [128, 512], BF16, 2, ph)
            KKp = Pool("kkp", [128, 512], BF16, 2, ph)
            VVp = Pool("vvp", [128, 512], BF16, 2, ph)
            E1p = Pool("e1p", [128, 512], F32, 2, ph)
            E2p = Pool("e2p", [128, 512], F32, 2, ph)
            QSp = Pool("qsp", [128, 512], BF16, 2, ph)
            KSp = Pool("ksp", [128, 512], BF16, 2, ph)
            KHp = Pool("khp", [128, 512], BF16, 2, ph)
            KHCp = Pool("khcp", [128, 4, 512], BF16, 2, ph)
            QSCp = Pool("qscp", [128, 4, 128], BF16, 4, ph)
            ATp = Pool("atp", [128, 128], BF16, 3, ph)
            OOp = Pool("oop", [128, 512], F32, 2, ph)
            for d_ in range(2):
                Um_f = UF_f if d_ == 0 else UB_f
                Dm_f = DF_f if d_ == 0 else DB_f
                Um_b = UF_b if d_ == 0 else UB_b
                if d_ == 0:
                    order = list(range(NT))
                else:
                    order = list(range(NCT - 1, -1, -1)) + list(range(NT - 1, NCT - 1, -1))
                corder = [0, 1, 2, 3] if d_ == 0 else [3, 2, 1, 0]
                cur = []
                for h in range(4):
                    memset("pool", SF[h][0][:], 0.0, [SF[h][1]])
                    sb_, bsb_ = SBF[h].next()
                    memset("pool", sb_[:], 0.0, [bsb_])
                    cur.append((sb_, bsb_))
                for i in order:
                    t0 = 128 * i
                    g, bg = GP.next(); ld("sp", g[:], HG[t0:t0 + 128, 512 * d_:512 * d_ + 512], bg, [dB["HG"]])
                    qT, bqT = QTp.next(); ld("sp", qT[:].rearrange("p (h t) -> p h t", h=4), HQ[:, :, t0:t0 + 128].rearrange("h p t -> p h t"), bqT, [dB["HQ"]])
                    kT, bkT = KTp.next(); ld("sp", kT[:].rearrange("p (h t) -> p h t", h=4), HK[4 * d_:4 * d_ + 4, :, t0:t0 + 128].rearrange("h p t -> p h t"), bkT, [dB["HK"]])
                    kk, bkk = KKp.next(); ld("sp", kk[:], HKt[t0:t0 + 128, 512 * d_:512 * d_ + 512], bkk, [dB["HKt"]])
                    vv, bvv = VVp.next(); ld("sp", vv[:], HV[t0:t0 + 128, :], bvv, [dB["HV"]])
                    pB, bpB = PS[0]
                    for h in range(4):
                        mmp(pB[:, 128 * h:128 * h + 128], g[:, 128 * h:128 * h + 128], Um_f, True, True, [bg, bCF], bpB) if h else \
                            mm(pB[:, 0:128], g[:, 0:128], Um_f, True, True, [bg, bCF], bpB)
                    e1, be1 = E1p.next(); act(e1[:], pB[:], AF.Exp, [bpB], [be1])
                    e2, be2 = E2p.next(); act(e2[:], pB[:], AF.Exp, [bpB], [be2], scale=-1.0)
                    qs, bqs = QSp.next(); tt("dve", qs[:], qT[:], e1[:], ALU.mult, [bqT, be1], [bqs])
                    ks, bks = KSp.next(); tt("dve", ks[:], kT[:], e2[:], ALU.mult, [bkT, be2], [bks])
                    pD, bpD = PS[1]
                    mm(pD[:], Dm_f, g[:], True, True, [bCF, bg], bpD)
                    e3, be3 = E2p.next(); act(e3[:], pD[:], AF.Exp, [bpD], [be3])
                    kh, bkh = KHp.next(); tt("dve", kh[:], kk[:], e3[:], ALU.mult, [bkk, be3], [bkh])
                    khc, bkhc = KHCp.next()
                    for c in range(4):
                        ts("dve", khc[:, c, :], kh[:], RM[:, c:c + 1], None, ALU.mult, None, [bkh, bRM], (), pr=[bkhc])
                    pO, bpO = PS[2 + (i % 2)]
                    for h in range(4):
                        hs = slice(128 * h, 128 * h + 128)
                        qsc, bqsc = QSCp.next()
                        for c in range(4):
                            tt("pool", qsc[:, c, :], qs[:, hs], BDC[:, c, :], ALU.mult, [bqs, bCB], (), pr=[bqsc])
                        pA, bpA = PS[4 + (h % 2)]
                        mm(pA[:, 0:128], ks[:, hs], qs[:, hs], True, True, [bks, bqs], bpA)
                        at_, bat = ATp.next()
                        tt("dve", at_[:], pA[:, 0:128], Um_b, ALU.mult, [bpA, bCB], [bat])
                        if h == 0:
                            mm(pO[:, hs], at_[:], vv[:, hs], True, False, [bat, bvv], bpO)
                        else:
                            mmp(pO[:, hs], at_[:], vv[:, hs], True, False, [bat, bvv], bpO)
                        sf, bsf = SF[h]
                        for ci, c in enumerate(corder):
                            sb_, bsb_ = cur[h]
                            mmp(pO[:, hs], qsc[:, c, :], sb_[:], False, ci == 3, [bqsc, bsb_], bpO)
                            pU, bpU = PS[6 + (ci % 2)]
                            mm(pU[:, 0:128], khc[:, c, hs], vv[:, hs], True, True, [bkhc, bvv], bpU)
                            tl = 32 * c + 31 if d_ == 0 else 32 * c
                            stt(sf[:], sf[:], e1[:, 128 * h + tl:128 * h + tl + 1], pU[:, 0:128], ALU.mult, ALU.add, [bsf, be1, bpU], [bsf])
                            nb_, bnb_ = SBF[h].next()
                            cp("act", nb_[:], sf[:], [bsf], [bnb_])
                            cur[h] = (nb_, bnb_)
                    oo, boo = OOp.next()
                    if d_ == 0:
                        cp("act", oo[:], pO[:], [bpO], [boo])
                        stq("sp", OF[t0:t0 + 128, :], oo[:], boo, dB["OF"])
                    else:
                        of_, bof = GP.next()
                        ld("sp", of_[:], OF[t0:t0 + 128, :], bof, [dB["OF"]])
                        tt("dve", oo[:], pO[:], of_[:], ALU.add, [bpO, bof], [boo])
                        gt_, bgt = WKB.next()
                        ld("sp", gt_[:, 0:512], HGT[t0:t0 + 128, :], bgt, [dB["HGT"]])
                        ss, bss = SM.next()
                        junk, bj = WK4.next()
                        for h in range(4):
                            hs = slice(128 * h, 128 * h + 128)
                            S.op("act", lambda e, junk=junk, oo=oo, ss=ss, hs=hs, h=h: e.activation(out=junk[:, hs], in_=oo[:, hs], func=AF.Square, accum_out=ss[:, h:h + 1]),
                                 reads=[boo], partial=[bj, bss])
                        t, b = SM.next()
                        S.op("act", lambda e, t=t, ss=ss: e.activation(out=t[:, 0:4], in_=ss[:, 0:4], func=AF.Sqrt, bias=eps_t[:, 0:1], scale=1.0 / 128),
                             reads=[bss, beps], writes=[b])
                        S.op("dve", lambda e, t=t: e.reciprocal(out=t[:, 4:8], in_=t[:, 0:4]), reads=[b], writes=[b])
                        oc, boc = WKB.next()
                        for h in range(4):
                            hs = slice(128 * h, 128 * h + 128)
                            stt(junk[:, hs], oo[:, hs], t[:, 4 + h:5 + h], HNW[:], ALU.mult, ALU.mult, [boo, b, bHNW], (), pr=[bj])
                        tt("dve", oc[:, 0:512], junk[:, 0:512], gt_[:, 0:512], ALU.mult, [bj, bgt], [boc])
                        stq("sp", OC[t0:t0 + 128, :], oc[:, 0:512], boc, dB["OC"])

            ph = new_phase()
            WBR, bWBR = SB("WBR", [128, 12, D], BF16, ph)
            ld("pool", WBR[:], w_br[l].rearrange("i (k p) n -> p (i k) n", p=128), bWBR)
            WO, bWO = SB("WO", [128, 8, D], BF16, ph)
            ld("pool", WO[:], w_out[l].rearrange("(k p) n -> p k n", p=128), bWO)
            WR, bWR = SB("WR", [128, 8, NE], F32, ph)
            ld("sp", WR[:], w_router[l].rearrange("(k p) n -> p k n", p=128), bWR)
            BR, bBR = SB("BR", [128, NE], F32, ph)
            ld("sp", BR[:], b_router[l].partition_broadcast(128), bBR)
            memset("pool", RS_[:], 0.0, [bRS])
            OTp = Pool("otp", [128, 12, 128], BF16, 2, ph)
            GTp = Pool("gtp", [128, 3072], BF16, 2, ph)
            H2Tp = Pool("h2t", [128, 8, 128], F32, 2, ph)
            SMR = Pool("smr", [128, 4, NE], F32, 3, ph)
            zt, bzt = WKB.next()
            memset("pool", zt[:], 0.0, [bzt])
            for r0 in range(0, NSLOT, 128):
                stq("sp", XBUF[r0:r0 + 128, :], zt[:], bzt, dB["XBUF"])
            for i in range(NT):
                t0 = 128 * i
                ms, bms = (MODC, bMODC) if i < NCT else (MODL, bMODL)
                ob3, bob3 = WKB.next(), None
                oin, boin = ob3
                oin2, boin2 = WKB.next()
                ld("sp", oin[:, 0:512], OA[t0:t0 + 128, :], boin, [dB["OA"]])
                ld("sp", oin[:, 512:1024], OB[t0:t0 + 128, :], boin, [dB["OB"]])
                ld("sp", oin2[:, 0:512], OC[t0:t0 + 128, :], boin2, [dB["OC"]])
                gts, bgts = GTp.next()
                ld("sp", gts[:], GT[t0:t0 + 128, :], bgts, [dB["GT"]])
                pt, bpt = PS[7]
                ptb = pt[:].bitcast(BF16)
                oT, boT = OTp.next()
                for kc in range(8):
                    tr(ptb[:, kc * 128:(kc + 1) * 128], oin[:, kc * 128:(kc + 1) * 128], ID_b, [boin, bCB], bpt, kc == 0)
                cp("act", oT[:, 0:8, :], ptb.rearrange("p (k t) -> p k t", k=8), [bpt], [boT])
                for kc in range(4):
                    tr(ptb[:, kc * 128:(kc + 1) * 128], oin2[:, kc * 128:(kc + 1) * 128], ID_b, [boin2, bCB], bpt, kc == 0)
                cp("act", oT[:, 8:12, :], ptb[:, 0:512].rearrange("p (k t) -> p k t", k=4), [bpt], (), pr=[boT])
                y, by = WK4.next()
                tmpy, btmpy = WK4.next()
                for br_ in range(3):
                    for cb in range(2):
                        pp, bpp = PS[cb]
                        for kc in range(4):
                            mm(pp[:], oT[:, 4 * br_ + kc, :], WBR[:, 4 * br_ + kc, 512 * cb:512 * cb + 512], kc == 0, kc == 3, [boT, bWBR], bpp)
                        gsl = gts[:, D * br_ + 512 * cb:D * br_ + 512 * cb + 512]
                        if br_ == 0:
                            tt("dve", y[:, 512 * cb:512 * cb + 512], pp[:], gsl, ALU.mult, [bpp, bgts], (), pr=[by])
                        else:
                            tt("dve", tmpy[:, 512 * cb:512 * cb + 512], pp[:], gsl, ALU.mult, [bpp, bgts], (), pr=[btmpy])
                            tt("pool", y[:, 512 * cb:512 * cb + 512], y[:, 512 * cb:512 * cb + 512], tmpy[:, 512 * cb:512 * cb + 512], ALU.add, [btmpy, by], [by])
                yb, byb = WKB.next()
                cp("act", yb[:], y[:], [by], [byb])
                for kc in range(8):
                    tr(ptb[:, kc * 128:(kc + 1) * 128], yb[:, kc * 128:(kc + 1) * 128], ID_b, [byb, bCB], bpt, kc == 0)
                yT, byT = WKB.next()
                cp("act", yT[:], ptb, [bpt], [byT])
                ss, bss = SM.next()
                junk, bj = WK4.next()
                pps = []
                for cb in range(2):
                    pp, bpp = PS[2 + cb]
                    for kc in range(8):
                        mm(pp[:], yT[:, kc * 128:(kc + 1) * 128], WO[:, kc, 512 * cb:512 * cb + 512], kc == 0, kc == 7, [byT, bWO], bpp)
                    S.op("act", lambda e, junk=junk, pp=pp, ss=ss, cb=cb: e.activation(out=junk[:, 512 * cb:512 * cb + 512], in_=pp[:], func=AF.Square, accum_out=ss[:, cb:cb + 1]),
                         reads=[bpp], partial=[bj, bss])
                    pps.append((pp, bpp))
                tt("dve", ss[:, 2:3], ss[:, 0:1], ss[:, 1:2], ALU.add, [bss], [bss])
                rs, brs = rstd_from_ss(ss[:, 2:3], bss, D)
                xt, bx = WK4.next()
                ld("sp", xt[:], XR[t0:t0 + 128, :], bx, [dB["XR"]])
                for cb in range(2):
                    pp, bpp = pps[cb]
                    stt(junk[:, 512 * cb:512 * cb + 512], pp[:], rs, ms[:, 2, 512 * cb:512 * cb + 512], ALU.mult, ALU.mult, [bpp, brs, bms], (), pr=[bj])
                tt("dve", xt[:], xt[:], junk[:], ALU.add, [bx, bj], [bx])
                stq("sp", XR[t0:t0 + 128, :], xt[:], bx, dB["XR"])
                h2f, bh2f = WK4.next()
                hb2, bhb2 = norm_mod_T(xt, bx, ms, bms, 4, 3, t0, keep=(h2f, bh2f))
                h2T, bh2T = H2Tp.next()
                for half in range(2):
                    pr0, bpr0 = PS[4 + half]
                    for kc in range(4):
                        k2 = 4 * half + kc
                        tr(pr0[:, kc * 128:(kc + 1) * 128], h2f[:, k2 * 128:(k2 + 1) * 128], ID_f, [bh2f, bCF], bpr0, kc == 0)
                    cp("act", h2T[:, 4 * half:4 * half + 4, :], pr0[:].rearrange("p (k t) -> p k t", k=4), [bpr0], (), pr=[bh2T])
                pl, bpl = PS[6]
                for kc in range(8):
                    mm(pl[:, 0:NE], h2T[:, kc, :], WR[:, kc, :], kc == 0, kc == 7, [bh2T, bWR], bpl)
                sm, bsm = SMR.next()
                Lg = sm[:, 0, :]; Mk = sm[:, 1, :]; Pb = sm[:, 2, :]; Oh = sm[:, 3, :]
                tt("dve", Lg, pl[:, 0:NE], BR[:], ALU.add, [bpl, bBR], [bsm])
                t8, bt8 = SM.next()
                S.op("dve", lambda e, t8=t8, Lg=Lg: e.max(out=t8[:, 0:8], in_=Lg), reads=[bsm], writes=[bt8])
                ts("dve", t8[:, 8:9], t8[:, 0:1], -1.0, None, ALU.mult, None, [bt8], [bt8])
                S.op("act", lambda e, t8=t8: e.activation(out=t8[:, 9:13], in_=t8[:, 0:4], func=AF.Exp, bias=t8[:, 8:9], accum_out=t8[:, 13:14]),
                     reads=[bt8], writes=[bt8])
                S.op("dve", lambda e, t8=t8: e.reciprocal(out=t8[:, 14:15], in_=t8[:, 13:14]), reads=[bt8], writes=[bt8])
                ts("dve", G4[:, i, :], t8[:, 9:13], t8[:, 14:15], None, ALU.mult, None, [bt8], (), pr=[bG4])
                ts("dve", Mk, Lg, t8[:, 3:4], None, ALU.is_ge, None, [bsm, bt8], [bsm])
                pq, bpq = PS[6]
                mm(pq[:, 64:64 + NE], UTS_f, Mk, True, False, [bCF, bsm], bpq)
                mm(pq[:, 64:64 + NE], ONES_f, RS_[:], False, True, [bCF, bRS], bpq)
                tt("dve", Pb, pq[:, 64:64 + NE], EBASE[:, 0:NE], ALU.add, [bpq, bCF], [bsm])
                tt("pool", RS_[:], RS_[:], Mk, ALU.add, [bRS, bsm], [bRS])
                df, bdf = SM.next()
                for k in range(4):
                    ts("dve", Oh, Lg, t8[:, k:k + 1], None, ALU.is_equal, None, [bsm, bt8], [bsm])
                    tt("dve", Oh, Oh, Pb, ALU.mult, [bsm], [bsm])
                    S.op("dve", lambda e, df=df, Oh=Oh, k=k: e.reduce_sum(out=df[:, k:k + 1], in_=Oh, axis=AX.X), reads=[bsm], partial=[bdf])
                ts("dve", df[:, 0:4], df[:, 0:4], float(NSLOT - 1), None, ALU.min, None, [bdf], [bdf])
                cp("dve", DEST[:, i, :], df[:, 0:4], [bdf], (), pr=[bDEST])
                for k in range(4):
                    S.dma("pool", lambda e, i=i, k=k, hb2=hb2: e.indirect_dma_start(
                        out=XBUF, out_offset=bass.IndirectOffsetOnAxis(ap=DEST[:, i, k:k + 1], axis=0),
                        in_=hb2[:, :], in_offset=None),
                        bhb2.ds, reads=[bhb2, bDEST], writes=[dB["XBUF"]] if (i == 0 and k == 0) else (), partial=() if (i == 0 and k == 0) else [dB["XBUF"]])

            ph = new_phase()
            WGU = Pool("wgu", [128, 8, 2 * D], BF16, 1, ph)
            WDN = Pool("wdn", [128, 8, D], BF16, 1, ph)
            BGU = Pool("bgu", [128, 16], F32, 2, ph)
            BDN = Pool("bdn", [128, D], F32, 1, ph)
            XSp = Pool("xsp", [128, D], BF16, 3, ph)
            XTp = Pool("xtp", [128, 8, 512], BF16, 2, ph)
            ATT = Pool("att", [128, 8, 512], BF16, 1, ph)
            GCp = Pool("gcp", [128, 512], F32, 2, ph)
            SGp = Pool("sgp", [128, 512], F32, 2, ph)
            UCp = Pool("ucp", [128, 512], F32, 2, ph)
            YTp = Pool("ytp", [128, D], BF16, 2, ph)
            for e_ in range(NE):
                wg, bwg = WGU.next(); wd, bwd = WDN.next(); bg_, bbg = BGU.next(); bd_, bbd = BDN.next()
                for kc in range(8):
                    S.dma("pool", lambda e, wg=wg, kc=kc, e_=e_: e.dma_start(out=wg[:, kc, :], in_=w_gu[l, e_, kc * 128:(kc + 1) * 128, :]),
                          bwg.ds, writes=[bwg] if kc == 0 else (), partial=() if kc == 0 else [bwg])
                ld("pool", wd[:], w_dn[l, e_].rearrange("(k p) n -> p k n", p=128), bwd)
                ld("sp", bg_[:], b_gu_t[l, e_], bbg)
                ld("sp", bd_[:], b_dn[l, e_].partition_broadcast(128), bbd)
                for blk in range(CAP // 512):
                    r0 = e_ * CAP + 512 * blk
                    xT, bxT = XTp.next()
                    for s in range(4):
                        xs, bxs = XSp.next()
                        ld("sp", xs[:], XBUF[r0 + 128 * s:r0 + 128 * s + 128, :], bxs, [dB["XBUF"]])
                        pt, bpt = PS[6 + (s % 2)]
                        ptb = pt[:].bitcast(BF16)
                        for kc in range(8):
                            tr(ptb[:, kc * 128:(kc + 1) * 128], xs[:, kc * 128:(kc + 1) * 128], ID_b, [bxs, bCB], bpt, kc == 0)
                        cp("act", xT[:, :, 128 * s:128 * s + 128], ptb.rearrange("p (k t) -> p k t", k=8), [bpt], (), pr=[bxT])
                    aT, baT = ATT.next()
                    for f in range(8):
                        pg, bpg = PS[0 + (f % 2) * 2]
                        pu, bpu = PS[1 + (f % 2) * 2]
                        for kc in range(8):
                            mm(pg[:], wg[:, kc, 128 * f:128 * f + 128], xT[:, kc, :], kc == 0, kc == 7, [bwg, bxT], bpg)
                        for kc in range(8):
                            mm(pu[:], wg[:, kc, D + 128 * f:D + 128 * f + 128], xT[:, kc, :], kc == 0, kc == 7, [bwg, bxT], bpu)
                        gc, bgc = GCp.next(); sg, bsg = SGp.next(); uc, buc = UCp.next()
                        ts("dve", gc[:], pg[:], bg_[:, f:f + 1], 7.0, ALU.add, ALU.min, [bpg, bbg], [bgc])
                        act(sg[:], gc[:], AF.Silu, [bgc], [bsg], scale=1.702)
                        ts("dve", uc[:], pu[:], bg_[:, 8 + f:9 + f], 7.0, ALU.add, ALU.min, [bpu, bbg], [buc])
                        ts("dve", uc[:], uc[:], -7.0, 1.0, ALU.max, ALU.add, [buc], [buc])
                        stt(aT[:, f, :], uc[:], 1.0 / 1.702, sg[:], ALU.mult, ALU.mult, [buc, bsg], (), pr=[baT])
                    for s in range(4):
                        yt, byt = YTp.next()
                        for cb in range(2):
                            pp, bpp = PS[4 + cb]
                            for f in range(8):
                                mm(pp[:], aT[:, f, 128 * s:128 * s + 128], wd[:, f, 512 * cb:512 * cb + 512], f == 0, f == 7, [baT, bwd], bpp)
                            tt("dve", yt[:, 512 * cb:512 * cb + 512], pp[:], bd_[:, 512 * cb:512 * cb + 512], ALU.add, [bpp, bbd], (), pr=[byt])
                        stq("sp", YBUF[r0 + 128 * s:r0 + 128 * s + 128, :], yt[:], byt, dB["YBUF"])

            ph = new_phase()
            YG = Pool("yg", [128, 4, D], BF16, 2, ph)
            for i in range(NT):
                t0 = 128 * i
                ms, bms = (MODC, bMODC) if i < NCT else (MODL, bMODL)
                yg, byg = YG.next()
                for k in range(4):
                    S.dma("pool", lambda e, yg=yg, i=i, k=k: e.indirect_dma_start(
                        out=yg[:, k, :], out_offset=None, in_=YBUF,
                        in_offset=bass.IndirectOffsetOnAxis(ap=DEST[:, i, k:k + 1], axis=0)),
                        byg.ds, reads=[dB["YBUF"], bDEST], writes=[byg] if k == 0 else (), partial=() if k == 0 else [byg])
                f_, bf_ = WK4.next()
                ts("dve", f_[:], yg[:, 0, :], G4[:, i, 0:1], None, ALU.mult, None, [byg, bG4], [bf_])
                for k in range(1, 4):
                    stt(f_[:], yg[:, k, :], G4[:, i, k:k + 1], f_[:], ALU.mult, ALU.add, [byg, bG4, bf_], [bf_])
                junk, bj = WK4.next()
                ss, bss = SM.next()
                act(junk[:], f_[:], AF.Square, [bf_], [bj, bss], accum_out=ss[:, 0:1])
                rs, brs = rstd_from_ss(ss[:, 0:1], bss, D)
                xt, bx = WK4.next()
                ld("sp", xt[:], XR[t0:t0 + 128, :], bx, [dB["XR"]])
                stt(junk[:], f_[:], rs, ms[:, 5, :], ALU.mult, ALU.mult, [bf_, brs, bms], [bj])
                tt("dve", xt[:], xt[:], junk[:], ALU.add, [bx, bj], [bx])
                if l == nlayers - 1 and i >= NCT:
                    stq("sp", out[t0 - L_:t0 - L_ + 128, :], xt[:], bx, dB["out"])
                else:
                    stq("sp", XR[t0:t0 + 128, :], xt[:], bx, dB["XR"])

            S.barrier(list(DS.values())); S.emit(); phase[0].close(); phase[0] = None
            lay.close()
        S.wait_all("sp", list(dB.values()))
        S.emit()
        print("sems used:", S.nsem, "instr:", S.ninstr)
    return nc


def make_consts(S_, L_):
    T = S_ + L_
    GRID_W = 64
    rows = S_ // GRID_W
    row = np.repeat(np.arange(rows, dtype=np.float32), GRID_W)
    col = np.tile(np.arange(GRID_W, dtype=np.float32), rows)
    inv = (10000.0 ** (-np.arange(16, dtype=np.float32) / 16)).astype(np.float32)
    ang_r = row[:, None] * inv
    ang_c = col[:, None] * inv
    cos64 = np.ones((64, T), np.float32)
    sin64 = np.zeros((64, T), np.float32)
    for d in range(64):
        half, j = d // 32, d % 32
        n = j % 16
        ang = (ang_r if half == 0 else ang_c)[:, n]
        cos64[d, L_:] = np.cos(ang)
        sin64[d, L_:] = (-np.sin(ang)) if j < 16 else np.sin(ang)
    ropc = np.concatenate([cos64, cos64], 0)
    rops = np.concatenate([sin64, sin64], 0)
    s = np.arange(128)[:, None]
    t = np.arange(128)[None, :]
    same = (s // 32) == (t // 32)
    UF = (same & (s <= t)).astype(np.float32)
    UB = (same & (s >= t)).astype(np.float32)
    BD = same.astype(np.float32)
    cst_f = np.zeros((128, 8, 128), np.float32)
    cst_f[:, 0] = UF; cst_f[:, 1] = UB; cst_f[:, 2] = BD - UF; cst_f[:, 3] = BD - UB
    cst_f[:, 4] = (s < t).astype(np.float32)
    cst_f[:, 5] = 1.0
    cst_f[:, 6] = np.eye(128, dtype=np.float32)
    cst_f[:, 7, 0:NE] = (np.arange(NE) * CAP).astype(np.float32)[None, :]
    cst_b = np.zeros((128, 12, 128), np.float32)
    cst_b[:, 0] = np.eye(128)
    cst_b[:, 1] = (s >= t)
    cst_b[:, 2] = (s <= t)
    cst_b[:, 3] = UF; cst_b[:, 4] = UB
    for c in range(4):
        cst_b[:, 5 + c] = ((t // 32) == c).astype(np.float32) * np.ones((128, 1), np.float32)
    cst_b[0:64, 9, 0] = 1.0
    cst_b[64:128, 9, 1] = 1.0
    rowmask = np.zeros((128, 4), np.float32)
    for c in range(4):
        rowmask[32 * c:32 * c + 32, c] = 1.0
    return dict(ropc=ropc, rops=rops, cst_f=cst_f, cst_b=cst_b.astype(ml_dtypes.bfloat16), rowmask=rowmask)


def rope_perm_cols():
    cols = []
    for (c0, w) in ((C_AQ, 512), (C_AK, 128), (C_BQ, 512), (C_BK, 512)):
        for m in range(w):
            blk, j = m // 32, m % 32
            cols.append(c0 + blk * 32 + (j + 16) % 32)
    return np.array(cols, dtype=np.int64)


def make_in_maps(inputs, S_, L_, n_cores=8):
    f = lambda a: np.ascontiguousarray(np.asarray(a, dtype=np.float32))
    x = f(inputs["x"]); c = f(inputs["c"]); ctx = f(inputs["ctx"]); c_ctx = f(inputs["c_ctx"])
    w_in = f(inputs["w_in"])
    consts = make_consts(S_, L_)
    shared = dict(
        cc_col=np.ascontiguousarray(c_ctx.reshape(8, 128).T),
        w_mod=f(inputs["w_mod"]), b_mod=f(inputs["b_mod"]), norm_g=f(inputs["norm_g"]).reshape(2, 4 * D),
        w_in=w_in, w_inp=np.ascontiguousarray(w_in[:, :, rope_perm_cols()]),
        sink=f(inputs["attn_sink"]), dlam=f(inputs["diff_lambda"]).reshape(2, 256), dnw=f(inputs["diff_norm_w"]),
        lbl=f(inputs["hgrn_lb_logits"]).reshape(2, 1024),
        lbl_fm=np.ascontiguousarray(f(inputs["hgrn_lb_logits"]).reshape(2, 8, 128).transpose(0, 2, 1)),
        hnw=f(inputs["hgrn_norm_w"]), w_br=f(inputs["w_branch"]), w_out=f(inputs["w_out"]),
        w_router=f(inputs["w_router"]), b_router=f(inputs["b_router"]),
        w_gu=f(inputs["w_gate_up"]),
        b_gu_t=np.ascontiguousarray(f(inputs["b_gate_up"]).reshape(2, NE, 16, 128).transpose(0, 1, 3, 2)),
        w_dn=f(inputs["w_down"]), b_dn=f(inputs["b_down"]),
    )
    shared.update(consts)
    maps = []
    B = x.shape[0]
    for core in range(n_cores):
        b = core * B // n_cores
        m = dict(shared)
        m["xin"] = x[b]
        m["ctxin"] = ctx[b]
        m["c_col"] = np.ascontiguousarray(c[b].reshape(8, 128).T)
        maps.append(m)
    return maps


def kernel(**inputs):
    x = np.asarray(inputs["x"])
    B, S_, _ = x.shape
    L_ = np.asarray(inputs["ctx"]).shape[1]
    nc = bass.Bass("TRN2", target_bir_lowering=False)
    build(nc, S_, L_)
    maps = make_in_maps(inputs, S_, L_)
    res = run_bass_kernel_spmd(nc, maps, core_ids=list(range(8)))
    outs = [np.asarray(res.results[(b * 8) // B]["out"]) for b in range(B)]
    return np.stack(outs, 0).astype(np.float32)
```

```python
import contextlib
import numpy as np
import ml_dtypes
import concourse.bass as bass
import concourse.mybir as mybir
from concourse.bass_utils import run_bass_kernel_spmd

F32 = mybir.dt.float32
BF16 = mybir.dt.bfloat16
I32 = mybir.dt.int32
AF = mybir.ActivationFunctionType
ALU = mybir.AluOpType
AX = mybir.AxisListType

EPOCH = 30000
DEPOCH = 1800
COMPUTE = ("pe", "act", "dve", "pool")
ALLENG = ("pe", "act", "dve", "pool", "sp")

D = 1024
NE = 32
CAP = 3072
RMS_EPS = 1e-6


class DSem:
    __slots__ = ("name", "ep", "cnt")

    def __init__(self, name):
        self.name = name
        self.ep = 0
        self.cnt = 0


class Buf:
    __slots__ = ("name", "w", "r", "ds")

    def __init__(self, name, ds=None):
        self.name = name
        self.w = {}
        self.r = {}
        self.ds = ds


class Sched:
    def __init__(self, nc, stack):
        self.nc = nc
        self.stack = stack
        self.streams = {e: [] for e in ALLENG}
        self.cnt = {e: 0 for e in COMPUTE}
        self.waited = {e: {} for e in ALLENG}
        self.sems = {}
        self.nsem = 0

    def sem(self, key):
        s = self.sems.get(key)
        if s is None:
            s = self.stack.enter_context(self.nc.semaphore("s%d" % self.nsem))
            self.nsem += 1
            self.sems[key] = s
        return s

    def _wait(self, eng, key, val):
        if val <= 0:
            return
        if eng == "pe" and key[0] == "pe":
            return
        w = self.waited[eng]
        if w.get(key, 0) >= val:
            return
        w[key] = val
        self.streams[eng].append(("w", key, val))

    def _deps(self, eng, reads, writes, partial):
        for b in reads:
            for k, v in b.w.items():
                self._wait(eng, k, v)
        for b in writes:
            for k, v in b.w.items():
                self._wait(eng, k, v)
            for k, v in b.r.items():
                self._wait(eng, k, v)
        for b in partial:
            for k, v in b.r.items():
                self._wait(eng, k, v)

    def _record(self, tok, reads, writes, partial):
        k, v = tok
        for b in reads:
            if b.r.get(k, 0) < v:
                b.r[k] = v
        for b in writes:
            b.w = {k: v}
            b.r = {}
        for b in partial:
            if b.w.get(k, 0) < v:
                b.w[k] = v

    def op(self, eng, fn, reads=(), writes=(), partial=()):
        self._deps(eng, reads, writes, partial)
        n = self.cnt[eng]
        self.cnt[eng] = n + 1
        key = (eng, n // EPOCH)
        val = n % EPOCH + 1
        self.sem(key)
        self.streams[eng].append(("o", fn, key, 1))
        self._record((key, val), reads, writes, partial)

    def dma(self, eng, fn, ds, reads=(), writes=(), partial=()):
        self._deps(eng, reads, writes, partial)
        key = ("d", ds.name, ds.ep)
        self._wait(eng, key, ds.cnt)
        if ds.cnt >= 16 * DEPOCH:
            ds.ep += 1
            ds.cnt = 0
            key = ("d", ds.name, ds.ep)
        self.sem(key)
        ds.cnt += 16
        self.streams[eng].append(("o", fn, key, 16))
        self._record((key, ds.cnt), reads, writes, partial)

    def barrier(self, dsems):
        toks = []
        for eng in COMPUTE:
            c = self.cnt[eng]
            if c > 0:
                toks.append(((eng, (c - 1) // EPOCH), (c - 1) % EPOCH + 1))
        for ds in dsems:
            if ds.cnt > 0:
                toks.append((("d", ds.name, ds.ep), ds.cnt))
        for e in ALLENG:
            for k, v in toks:
                self._wait(e, k, v)

    def wait_all(self, eng, bufs):
        for b in bufs:
            for k, v in b.w.items():
                self._wait(eng, k, v)
            for k, v in b.r.items():
                self._wait(eng, k, v)

    def emit(self):
        nc = self.nc
        engmap = {"pe": "tensor", "act": "scalar", "dve": "vector", "pool": "gpsimd", "sp": "sync"}
        sems = self.sems
        streams = self.streams
        with nc.Block() as block:
            def mk(ename):
                def body(e):
                    for item in streams[ename]:
                        if item[0] == "w":
                            e.wait_ge(sems[item[1]], item[2])
                        else:
                            item[1](e).then_inc(sems[item[2]], item[3])
                return body
            for ename in ALLENG:
                if streams[ename]:
                    getattr(block, engmap[ename])(mk(ename))
        self.ninstr = getattr(self, "ninstr", 0) + sum(len(v) for v in streams.values())
        self.streams = {e: [] for e in ALLENG}


C_AQ, C_AK, C_AV, C_BQ, C_BK, C_BV, C_CQ, C_CI, C_FF, C_FB, C_CG, C_GT = (
    0, 512, 640, 768, 1280, 1792, 2304, 2816, 3328, 3840, 4352, 4864)
IN_W = 7936
P_AQ, P_AK, P_BQ, P_BK = 0, 512, 640, 1152
ROPE_W = 1664


def build(nc, S_, L_, dbg=(), nlayers=2):
    T = S_ + L_
    NT = T // 128
    NCT = L_ // 128
    NLT = S_ // 128
    tok_blocks = [(0, L_)] + [(L_ + 512 * i, 512) for i in range(S_ // 512)]
    NSLOT = NE * CAP

    def din(name, shape, dt=F32):
        return nc.dram_tensor(name, list(shape), dt, kind="ExternalInput").ap()

    def dscr(name, shape, dt):
        kind = "ExternalOutput" if name in dbg else "Internal"
        return nc.dram_tensor(name, list(shape), dt, kind=kind).ap()

    xin = din("xin", [S_, D]); ctxin = din("ctxin", [L_, D])
    c_col = din("c_col", [128, 8]); cc_col = din("cc_col", [128, 8])
    w_mod = din("w_mod", [2, D, 6 * D]); b_mod = din("b_mod", [2, 6 * D]); norm_g = din("norm_g", [2, 4 * D])
    w_in = din("w_in", [2, D, IN_W]); w_inp = din("w_inp", [2, D, ROPE_W])
    sink_in = din("sink", [2, 8]); dlam = din("dlam", [2, 256]); dnw = din("dnw", [2, 128])
    lbl = din("lbl", [2, 1024]); lbl_fm = din("lbl_fm", [2, 128, 8]); hnw = din("hnw", [2, 128])
    w_br = din("w_br", [2, 3, 512, D]); w_out = din("w_out", [2, D, D])
    w_router = din("w_router", [2, D, NE]); b_router = din("b_router", [2, NE])
    w_gu = din("w_gu", [2, NE, D, 2 * D]); b_gu_t = din("b_gu_t", [2, NE, 128, 16])
    w_dn = din("w_dn", [2, NE, D, D]); b_dn = din("b_dn", [2, NE, D])
    ropc = din("ropc", [128, T]); rops = din("rops", [128, T])
    cst_f = din("cst_f", [128, 8, 128])
    cst_b = din("cst_b", [128, 12, 128], BF16)
    rowmask_in = din("rowmask", [128, 4])
    out = nc.dram_tensor("out", [S_, D], F32, kind="ExternalOutput").ap()

    XR = dscr("XR", [T, D], F32)
    HT = dscr("HT", [8, 128, T], BF16)
    QA = dscr("QA", [8, 64, T], BF16); QAN = dscr("QAN", [8, T], F32)
    KA = dscr("KA", [2, 64, T], BF16); KAN = dscr("KAN", [2, T], F32)
    VA = dscr("VA", [T, 128], BF16)
    QB = dscr("QB", [8, 64, T], BF16); QBN = dscr("QBN", [8, T], F32)
    KB = dscr("KB", [8, 64, T], BF16); KBN = dscr("KBN", [8, T], F32)
    VB = dscr("VB", [T, 512], BF16)
    KMX = dscr("KMX", [10, 1], F32)
    HQ = dscr("HQ", [4, 128, T], BF16); HK = dscr("HK", [8, 128, T], BF16)
    HKt = dscr("HKt", [T, 1024], BF16); HG = dscr("HG", [T, 1024], F32)
    HV = dscr("HV", [T, 512], BF16); HGT = dscr("HGT", [T, 512], BF16)
    GT = dscr("GT", [T, 3072], BF16)
    OA = dscr("OA", [T, 512], BF16); OB = dscr("OB", [T, 512], BF16); OC = dscr("OC", [T, 512], BF16)
    OF = dscr("OF", [T, 512], F32)
    XBUF = dscr("XBUF", [NSLOT, D], BF16); YBUF = dscr("YBUF", [NSLOT, D], BF16)

    with contextlib.ExitStack() as st:
        S = Sched(nc, st)
        DS = {}
        uniq = [0]

        def SB(name, shape, dt, stack=None):
            uniq[0] += 1
            t = (stack or st).enter_context(nc.sbuf_tensor("%s_%d" % (name, uniq[0]), list(shape), dt))
            ds = DS.get(name)
            if ds is None:
                ds = DS[name] = DSem(name)
            return t, Buf(name, ds)

        class Pool:
            def __init__(self, name, shape, dt, n, stack=None):
                self.items = [SB("%s%d" % (name, i), shape, dt, stack) for i in range(n)]
                self.i = 0

            def next(self):
                it = self.items[self.i % len(self.items)]
                self.i += 1
                return it

        dB = {n: Buf(n) for n in ["XR", "HT", "QA", "QAN", "KA", "KAN", "VA", "QB", "QBN", "KB", "KBN", "VB", "KMX",
                                  "HQ", "HK", "HKt", "HG", "HV", "HGT", "GT", "OA", "OB", "OC", "OF", "XBUF", "YBUF", "out"]}

        def ld(eng, dst, src, dbuf, rd=()):
            S.dma(eng, lambda e: e.dma_start(out=dst, in_=src), dbuf.ds, reads=rd, writes=[dbuf])

        def stq(eng, dst, src, sbuf, dram):
            S.dma("act", lambda e: e.dma_start(out=dst, in_=src), sbuf.ds, reads=[sbuf], partial=[dram])

        def mm(o, lhsT, rhs, first, last, rd, pb):
            S.op("pe", lambda e: e.matmul(o, lhsT=lhsT, rhs=rhs, start=first, stop=last), reads=rd,
                 writes=[pb] if first else (), partial=() if first else [pb])

        def mmp(o, lhsT, rhs, first, last, rd, pb):
            S.op("pe", lambda e: e.matmul(o, lhsT=lhsT, rhs=rhs, start=first, stop=last), reads=rd, partial=[pb])

        def tr(o, i_, idt, rd, pb, first):
            S.op("pe", lambda e: e.transpose(out=o, in_=i_, identity=idt), reads=rd,
                 writes=[pb] if first else (), partial=() if first else [pb])

        def act(o, i_, func, rd, wr, eng="act", **kw):
            S.op(eng, lambda e: e.activation(out=o, in_=i_, func=func, **kw), reads=rd, writes=wr)

        def tt(eng, o, a, b, op, rd, wr, pr=()):
            S.op(eng, lambda e: e.tensor_tensor(out=o, in0=a, in1=b, op=op), reads=rd, writes=wr, partial=pr)

        def ts(eng, o, a, s1, s2, op0, op1, rd, wr, pr=(), **kw):
            if s2 is None:
                S.op(eng, lambda e: e.tensor_scalar(out=o, in0=a, scalar1=s1, scalar2=None, op0=op0, **kw), reads=rd, writes=wr, partial=pr)
            else:
                S.op(eng, lambda e: e.tensor_scalar(out=o, in0=a, scalar1=s1, scalar2=s2, op0=op0, op1=op1, **kw), reads=rd, writes=wr, partial=pr)

        def stt(o, a, s, b, op0, op1, rd, wr, pr=()):
            S.op("dve", lambda e: e.scalar_tensor_tensor(out=o, in0=a, scalar=s, in1=b, op0=op0, op1=op1), reads=rd, writes=wr, partial=pr)

        def cp(eng, o, i_, rd, wr, pr=()):
            if eng == "act":
                S.op("act", lambda e: e.copy(out=o, in_=i_), reads=rd, writes=wr, partial=pr)
            else:
                S.op(eng, lambda e: e.tensor_copy(out=o, in_=i_), reads=rd, writes=wr, partial=pr)

        def memset(eng, o, v, wr):
            S.op(eng, lambda e: e.memset(o, v), writes=wr)

        CF, bCF = SB("CF", [128, 8, 128], F32)
        CB, bCB = SB("CB", [128, 12, 128], BF16)
        RM, bRM = SB("RM", [128, 4], F32)
        ld("sp", CF[:], cst_f, bCF); ld("sp", CB[:], cst_b, bCB); ld("sp", RM[:], rowmask_in, bRM)
        UF_f, UB_f, DF_f, DB_f, UTS_f, ONES_f, ID_f, EBASE = [CF[:, i, :] for i in range(8)]
        ID_b, MPREV, MNEXT, UF_b, UB_b = [CB[:, i, :] for i in range(5)]
        BDC = CB[:, 5:9, :]
        BONES = CB[:, 9, 0:2]

        PS = [(st.enter_context(nc.psum_tensor("ps%d" % i, [128, 512], F32)), Buf("ps%d" % i)) for i in range(8)]

        MODL, bMODL = SB("MODL", [128, 6, D], F32)
        MODC, bMODC = SB("MODC", [128, 6, D], F32)
        eps_t, beps = SB("eps_t", [128, 1], F32)
        memset("pool", eps_t[:], RMS_EPS, [beps])

        WK4 = Pool("wk4", [128, D], F32, 4)
        WKB = Pool("wkb", [128, D], BF16, 4)
        SM = Pool("sm", [128, 16], F32, 8)
        phase = [None]

        def new_phase():
            if phase[0] is not None:
                S.barrier(list(DS.values()))
                S.emit()
                phase[0].close()
            phase[0] = contextlib.ExitStack()
            return phase[0]

        def rstd_from_ss(ss_ap, bss, n):
            t, b = SM.next()
            S.op("act", lambda e: e.activation(out=t[:, 0:1], in_=ss_ap, func=AF.Sqrt, bias=eps_t[:, 0:1], scale=1.0 / n),
                 reads=[bss, beps], writes=[b])
            t2, b2 = SM.next()
            S.op("dve", lambda e: e.reciprocal(out=t2[:, 0:1], in_=t[:, 0:1]), reads=[b], writes=[b2])
            return t2[:, 0:1], b2

        def norm_mod_T(xt, bx, ms, bms, gs, shs, t0, keep=None):
            junk, bj = WK4.next()
            ss, bss = SM.next()
            act(junk[:], xt[:], AF.Square, [bx], [bj, bss], accum_out=ss[:, 0:1])
            rs, brs = rstd_from_ss(ss[:, 0:1], bss, D)
            tmp, btmp = WK4.next()
            stt(tmp[:], xt[:], rs, ms[:, gs, :], ALU.mult, ALU.mult, [bx, brs, bms], [btmp])
            hb, bhb = WKB.next()
            tt("pool", hb[:], tmp[:], ms[:, shs, :], ALU.add, [btmp, bms], [bhb])
            if keep is not None:
                tt("dve", keep[0][:], tmp[:], ms[:, shs, :], ALU.add, [btmp, bms], [keep[1]])
            pt, bpt = PS[7]
            ptb = pt[:].bitcast(BF16)
            for kc in range(8):
                tr(ptb[:, kc * 128:(kc + 1) * 128], hb[:, kc * 128:(kc + 1) * 128], ID_b, [bhb, bCB], bpt, kc == 0)
            hT, bhT = WKB.next()
            cp("act", hT[:], ptb, [bpt], [bhT])
            stq("sp", HT[:, :, t0:t0 + 128].rearrange("k p t -> p k t"), hT[:].rearrange("p (k t) -> p k t", k=8), bhT, dB["HT"])
            return hb, bhb

        for l in range(nlayers):
            lam_init = 0.8 - 0.6 * float(np.exp(-0.3 * l))
            if phase[0] is not None:
                S.barrier(list(DS.values())); S.emit(); phase[0].close(); phase[0] = None
            lay = contextlib.ExitStack()
            LBT, bLBT = SB("LBT", [128, 1024], F32, lay)
            OMT, bOMT = SB("OMT", [128, 1024], F32, lay)
            LBF, bLBF = SB("LBF", [128, 8], F32, lay)
            OMF, bOMF = SB("OMF", [128, 8], F32, lay)
            DL, bDL = SB("DL", [128, 256], F32, lay)
            LV, bLV = SB("LV", [128, 8], F32, lay)
            DNW, bDNW = SB("DNW", [128, 128], F32, lay)
            HNW, bHNW = SB("HNW", [128, 128], F32, lay)
            SK, bSK = SB("SK", [128, 8], F32, lay)
            KMB, bKMB = SB("KMB", [128, 10], F32, lay)
            NKM, bNKM = SB("NKM", [128, 10], F32, lay)
            NKM8, bNKM8 = SB("NKM8", [128, 10], F32, lay)
            RS_, bRS = SB("RS", [128, NE], F32, lay)
            DEST, bDEST = SB("DEST", [128, NT + 1, 4], I32, lay)
            G4, bG4 = SB("G4", [128, NT, 4], F32, lay)
            ph = new_phase()
            W16a, bW16a = SB("W16a", [128, 8, 512], F32, ph)
            W16b, bW16b = SB("W16b", [128, 8, 512], F32, ph)
            cs, bcs = SB("cs", [128, 16], F32, ph)
            ld("sp", cs[:, 0:8], c_col, bcs); ld("sp", cs[:, 8:16], cc_col, bcs)
            act(cs[:], cs[:], AF.Silu, [bcs], [bcs])
            LH, bLH = SB("LH", [128, 16, 128], F32, ph)
            for j in range(16):
                cp("pool", LH[:, j, :], cs[:, j:j + 1].to_broadcast([128, 128]), [bcs], (), pr=[bLH])
            NG, bNG = W16b, bW16b
            for cb in range(12):
                ld("sp", W16a[:], w_mod[l, :, cb * 512:(cb + 1) * 512].rearrange("(k p) n -> p k n", p=128), bW16a)
                bm, bbm = WK4.next()
                ld("sp", bm[:, 0:512], b_mod[l, cb * 512:(cb + 1) * 512].partition_broadcast(128), bbm)
                for si, (ms, bms) in enumerate(((MODL, bMODL), (MODC, bMODC))):
                    pp, bpp = PS[si]
                    for kc in range(8):
                        mm(pp[:], LH[:, si * 8 + kc, :], W16a[:, kc, :], kc == 0, kc == 7, [bLH, bW16a], bpp)
                    tt("dve", ms[:, cb // 2, (cb % 2) * 512:(cb % 2) * 512 + 512], pp[:], bm[:, 0:512], ALU.add, [bpp, bbm], (), pr=[bms])
            ld("sp", NG[:].rearrange("p k n -> p (k n)"), norm_g[l].partition_broadcast(128), bNG)
            NGv = NG[:].rearrange("p k n -> p (k n)")
            for ms, bms in ((MODL, bMODL), (MODC, bMODC)):
                stt(ms[:, 1, :], ms[:, 1, :], 1.0, NGv[:, 0:D], ALU.add, ALU.mult, [bms, bNG], [bms])
                tt("dve", ms[:, 2, :], ms[:, 2, :], NGv[:, D:2 * D], ALU.mult, [bms, bNG], [bms])
                stt(ms[:, 4, :], ms[:, 4, :], 1.0, NGv[:, 2 * D:3 * D], ALU.add, ALU.mult, [bms, bNG], [bms])
                tt("dve", ms[:, 5, :], ms[:, 5, :], NGv[:, 3 * D:4 * D], ALU.mult, [bms, bNG], [bms])

            if l == 0:
                memset("pool", LBT[:], 0.0, [bLBT]); memset("pool", OMT[:], 1.0, [bOMT])
                memset("pool", LBF[:], 0.0, [bLBF]); memset("pool", OMF[:], 1.0, [bOMF])
            else:
                t1, b1 = WK4.next()
                ld("sp", LBT[:], lbl[1].partition_broadcast(128), bLBT)
                ld("sp", t1[:], lbl[0].partition_broadcast(128), b1)
                tt("dve", LBT[:], LBT[:], t1[:], ALU.subtract, [bLBT, b1], [bLBT])
                act(LBT[:], LBT[:], AF.Sigmoid, [bLBT], [bLBT])
                ts("dve", OMT[:], LBT[:], -1.0, 1.0, ALU.mult, ALU.add, [bLBT], [bOMT])
                t2, b2 = SM.next()
                ld("sp", LBF[:], lbl_fm[1], bLBF); ld("sp", t2[:, 0:8], lbl_fm[0], b2)
                tt("dve", LBF[:], LBF[:], t2[:, 0:8], ALU.subtract, [bLBF, b2], [bLBF])
                act(LBF[:], LBF[:], AF.Sigmoid, [bLBF], [bLBF])
                ts("dve", OMF[:], LBF[:], -1.0, 1.0, ALU.mult, ALU.add, [bLBF], [bOMF])
            ld("sp", DL[:], dlam[l].partition_broadcast(128), bDL)
            tt("dve", DL[:, 0:64], DL[:, 0:64], DL[:, 64:128], ALU.mult, [bDL], [bDL])
            tt("dve", DL[:, 128:192], DL[:, 128:192], DL[:, 192:256], ALU.mult, [bDL], [bDL])
            S.op("dve", lambda e: e.reduce_sum(out=LV[:, 0:1], in_=DL[:, 0:64], axis=AX.X), reads=[bDL], writes=[bLV])
            S.op("dve", lambda e: e.reduce_sum(out=LV[:, 1:2], in_=DL[:, 128:192], axis=AX.X), reads=[bDL], partial=[bLV])
            act(LV[:, 0:2], LV[:, 0:2], AF.Exp, [bLV], [bLV])
            tt("dve", LV[:, 2:3], LV[:, 1:2], LV[:, 0:1], ALU.subtract, [bLV], [bLV])
            ts("dve", LV[:, 3:4], LV[:, 2:3], -lam_init, None, ALU.add, None, [bLV], [bLV])
            NEGLAM = LV[:, 3:4]
            ld("sp", DNW[:], dnw[l].partition_broadcast(128), bDNW)
            ts("dve", DNW[:], DNW[:], 1.0 - lam_init, None, ALU.mult, None, [bDNW], [bDNW])
            ld("sp", HNW[:], hnw[l].partition_broadcast(128), bHNW)
            ld("sp", SK[:], sink_in[l].partition_broadcast(128), bSK)

            ph = new_phase()
            if l == 0:
                S.dma("sp", lambda e: e.dma_start(out=XR[0:L_, :], in_=ctxin), bCF.ds, partial=[dB["XR"]])
                for r0 in range(0, S_, 512):
                    S.dma("sp", lambda e, r0=r0: e.dma_start(out=XR[L_ + r0:L_ + r0 + 512, :], in_=xin[r0:r0 + 512, :]), bCB.ds, partial=[dB["XR"]])
            for i in range(NT):
                t0 = 128 * i
                ms, bms = (MODC, bMODC) if i < NCT else (MODL, bMODL)
                xt, bx = WK4.next()
                ld("sp", xt[:], XR[t0:t0 + 128, :], bx, [dB["XR"]])
                norm_mod_T(xt, bx, ms, bms, 1, 0, t0)

            ph = new_phase()
            WB8 = Pool("wb8", [128, 8, 512], BF16, 2, ph)
            HB = Pool("hb", [128, 8, 512], BF16, 3, ph)
            fm_jobs = []
            for i in range(4):
                fm_jobs.append((C_AQ + 128 * i, P_AQ + 128 * i, "rope", (QA, QAN, "QA", "QAN", 2 * i)))
            fm_jobs.append((C_AK, P_AK, "rope", (KA, KAN, "KA", "KAN", 0)))
            for i in range(4):
                fm_jobs.append((C_BQ + 128 * i, P_BQ + 128 * i, "rope", (QB, QBN, "QB", "QBN", 2 * i)))
            for i in range(4):
                fm_jobs.append((C_BK + 128 * i, P_BK + 128 * i, "rope", (KB, KBN, "KB", "KBN", 2 * i)))
            for i in range(4):
                fm_jobs.append((C_CQ + 128 * i, None, "silu", i))
            for i in range(4):
                fm_jobs.append((C_FF + 128 * i, None, "kfm", i))
            for i in range(4):
                fm_jobs.append((C_FB + 128 * i, None, "kfm", 4 + i))
            for (c0, p0, kind, info) in fm_jobs:
                wt, bwt = WB8.next()
                ld("pool", wt[:, :, 0:128], w_in[l, :, c0:c0 + 128].rearrange("(k p) n -> p k n", p=128), bwt)
                if kind == "rope":
                    ld("pool", wt[:, :, 128:256], w_inp[l, :, p0:p0 + 128].rearrange("(k p) n -> p k n", p=128), bwt)
                for (t0, n) in tok_blocks:
                    hb, bhb = HB.next()
                    ld("sp", hb[:, :, 0:n], HT[:, :, t0:t0 + n].rearrange("k p t -> p k t"), bhb, [dB["HT"]])
                    pa, bpa = PS[0]
                    for kc in range(8):
                        mm(pa[:, 0:n], wt[:, kc, 0:128], hb[:, kc, 0:n], kc == 0, kc == 7, [bwt, bhb], bpa)
                    if kind == "rope":
                        Qd, Nd, qn, nn, h0 = info
                        pb_, bpb = PS[1]
                        for kc in range(8):
                            mm(pb_[:, 0:n], wt[:, kc, 128:256], hb[:, kc, 0:n], kc == 0, kc == 7, [bwt, bhb], bpb)
                        rc, brc = WK4.next()
                        ld("sp", rc[:, 0:n], ropc[:, t0:t0 + n], brc); ld("sp", rc[:, 512:512 + n], rops[:, t0:t0 + n], brc)
                        t1, b1 = WK4.next()
                        tt("dve", t1[:, 0:n], pa[:, 0:n], rc[:, 0:n], ALU.mult, [bpa, brc], [b1])
                        tt("dve", t1[:, 512:512 + n], pb_[:, 0:n], rc[:, 512:512 + n], ALU.mult, [bpb, brc], (), pr=[b1])
                        qt, bqt = WKB.next()
                        tt("pool", qt[:, 0:n], t1[:, 0:n], t1[:, 512:512 + n], ALU.add, [b1], [bqt])
                        sq, bsq = WKB.next()
                        act(sq[:, 0:n], qt[:, 0:n], AF.Square, [bqt], [bsq])
                        pn, bpn = PS[2]
                        mm(pn[0:2, 0:n], BONES, sq[:, 0:n], True, True, [bCB, bsq], bpn)
                        nr, bnr = WK4.next()
                        act(nr[0:2, 0:n], pn[0:2, 0:n], AF.Sqrt, [bpn], [bnr])
                        stq("sp", Nd[h0:h0 + 2, t0:t0 + n], nr[0:2, 0:n], bnr, dB[nn])
                        stq("sp", Qd[h0, :, t0:t0 + n], qt[0:64, 0:n], bqt, dB[qn])
                        stq("sp", Qd[h0 + 1, :, t0:t0 + n], qt[64:128, 0:n], bqt, dB[qn])
                    elif kind == "silu":
                        qt, bqt = WKB.next()
                        act(qt[:, 0:n], pa[:, 0:n], AF.Silu, [bpa], [bqt])
                        stq("sp", HQ[info, :, t0:t0 + n], qt[:, 0:n], bqt, dB["HQ"])
                    else:
                        t1, b1 = WK4.next()
                        act(t1[:, 0:n], pa[:, 0:n], AF.Sigmoid, [bpa], [b1], scale=-1.0)
                        qt, bqt = WKB.next()
                        ts("dve", qt[:, 0:n], t1[:, 0:n], OMF[:, info:info + 1], None, ALU.mult, None, [b1, bOMF], [bqt])
                        stq("sp", HK[info, :, t0:t0 + n], qt[:, 0:n], bqt, dB["HK"])

            tm_jobs = [(C_AV, 128, "copy", (VA, "VA", 0)), (C_BV, 512, "copy", (VB, "VB", 0)), (C_CI, 512, "copy", (HV, "HV", 0)),
                       (C_CG, 512, "silu", (HGT, "HGT", 0)), (C_FF, 512, "logf", 0), (C_FB, 512, "logf", 1)]
            for j in range(6):
                tm_jobs.append((C_GT + 512 * j, 512, "sigm", (GT, "GT", 512 * j)))
            for (c0, w, kind, info) in tm_jobs:
                wt, bwt = WB8.next()
                ld("pool", wt[:, :, 0:w], w_in[l, :, c0:c0 + w].rearrange("(k p) n -> p k n", p=128), bwt)
                for (t0, n) in tok_blocks:
                    hb, bhb = HB.next()
                    ld("sp", hb[:, :, 0:n], HT[:, :, t0:t0 + n].rearrange("k p t -> p k t"), bhb, [dB["HT"]])
                    for s in range(n // 128):
                        tt0 = t0 + 128 * s
                        pa, bpa = PS[s % 2]
                        for kc in range(8):
                            mm(pa[:, 0:w], hb[:, kc, 128 * s:128 * s + 128], wt[:, kc, 0:w], kc == 0, kc == 7, [bwt, bhb], bpa)
                        if kind in ("copy", "silu", "sigm"):
                            dst, dn, co = info
                            ot, bot = WKB.next()
                            fn = {"copy": AF.Copy, "silu": AF.Silu, "sigm": AF.Sigmoid}[kind]
                            act(ot[:, 0:w], pa[:, 0:w], fn, [bpa], [bot])
                            stq("sp", dst[tt0:tt0 + 128, co:co + w], ot[:, 0:w], bot, dB[dn])
                        else:
                            d_ = info
                            t1, b1 = WK4.next()
                            act(t1[:, 0:512], pa[:, 0:512], AF.Sigmoid, [bpa], [b1])
                            tt("dve", t1[:, 0:512], t1[:, 0:512], OMT[:, d_ * 512:(d_ + 1) * 512], ALU.mult, [b1, bOMT], [b1])
                            tt("dve", t1[:, 0:512], t1[:, 0:512], LBT[:, d_ * 512:(d_ + 1) * 512], ALU.add, [b1, bLBT], [b1])
                            kt, bkt = WKB.next()
                            ts("dve", kt[:, 0:512], t1[:, 0:512], -1.0, 1.0, ALU.mult, ALU.add, [b1], [bkt])
                            stq("sp", HKt[tt0:tt0 + 128, d_ * 512:(d_ + 1) * 512], kt[:, 0:512], bkt, dB["HKt"])
                            act(t1[:, 512:1024], t1[:, 0:512], AF.Ln, [b1], [b1])
                            stq("sp", HG[tt0:tt0 + 128, d_ * 512:(d_ + 1) * 512], t1[:, 512:1024], b1, dB["HG"])

            ph = new_phase()
            kmrow, bkmrow = SB("kmrow", [1, T], F32, ph)
            kmv, bkmv = SB("kmv", [1, 16], F32, ph)
            for ku in range(10):
                src = KAN[ku:ku + 1, :] if ku < 2 else KBN[ku - 2:ku - 1, :]
                ld("sp", kmrow[:], src, bkmrow, [dB["KAN"], dB["KBN"]])
                S.op("dve", lambda e, ku=ku: e.reduce_max(out=kmv[:, ku:ku + 1], in_=kmrow[:], axis=AX.X), reads=[bkmrow], partial=[bkmv])
            stq("sp", KMX.rearrange("a b -> b a"), kmv[:, 0:10], bkmv, dB["KMX"])
            ld("sp", KMB[:], KMX.rearrange("a b -> (a b)").partition_broadcast(128), bKMB, [dB["KMX"]])
            ts("dve", NKM[:], KMB[:], -1.0, None, ALU.mult, None, [bKMB], [bNKM])
            ts("dve", NKM8[:], KMB[:], -0.125, None, ALU.mult, None, [bKMB], [bNKM8])

            ph = new_phase()
            KAUG = [SB("kaug%d" % i, [65, T], BF16, ph) for i in range(2)]
            VS, bVS = SB("vs", [128, NT, 132], BF16, ph)
            memset("pool", VS[:], 1.0, [bVS])
            QAUG = Pool("qaug", [65, 512], BF16, 2, ph)
            QNR = Pool("qnr", [65, 512], F32, 2, ph)
            PT = Pool("pt", [128, 512], BF16, 4, ph)
            OT = Pool("ot", [128, 512], F32, 2, ph)
            OT2 = Pool("ot2", [128, 512], F32, 2, ph)
            for i in range(2):
                memset("pool", KAUG[i][0][64:65, :], 1.0, [KAUG[i][1]])

            def load_k(slot, Kd, kname, ku):
                kt_, bk_ = KAUG[slot]
                S.dma("sp", lambda e: e.dma_start(out=kt_[0:64, :], in_=Kd[ku]), bk_.ds, reads=[dB[kname]], writes=[bk_])

            def load_v(Vd, vname, v0, dv):
                for j0 in range(0, NT, 11):
                    j1 = min(NT, j0 + 11)
                    S.dma("sp", lambda e, j0=j0, j1=j1: e.dma_start(out=VS[:, j0:j1, 0:dv], in_=Vd[128 * j0:128 * j1, v0:v0 + dv].rearrange("(j p) d -> p j d", p=128)),
                          bVS.ds, reads=[dB[vname]], writes=[bVS] if j0 == 0 else (), partial=() if j0 == 0 else [bVS])
                memset("pool", VS[:, :, dv:dv + 1], 1.0, [bVS])

            def attn_unit(Qd, Nd, qname, nname, u, kslot, kmcol, t0, n, keys, dv, accs):
                kt_, bk_ = KAUG[kslot]
                qa, bqa = QAUG.next()
                ld("sp", qa[0:64, 0:n], Qd[u, :, t0:t0 + n], bqa, [dB[qname]])
                qn, bqn = QNR.next()
                ld("sp", qn[64:65, 0:n], Nd[u:u + 1, t0:t0 + n], bqn, [dB[nname]])
                S.op("act", lambda e: e.activation(out=qa[64:65, 0:n], in_=qn[64:65, 0:n], func=AF.Copy, scale=NKM[64:65, kmcol:kmcol + 1]),
                     reads=[bqn, bNKM], partial=[bqa])
                nk = len(keys)

                def issue_st(ki):
                    pS, bpS = PS[ki % 2]
                    j = keys[ki][0]
                    mm(pS[:, 0:n], kt_[:, 128 * j:128 * j + 128], qa[:, 0:n], True, True, [bk_, bqa], bpS)
                    return pS, bpS

                cur_st = issue_st(0)
                for ki, (j, mask) in enumerate(keys):
                    pS, bpS = cur_st
                    if ki + 1 < nk:
                        cur_st = issue_st(ki + 1)
                    pt_, bpt_ = PT.next()
                    act(pt_[:, 0:n], pS[:, 0:n], AF.Exp, [bpS], [bpt_], scale=0.125)
                    if mask is not None:
                        tt("pool", pt_[:, 0:n], pt_[:, 0:n], mask, ALU.mult, [bpt_, bCB], [bpt_])
                    for s in range(n // 128):
                        ao, ab = accs[s]
                        mmp(ao, pt_[:, 128 * s:128 * s + 128], VS[:, j, 0:dv + 1], ki == 0, ki == nk - 1, [bpt_, bVS], ab)

            def acc_views(dv, banks):
                res = []
                for s in range(4):
                    p, b = PS[banks[s // 2]]
                    res.append((p[:, (s % 2) * 256:(s % 2) * 256 + dv + 1], b))
                return res

            for ku in range(2):
                load_k(0, KA, "KA", ku)
                load_v(VA, "VA", 64 * ku, 64)
                for g in range(4):
                    hq = 4 * ku + g
                    for i in range(NT):
                        t0 = 128 * i
                        if i < NCT:
                            keys = [(j, None) for j in range(NCT)]
                        else:
                            keys = [(j, None) for j in range(NCT)]
                            if i > NCT:
                                keys.append((i - 1, MPREV))
                            keys.append((i, None))
                            if i < NT - 1:
                                keys.append((i + 1, MNEXT))
                        accs = acc_views(64, (2 + (i % 2) * 2, 3 + (i % 2) * 2))
                        pass
                        attn_unit(QA, QAN, "QA", "QAN", hq, 0, ku, t0, 128, keys, 64, accs)
                        qn, bqn = SM.next()
                        S.dma("sp", lambda e, qn=qn, t0=t0, hq=hq: e.dma_start(out=qn[:, 0:1], in_=QAN[hq, t0:t0 + 128].rearrange("(p o) -> p o", o=1)),
                              bqn.ds, reads=[dB["QAN"]], writes=[bqn])
                        es, bes = SM.next()
                        S.op("act", lambda e, es=es, qn=qn, ku=ku, hq=hq: e.activation(out=es[:, 0:1], in_=qn[:, 0:1], func=AF.Exp,
                                                                                        scale=NKM8[:, ku:ku + 1], bias=SK[:, hq:hq + 1]),
                             reads=[bqn, bNKM8, bSK], writes=[bes])
                        ao, ab = accs[0]
                        tt("dve", es[:, 1:2], es[:, 0:1], ao[:, 64:65], ALU.add, [bes, ab], [bes])
                        S.op("dve", lambda e, es=es: e.reciprocal(out=es[:, 2:3], in_=es[:, 1:2]), reads=[bes], writes=[bes])
                        o_, bo_ = WKB.next()
                        ts("dve", o_[:, 0:64], ao[:, 0:64], es[:, 2:3], None, ALU.mult, None, [ab, bes], [bo_])
                        stq("sp", OA[t0:t0 + 128, 64 * hq:64 * hq + 64], o_[:, 0:64], bo_, dB["OA"])

            for h in range(4):
                load_k(0, KB, "KB", 2 * h); load_k(1, KB, "KB", 2 * h + 1)
                load_v(VB, "VB", 128 * h, 128)
                for bi, (t0, n) in enumerate(tok_blocks):
                    keys = [(j, None) for j in range(NCT)] if t0 < L_ else [(j, None) for j in range(NT)]
                    O0, bO0 = OT.next()
                    for c in range(2):
                        accs = [(PS[2 + s_][0][:, 0:129], PS[2 + s_][1]) for s_ in range(4)]
                        attn_unit(QB, QBN, "QB", "QBN", 2 * h + c, c, 2 + 2 * h + c, t0, n, keys, 128, accs)
                        for s in range(n // 128):
                            a_, b_ = accs[s]
                            r, br = SM.next()
                            S.op("dve", lambda e, r=r, a_=a_: e.reciprocal(out=r[:, 0:1], in_=a_[:, 128:129]), reads=[b_], writes=[br])
                            if c == 0:
                                ts("dve", O0[:, 128 * s:128 * s + 128], a_[:, 0:128], r[:, 0:1], None, ALU.mult, None, [b_, br], (), pr=[bO0])
                            else:
                                tt("dve", r[:, 1:2], r[:, 0:1], NEGLAM, ALU.mult, [br, bLV], [br])
                                o0, bo0 = OT2.next()
                                stt(o0[:, 128:256], a_[:, 0:128], r[:, 1:2], O0[:, 128 * s:128 * s + 128], ALU.mult, ALU.add, [b_, br, bO0], [bo0])
                                ss, bss = SM.next()
                                act(o0[:, 256:384], o0[:, 128:256], AF.Square, [bo0], [bo0, bss], accum_out=ss[:, 0:1])
                                rs, brs = rstd_from_ss(ss[:, 0:1], bss, 128)
                                ob_, bob = WKB.next()
                                stt(ob_[:, 0:128], o0[:, 128:256], rs, DNW[:], ALU.mult, ALU.mult, [bo0, brs, bDNW], [bob])
                                tt0 = t0 + 128 * s
                                stq("sp", OB[tt0:tt0 + 128, 128 * h:128 * h + 128], ob_[:, 0:128], bob, dB["OB"])

            ph = new_phase()
            SF = [SB("sf%d" % h, [128, 128], F32, ph) for h in range(4)]
            SBF = [Pool("sbf%d_" % h, [128, 128], BF16, 2, ph) for h in range(4)]
            GP = Pool("gp", [128, 512], F32, 2, ph)
            QTp = Pool("qtp", [128, 512], BF16, 2, ph)
            KTp = Pool("ktp", [128, 512], BF16, 2, ph)
            KKp = Pool("kkp", [128, 512], BF16, 2, ph)
            VVp = Pool("vvp", [128, 512], BF16, 2, ph)
            E1p = Pool("e1p", [128, 512], F32, 2, ph)
            E2p = Pool("e2p", [128, 512], F32, 2, ph)
            QSp = Pool("qsp", [128, 512], BF16, 2, ph)
            KSp = Pool("ksp", [128, 512], BF16, 2, ph)
            KHp = Pool("khp", [128, 512], BF16, 2, ph)
            KHCp = Pool("khcp", [128, 4, 512], BF16, 2, ph)
            QSCp = Pool("qscp", [128, 4, 128], BF16, 8, ph)
            ATp = Pool("atp", [128, 128], BF16, 4, ph)
            OOp = Pool("oop", [128, 512], F32, 2, ph)
            for d_ in range(2):
                Um_f = UF_f if d_ == 0 else UB_f
                Dm_f = DF_f if d_ == 0 else DB_f
                Um_b = UF_b if d_ == 0 else UB_b
                if d_ == 0:
                    order = list(range(NT))
                else:
                    order = list(range(NCT - 1, -1, -1)) + list(range(NT - 1, NCT - 1, -1))
                corder = [0, 1, 2, 3] if d_ == 0 else [3, 2, 1, 0]
                cur = []
                for h in range(4):
                    memset("pool", SF[h][0][:], 0.0, [SF[h][1]])
                    sb_, bsb_ = SBF[h].next()
                    memset("pool", sb_[:], 0.0, [bsb_])
                    cur.append((sb_, bsb_))
                for i in order:
                    t0 = 128 * i
                    g, bg = GP.next(); ld("sp", g[:], HG[t0:t0 + 128, 512 * d_:512 * d_ + 512], bg, [dB["HG"]])
                    qT, bqT = QTp.next(); ld("sp", qT[:].rearrange("p (h t) -> p h t", h=4), HQ[:, :, t0:t0 + 128].rearrange("h p t -> p h t"), bqT, [dB["HQ"]])
                    kT, bkT = KTp.next(); ld("sp", kT[:].rearrange("p (h t) -> p h t", h=4), HK[4 * d_:4 * d_ + 4, :, t0:t0 + 128].rearrange("h p t -> p h t"), bkT, [dB["HK"]])
                    kk, bkk = KKp.next(); ld("sp", kk[:], HKt[t0:t0 + 128, 512 * d_:512 * d_ + 512], bkk, [dB["HKt"]])
                    vv, bvv = VVp.next(); ld("sp", vv[:], HV[t0:t0 + 128, :], bvv, [dB["HV"]])
                    pB, bpB = PS[0]
                    for h in range(4):
                        mmp(pB[:, 128 * h:128 * h + 128], g[:, 128 * h:128 * h + 128], Um_f, True, True, [bg, bCF], bpB) if h else \
                            mm(pB[:, 0:128], g[:, 0:128], Um_f, True, True, [bg, bCF], bpB)
                    e1, be1 = E1p.next(); act(e1[:], pB[:], AF.Exp, [bpB], [be1])
                    e2, be2 = E2p.next(); act(e2[:], pB[:], AF.Exp, [bpB], [be2], scale=-1.0)
                    qs, bqs = QSp.next(); tt("dve", qs[:], qT[:], e1[:], ALU.mult, [bqT, be1], [bqs])
                    ks, bks = KSp.next(); tt("dve", ks[:], kT[:], e2[:], ALU.mult, [bkT, be2], [bks])
                    pD, bpD = PS[1]
                    mm(pD[:], Dm_f, g[:], True, True, [bCF, bg], bpD)
                    e3, be3 = E2p.next(); act(e3[:], pD[:], AF.Exp, [bpD], [be3])
                    kh, bkh = KHp.next(); tt("dve", kh[:], kk[:], e3[:], ALU.mult, [bkk, be3], [bkh])
                    khc, bkhc = KHCp.next()
                    for c in range(4):
                        ts("dve", khc[:, c, :], kh[:], RM[:, c:c + 1], None, ALU.mult, None, [bkh, bRM], (), pr=[bkhc])
                    pOs = [PS[4 + h] for h in range(4)]
                    qscs = []
                    for h in range(4):
                        hs = slice(128 * h, 128 * h + 128)
                        qsc, bqsc = QSCp.next()
                        for c in range(4):
                            tt("pool", qsc[:, c, :], qs[:, hs], BDC[:, c, :], ALU.mult, [bqs, bCB], (), pr=[bqsc])
                        pA, bpA = PS[2 + (h % 2)]
                        mm(pA[:, 0:128], ks[:, hs], qs[:, hs], True, True, [bks, bqs], bpA)
                        at_, bat = ATp.next()
                        tt("dve", at_[:], pA[:, 0:128], Um_b, ALU.mult, [bpA, bCB], [bat])
                        pO, bpO = pOs[h]
                        mm(pO[:, 0:128], at_[:], vv[:, hs], True, False, [bat, bvv], bpO)
                        qscs.append((qsc, bqsc))
                    for ci, c in enumerate(corder):
                        tl = 32 * c + 31 if d_ == 0 else 32 * c
                        for h in range(4):
                            hs = slice(128 * h, 128 * h + 128)
                            pO, bpO = pOs[h]
                            sf, bsf = SF[h]
                            sb_, bsb_ = cur[h]
                            qsc, bqsc = qscs[h]
                            mmp(pO[:, 0:128], qsc[:, c, :], sb_[:], False, ci == 3, [bqsc, bsb_], bpO)
                            pU, bpU = PS[2 + (h % 2)]
                            mm(pU[:, 0:128], khc[:, c, hs], vv[:, hs], True, True, [bkhc, bvv], bpU)
                            stt(sf[:], sf[:], e1[:, 128 * h + tl:128 * h + tl + 1], pU[:, 0:128], ALU.mult, ALU.add, [bsf, be1, bpU], [bsf])
                            nb_, bnb_ = SBF[h].next()
                            cp("act", nb_[:], sf[:], [bsf], [bnb_])
                            cur[h] = (nb_, bnb_)
                    oo, boo = OOp.next()
                    if d_ == 0:
                        for h in range(4):
                            hs = slice(128 * h, 128 * h + 128)
                            cp("act", oo[:, hs], pOs[h][0][:, 0:128], [pOs[h][1]], (), pr=[boo])
                        stq("sp", OF[t0:t0 + 128, :], oo[:], boo, dB["OF"])
                    else:
                        of_, bof = GP.next()
                        ld("sp", of_[:], OF[t0:t0 + 128, :], bof, [dB["OF"]])
                        for h in range(4):
                            hs = slice(128 * h, 128 * h + 128)
                            tt("dve", oo[:, hs], pOs[h][0][:, 0:128], of_[:, hs], ALU.add, [pOs[h][1], bof], (), pr=[boo])
                        gt_, bgt = WKB.next()
                        ld("sp", gt_[:, 0:512], HGT[t0:t0 + 128, :], bgt, [dB["HGT"]])
                        ss, bss = SM.next()
                        junk, bj = WK4.next()
                        for h in range(4):
                            hs = slice(128 * h, 128 * h + 128)
                            S.op("act", lambda e, junk=junk, oo=oo, ss=ss, hs=hs, h=h: e.activation(out=junk[:, hs], in_=oo[:, hs], func=AF.Square, accum_out=ss[:, h:h + 1]),
                                 reads=[boo], partial=[bj, bss])
                        t, b = SM.next()
                        S.op("act", lambda e, t=t, ss=ss: e.activation(out=t[:, 0:4], in_=ss[:, 0:4], func=AF.Sqrt, bias=eps_t[:, 0:1], scale=1.0 / 128),
                             reads=[bss, beps], writes=[b])
                        S.op("dve", lambda e, t=t: e.reciprocal(out=t[:, 4:8], in_=t[:, 0:4]), reads=[b], writes=[b])
                        oc, boc = WKB.next()
                        for h in range(4):
                            hs = slice(128 * h, 128 * h + 128)
                            stt(junk[:, hs], oo[:, hs], t[:, 4 + h:5 + h], HNW[:], ALU.mult, ALU.mult, [boo, b, bHNW], (), pr=[bj])
                        tt("dve", oc[:, 0:512], junk[:, 0:512], gt_[:, 0:512], ALU.mult, [bj, bgt], [boc])
                        stq("sp", OC[t0:t0 + 128, :], oc[:, 0:512], boc, dB["OC"])

            ph = new_phase()
            WBR, bWBR = SB("WBR", [128, 12, D], BF16, ph)
            ld("pool", WBR[:], w_br[l].rearrange("i (k p) n -> p (i k) n", p=128), bWBR)
            WO, bWO = SB("WO", [128, 8, D], BF16, ph)
            ld("pool", WO[:], w_out[l].rearrange("(k p) n -> p k n", p=128), bWO)
            WR, bWR = SB("WR", [128, 8, NE], F32, ph)
            ld("sp", WR[:], w_router[l].rearrange("(k p) n -> p k n", p=128), bWR)
            BR, bBR = SB("BR", [128, NE], F32, ph)
            ld("sp", BR[:], b_router[l].partition_broadcast(128), bBR)
            memset("pool", RS_[:], 0.0, [bRS])
            OTp = Pool("otp", [128, 12, 128], BF16, 2, ph)
            GTp = Pool("gtp", [128, 3072], BF16, 2, ph)
            H2Tp = Pool("h2t", [128, 8, 128], F32, 2, ph)
            SMR = Pool("smr", [128, 4, NE], F32, 3, ph)
            zt, bzt = WKB.next()
            memset("pool", zt[:], 0.0, [bzt])
            for r0 in range(0, NSLOT, 128):
                stq("sp", XBUF[r0:r0 + 128, :], zt[:], bzt, dB["XBUF"])
            for i in range(NT):
                t0 = 128 * i
                ms, bms = (MODC, bMODC) if i < NCT else (MODL, bMODL)
                ob3, bob3 = WKB.next(), None
                oin, boin = ob3
                oin2, boin2 = WKB.next()
                ld("sp", oin[:, 0:512], OA[t0:t0 + 128, :], boin, [dB["OA"]])
                ld("sp", oin[:, 512:1024], OB[t0:t0 + 128, :], boin, [dB["OB"]])
                ld("sp", oin2[:, 0:512], OC[t0:t0 + 128, :], boin2, [dB["OC"]])
                gts, bgts = GTp.next()
                ld("sp", gts[:], GT[t0:t0 + 128, :], bgts, [dB["GT"]])
                pt, bpt = PS[7]
                ptb = pt[:].bitcast(BF16)
                oT, boT = OTp.next()
                for kc in range(8):
                    tr(ptb[:, kc * 128:(kc + 1) * 128], oin[:, kc * 128:(kc + 1) * 128], ID_b, [boin, bCB], bpt, kc == 0)
                cp("act", oT[:, 0:8, :], ptb.rearrange("p (k t) -> p k t", k=8), [bpt], [boT])
                for kc in range(4):
                    tr(ptb[:, kc * 128:(kc + 1) * 128], oin2[:, kc * 128:(kc + 1) * 128], ID_b, [boin2, bCB], bpt, kc == 0)
                cp("act", oT[:, 8:12, :], ptb[:, 0:512].rearrange("p (k t) -> p k t", k=4), [bpt], (), pr=[boT])
                y, by = WK4.next()
                tmpy, btmpy = WK4.next()
                for br_ in range(3):
                    for cb in range(2):
                        pp, bpp = PS[cb]
                        for kc in range(4):
                            mm(pp[:], oT[:, 4 * br_ + kc, :], WBR[:, 4 * br_ + kc, 512 * cb:512 * cb + 512], kc == 0, kc == 3, [boT, bWBR], bpp)
                        gsl = gts[:, D * br_ + 512 * cb:D * br_ + 512 * cb + 512]
                        if br_ == 0:
                            tt("dve", y[:, 512 * cb:512 * cb + 512], pp[:], gsl, ALU.mult, [bpp, bgts], (), pr=[by])
                        else:
                            tt("dve", tmpy[:, 512 * cb:512 * cb + 512], pp[:], gsl, ALU.mult, [bpp, bgts], (), pr=[btmpy])
                            tt("pool", y[:, 512 * cb:512 * cb + 512], y[:, 512 * cb:512 * cb + 512], tmpy[:, 512 * cb:512 * cb + 512], ALU.add, [btmpy, by], [by])
                yb, byb = WKB.next()
                cp("act", yb[:], y[:], [by], [byb])
                for kc in range(8):
                    tr(ptb[:, kc * 128:(kc + 1) * 128], yb[:, kc * 128:(kc + 1) * 128], ID_b, [byb, bCB], bpt, kc == 0)
                yT, byT = WKB.next()
                cp("act", yT[:], ptb, [bpt], [byT])
                ss, bss = SM.next()
                junk, bj = WK4.next()
                pps = []
                for cb in range(2):
                    pp, bpp = PS[2 + cb]
                    for kc in range(8):
                        mm(pp[:], yT[:, kc * 128:(kc + 1) * 128], WO[:, kc, 512 * cb:512 * cb + 512], kc == 0, kc == 7, [byT, bWO], bpp)
                    S.op("act", lambda e, junk=junk, pp=pp, ss=ss, cb=cb: e.activation(out=junk[:, 512 * cb:512 * cb + 512], in_=pp[:], func=AF.Square, accum_out=ss[:, cb:cb + 1]),
                         reads=[bpp], partial=[bj, bss])
                    pps.append((pp, bpp))
                tt("dve", ss[:, 2:3], ss[:, 0:1], ss[:, 1:2], ALU.add, [bss], [bss])
                rs, brs = rstd_from_ss(ss[:, 2:3], bss, D)
                xt, bx = WK4.next()
                ld("sp", xt[:], XR[t0:t0 + 128, :], bx, [dB["XR"]])
                for cb in range(2):
                    pp, bpp = pps[cb]
                    stt(junk[:, 512 * cb:512 * cb + 512], pp[:], rs, ms[:, 2, 512 * cb:512 * cb + 512], ALU.mult, ALU.mult, [bpp, brs, bms], (), pr=[bj])
                tt("dve", xt[:], xt[:], junk[:], ALU.add, [bx, bj], [bx])
                stq("sp", XR[t0:t0 + 128, :], xt[:], bx, dB["XR"])
                h2f, bh2f = WK4.next()
                hb2, bhb2 = norm_mod_T(xt, bx, ms, bms, 4, 3, t0, keep=(h2f, bh2f))
                h2T, bh2T = H2Tp.next()
                for half in range(2):
                    pr0, bpr0 = PS[4 + half]
                    for kc in range(4):
                        k2 = 4 * half + kc
                        tr(pr0[:, kc * 128:(kc + 1) * 128], h2f[:, k2 * 128:(k2 + 1) * 128], ID_f, [bh2f, bCF], bpr0, kc == 0)
                    cp("act", h2T[:, 4 * half:4 * half + 4, :], pr0[:].rearrange("p (k t) -> p k t", k=4), [bpr0], (), pr=[bh2T])
                pl, bpl = PS[6]
                for kc in range(8):
                    mm(pl[:, 0:NE], h2T[:, kc, :], WR[:, kc, :], kc == 0, kc == 7, [bh2T, bWR], bpl)
                sm, bsm = SMR.next()
                Lg = sm[:, 0, :]; Mk = sm[:, 1, :]; Pb = sm[:, 2, :]; Oh = sm[:, 3, :]
                tt("dve", Lg, pl[:, 0:NE], BR[:], ALU.add, [bpl, bBR], [bsm])
                t8, bt8 = SM.next()
                S.op("dve", lambda e, t8=t8, Lg=Lg: e.max(out=t8[:, 0:8], in_=Lg), reads=[bsm], writes=[bt8])
                ts("dve", t8[:, 8:9], t8[:, 0:1], -1.0, None, ALU.mult, None, [bt8], [bt8])
                S.op("act", lambda e, t8=t8: e.activation(out=t8[:, 9:13], in_=t8[:, 0:4], func=AF.Exp, bias=t8[:, 8:9], accum_out=t8[:, 13:14]),
                     reads=[bt8], writes=[bt8])
                S.op("dve", lambda e, t8=t8: e.reciprocal(out=t8[:, 14:15], in_=t8[:, 13:14]), reads=[bt8], writes=[bt8])
                ts("dve", G4[:, i, :], t8[:, 9:13], t8[:, 14:15], None, ALU.mult, None, [bt8], (), pr=[bG4])
                ts("dve", Mk, Lg, t8[:, 3:4], None, ALU.is_ge, None, [bsm, bt8], [bsm])
                pq, bpq = PS[6]
                mm(pq[:, 64:64 + NE], UTS_f, Mk, True, False, [bCF, bsm], bpq)
                mm(pq[:, 64:64 + NE], ONES_f, RS_[:], False, True, [bCF, bRS], bpq)
                tt("dve", Pb, pq[:, 64:64 + NE], EBASE[:, 0:NE], ALU.add, [bpq, bCF], [bsm])
                tt("pool", RS_[:], RS_[:], Mk, ALU.add, [bRS, bsm], [bRS])
                df, bdf = SM.next()
                for k in range(4):
                    ts("dve", Oh, Lg, t8[:, k:k + 1], None, ALU.is_equal, None, [bsm, bt8], [bsm])
                    tt("dve", Oh, Oh, Pb, ALU.mult, [bsm], [bsm])
                    S.op("dve", lambda e, df=df, Oh=Oh, k=k: e.reduce_sum(out=df[:, k:k + 1], in_=Oh, axis=AX.X), reads=[bsm], partial=[bdf])
                ts("dve", df[:, 0:4], df[:, 0:4], float(NSLOT - 1), None, ALU.min, None, [bdf], [bdf])
                cp("dve", DEST[:, i, :], df[:, 0:4], [bdf], (), pr=[bDEST])
                for k in range(4):
                    S.dma("pool", lambda e, i=i, k=k, hb2=hb2: e.indirect_dma_start(
                        out=XBUF, out_offset=bass.IndirectOffsetOnAxis(ap=DEST[:, i, k:k + 1], axis=0),
                        in_=hb2[:, :], in_offset=None),
                        bhb2.ds, reads=[bhb2, bDEST], writes=[dB["XBUF"]] if (i == 0 and k == 0) else (), partial=() if (i == 0 and k == 0) else [dB["XBUF"]])

            ph = new_phase()
            WGU = Pool("wgu", [128, 8, 2 * D], BF16, 1, ph)
            WDN = Pool("wdn", [128, 8, D], BF16, 1, ph)
            BGU = Pool("bgu", [128, 16], F32, 2, ph)
            BDN = Pool("bdn", [128, D], F32, 1, ph)
            XSp = Pool("xsp", [128, D], BF16, 3, ph)
            XTp = Pool("xtp", [128, 8, 512], BF16, 2, ph)
            ATT = Pool("att", [128, 8, 512], BF16, 1, ph)
            GCp = Pool("gcp", [128, 512], F32, 2, ph)
            SGp = Pool("sgp", [128, 512], F32, 2, ph)
            UCp = Pool("ucp", [128, 512], F32, 2, ph)
            YTp = Pool("ytp", [128, D], BF16, 2, ph)
            for e_ in range(NE):
                wg, bwg = WGU.next(); wd, bwd = WDN.next(); bg_, bbg = BGU.next(); bd_, bbd = BDN.next()
                for kc in range(8):
                    S.dma("pool", lambda e, wg=wg, kc=kc, e_=e_: e.dma_start(out=wg[:, kc, :], in_=w_gu[l, e_, kc * 128:(kc + 1) * 128, :]),
                          bwg.ds, writes=[bwg] if kc == 0 else (), partial=() if kc == 0 else [bwg])
                ld("pool", wd[:], w_dn[l, e_].rearrange("(k p) n -> p k n", p=128), bwd)
                ld("sp", bg_[:], b_gu_t[l, e_], bbg)
                ld("sp", bd_[:], b_dn[l, e_].partition_broadcast(128), bbd)
                for blk in range(CAP // 512):
                    r0 = e_ * CAP + 512 * blk
                    xT, bxT = XTp.next()
                    for s in range(4):
                        xs, bxs = XSp.next()
                        ld("sp", xs[:], XBUF[r0 + 128 * s:r0 + 128 * s + 128, :], bxs, [dB["XBUF"]])
                        pt, bpt = PS[6 + (s % 2)]
                        ptb = pt[:].bitcast(BF16)
                        for kc in range(8):
                            tr(ptb[:, kc * 128:(kc + 1) * 128], xs[:, kc * 128:(kc + 1) * 128], ID_b, [bxs, bCB], bpt, kc == 0)
                        cp("act", xT[:, :, 128 * s:128 * s + 128], ptb.rearrange("p (k t) -> p k t", k=8), [bpt], (), pr=[bxT])
                    aT, baT = ATT.next()
                    for f in range(8):
                        pg, bpg = PS[0 + (f % 2) * 2]
                        pu, bpu = PS[1 + (f % 2) * 2]
                        for kc in range(8):
                            mm(pg[:], wg[:, kc, 128 * f:128 * f + 128], xT[:, kc, :], kc == 0, kc == 7, [bwg, bxT], bpg)
                        for kc in range(8):
                            mm(pu[:], wg[:, kc, D + 128 * f:D + 128 * f + 128], xT[:, kc, :], kc == 0, kc == 7, [bwg, bxT], bpu)
                        gc, bgc = GCp.next(); sg, bsg = SGp.next(); uc, buc = UCp.next()
                        ts("dve", gc[:], pg[:], bg_[:, f:f + 1], 7.0, ALU.add, ALU.min, [bpg, bbg], [bgc])
                        act(sg[:], gc[:], AF.Silu, [bgc], [bsg], scale=1.702)
                        ts("dve", uc[:], pu[:], bg_[:, 8 + f:9 + f], 7.0, ALU.add, ALU.min, [bpu, bbg], [buc])
                        ts("dve", uc[:], uc[:], -7.0, 1.0, ALU.max, ALU.add, [buc], [buc])
                        stt(aT[:, f, :], uc[:], 1.0 / 1.702, sg[:], ALU.mult, ALU.mult, [buc, bsg], (), pr=[baT])
                    for s in range(4):
                        yt, byt = YTp.next()
                        for cb in range(2):
                            pp, bpp = PS[4 + cb]
                            for f in range(8):
                                mm(pp[:], aT[:, f, 128 * s:128 * s + 128], wd[:, f, 512 * cb:512 * cb + 512], f == 0, f == 7, [baT, bwd], bpp)
                            tt("dve", yt[:, 512 * cb:512 * cb + 512], pp[:], bd_[:, 512 * cb:512 * cb + 512], ALU.add, [bpp, bbd], (), pr=[byt])
                        stq("sp", YBUF[r0 + 128 * s:r0 + 128 * s + 128, :], yt[:], byt, dB["YBUF"])

            ph = new_phase()
            YG = Pool("yg", [128, 4, D], BF16, 2, ph)
            for i in range(NT):
                t0 = 128 * i
                ms, bms = (MODC, bMODC) if i < NCT else (MODL, bMODL)
                yg, byg = YG.next()
                for k in range(4):
                    S.dma("pool", lambda e, yg=yg, i=i, k=k: e.indirect_dma_start(
                        out=yg[:, k, :], out_offset=None, in_=YBUF,
                        in_offset=bass.IndirectOffsetOnAxis(ap=DEST[:, i, k:k + 1], axis=0)),
                        byg.ds, reads=[dB["YBUF"], bDEST], writes=[byg] if k == 0 else (), partial=() if k == 0 else [byg])
                f_, bf_ = WK4.next()
                ts("dve", f_[:], yg[:, 0, :], G4[:, i, 0:1], None, ALU.mult, None, [byg, bG4], [bf_])
                for k in range(1, 4):
                    stt(f_[:], yg[:, k, :], G4[:, i, k:k + 1], f_[:], ALU.mult, ALU.add, [byg, bG4, bf_], [bf_])
                junk, bj = WK4.next()
                ss, bss = SM.next()
                act(junk[:], f_[:], AF.Square, [bf_], [bj, bss], accum_out=ss[:, 0:1])
                rs, brs = rstd_from_ss(ss[:, 0:1], bss, D)
                xt, bx = WK4.next()
                ld("sp", xt[:], XR[t0:t0 + 128, :], bx, [dB["XR"]])
                stt(junk[:], f_[:], rs, ms[:, 5, :], ALU.mult, ALU.mult, [bf_, brs, bms], [bj])
                tt("dve", xt[:], xt[:], junk[:], ALU.add, [bx, bj], [bx])
                if l == nlayers - 1 and i >= NCT:
                    stq("sp", out[t0 - L_:t0 - L_ + 128, :], xt[:], bx, dB["out"])
                else:
                    stq("sp", XR[t0:t0 + 128, :], xt[:], bx, dB["XR"])

            S.barrier(list(DS.values())); S.emit(); phase[0].close(); phase[0] = None
            lay.close()
        S.wait_all("sp", list(dB.values()))
        S.emit()
        print("sems used:", S.nsem, "instr:", S.ninstr)
    return nc


def make_consts(S_, L_):
    T = S_ + L_
    GRID_W = 64
    rows = S_ // GRID_W
    row = np.repeat(np.arange(rows, dtype=np.float32), GRID_W)
    col = np.tile(np.arange(GRID_W, dtype=np.float32), rows)
    inv = (10000.0 ** (-np.arange(16, dtype=np.float32) / 16)).astype(np.float32)
    ang_r = row[:, None] * inv
    ang_c = col[:, None] * inv
    cos64 = np.ones((64, T), np.float32)
    sin64 = np.zeros((64, T), np.float32)
    for d in range(64):
        half, j = d // 32, d % 32
        n = j % 16
        ang = (ang_r if half == 0 else ang_c)[:, n]
        cos64[d, L_:] = np.cos(ang)
        sin64[d, L_:] = (-np.sin(ang)) if j < 16 else np.sin(ang)
    ropc = np.concatenate([cos64, cos64], 0)
    rops = np.concatenate([sin64, sin64], 0)
    s = np.arange(128)[:, None]
    t = np.arange(128)[None, :]
    same = (s // 32) == (t // 32)
    UF = (same & (s <= t)).astype(np.float32)
    UB = (same & (s >= t)).astype(np.float32)
    BD = same.astype(np.float32)
    cst_f = np.zeros((128, 8, 128), np.float32)
    cst_f[:, 0] = UF; cst_f[:, 1] = UB; cst_f[:, 2] = BD - UF; cst_f[:, 3] = BD - UB
    cst_f[:, 4] = (s < t).astype(np.float32)
    cst_f[:, 5] = 1.0
    cst_f[:, 6] = np.eye(128, dtype=np.float32)
    cst_f[:, 7, 0:NE] = (np.arange(NE) * CAP).astype(np.float32)[None, :]
    cst_b = np.zeros((128, 12, 128), np.float32)
    cst_b[:, 0] = np.eye(128)
    cst_b[:, 1] = (s >= t)
    cst_b[:, 2] = (s <= t)
    cst_b[:, 3] = UF; cst_b[:, 4] = UB
    for c in range(4):
        cst_b[:, 5 + c] = ((t // 32) == c).astype(np.float32) * np.ones((128, 1), np.float32)
    cst_b[0:64, 9, 0] = 1.0
    cst_b[64:128, 9, 1] = 1.0
    rowmask = np.zeros((128, 4), np.float32)
    for c in range(4):
        rowmask[32 * c:32 * c + 32, c] = 1.0
    return dict(ropc=ropc, rops=rops, cst_f=cst_f, cst_b=cst_b.astype(ml_dtypes.bfloat16), rowmask=rowmask)


def rope_perm_cols():
    cols = []
    for (c0, w) in ((C_AQ, 512), (C_AK, 128), (C_BQ, 512), (C_BK, 512)):
        for m in range(w):
            blk, j = m // 32, m % 32
            cols.append(c0 + blk * 32 + (j + 16) % 32)
    return np.array(cols, dtype=np.int64)


def make_in_maps(inputs, S_, L_, n_cores=8):
    f = lambda a: np.ascontiguousarray(np.asarray(a, dtype=np.float32))
    x = f(inputs["x"]); c = f(inputs["c"]); ctx = f(inputs["ctx"]); c_ctx = f(inputs["c_ctx"])
    w_in = f(inputs["w_in"])
    consts = make_consts(S_, L_)
    shared = dict(
        cc_col=np.ascontiguousarray(c_ctx.reshape(8, 128).T),
        w_mod=f(inputs["w_mod"]), b_mod=f(inputs["b_mod"]), norm_g=f(inputs["norm_g"]).reshape(2, 4 * D),
        w_in=w_in, w_inp=np.ascontiguousarray(w_in[:, :, rope_perm_cols()]),
        sink=f(inputs["attn_sink"]), dlam=f(inputs["diff_lambda"]).reshape(2, 256), dnw=f(inputs["diff_norm_w"]),
        lbl=f(inputs["hgrn_lb_logits"]).reshape(2, 1024),
        lbl_fm=np.ascontiguousarray(f(inputs["hgrn_lb_logits"]).reshape(2, 8, 128).transpose(0, 2, 1)),
        hnw=f(inputs["hgrn_norm_w"]), w_br=f(inputs["w_branch"]), w_out=f(inputs["w_out"]),
        w_router=f(inputs["w_router"]), b_router=f(inputs["b_router"]),
        w_gu=f(inputs["w_gate_up"]),
        b_gu_t=np.ascontiguousarray(f(inputs["b_gate_up"]).reshape(2, NE, 16, 128).transpose(0, 1, 3, 2)),
        w_dn=f(inputs["w_down"]), b_dn=f(inputs["b_down"]),
    )
    shared.update(consts)
    maps = []
    B = x.shape[0]
    for core in range(n_cores):
        b = core * B // n_cores
        m = dict(shared)
        m["xin"] = x[b]
        m["ctxin"] = ctx[b]
        m["c_col"] = np.ascontiguousarray(c[b].reshape(8, 128).T)
        maps.append(m)
    return maps


def kernel(**inputs):
    x = np.asarray(inputs["x"])
    B, S_, _ = x.shape
    L_ = np.asarray(inputs["ctx"]).shape[1]
    nc = bass.Bass("TRN2", target_bir_lowering=False)
    build(nc, S_, L_)
    maps = make_in_maps(inputs, S_, L_)
    res = run_bass_kernel_spmd(nc, maps, core_ids=list(range(8)))
    outs = [np.asarray(res.results[(b * 8) // B]["out"]) for b in range(B)]
    return np.stack(outs, 0).astype(np.float32)
```

```python
import contextlib
import numpy as np
import ml_dtypes
import concourse.bass as bass
import concourse.mybir as mybir
from concourse.bass_utils import run_bass_kernel_spmd

F32 = mybir.dt.float32
BF16 = mybir.dt.bfloat16
I32 = mybir.dt.int32
AF = mybir.ActivationFunctionType
ALU = mybir.AluOpType
AX = mybir.AxisListType

EPOCH = 30000
DEPOCH = 1800
COMPUTE = ("pe", "act", "dve", "pool")
ALLENG = ("pe", "act", "dve", "pool", "sp")

D = 1024
NE = 32
CAP = 3072
CAP1 = 1024
RMS_EPS = 1e-6


class DSem:
    __slots__ = ("name", "ep", "cnt")

    def __init__(self, name):
        self.name = name
        self.ep = 0
        self.cnt = 0


class Buf:
    __slots__ = ("name", "w", "r", "ds")

    def __init__(self, name, ds=None):
        self.name = name
        self.w = {}
        self.r = {}
        self.ds = ds


class Sched:
    def __init__(self, nc, stack):
        self.nc = nc
        self.stack = stack
        self.streams = {e: [] for e in ALLENG}
        self.cnt = {e: 0 for e in COMPUTE}
        self.waited = {e: {} for e in ALLENG}
        self.sems = {}
        self.nsem = 0

    def sem(self, key):
        s = self.sems.get(key)
        if s is None:
            s = self.stack.enter_context(self.nc.semaphore("s%d" % self.nsem))
            self.nsem += 1
            self.sems[key] = s
        return s

    def _wait(self, eng, key, val):
        if val <= 0:
            return
        if eng == "pe" and key[0] == "pe":
            return
        w = self.waited[eng]
        if w.get(key, 0) >= val:
            return
        w[key] = val
        self.streams[eng].append(("w", key, val))

    def _deps(self, eng, reads, writes, partial):
        for b in reads:
            for k, v in b.w.items():
                self._wait(eng, k, v)
        for b in writes:
            for k, v in b.w.items():
                self._wait(eng, k, v)
            for k, v in b.r.items():
                self._wait(eng, k, v)
        for b in partial:
            for k, v in b.r.items():
                self._wait(eng, k, v)

    def _record(self, tok, reads, writes, partial):
        k, v = tok
        for b in reads:
            if b.r.get(k, 0) < v:
                b.r[k] = v
        for b in writes:
            b.w = {k: v}
            b.r = {}
        for b in partial:
            if b.w.get(k, 0) < v:
                b.w[k] = v

    def op(self, eng, fn, reads=(), writes=(), partial=()):
        self._deps(eng, reads, writes, partial)
        n = self.cnt[eng]
        self.cnt[eng] = n + 1
        key = (eng, n // EPOCH)
        val = n % EPOCH + 1
        self.sem(key)
        self.streams[eng].append(("o", fn, key, 1))
        self._record((key, val), reads, writes, partial)

    def dma(self, eng, fn, ds, reads=(), writes=(), partial=()):
        self._deps(eng, reads, writes, partial)
        key = ("d", ds.name, ds.ep)
        self._wait(eng, key, ds.cnt)
        if ds.cnt >= 16 * DEPOCH:
            ds.ep += 1
            ds.cnt = 0
            key = ("d", ds.name, ds.ep)
        self.sem(key)
        ds.cnt += 16
        self.streams[eng].append(("o", fn, key, 16))
        self._record((key, ds.cnt), reads, writes, partial)

    def barrier(self, dsems):
        toks = []
        for eng in COMPUTE:
            c = self.cnt[eng]
            if c > 0:
                toks.append(((eng, (c - 1) // EPOCH), (c - 1) % EPOCH + 1))
        for ds in dsems:
            if ds.cnt > 0:
                toks.append((("d", ds.name, ds.ep), ds.cnt))
        for e in ALLENG:
            for k, v in toks:
                self._wait(e, k, v)

    def wait_all(self, eng, bufs):
        for b in bufs:
            for k, v in b.w.items():
                self._wait(eng, k, v)
            for k, v in b.r.items():
                self._wait(eng, k, v)

    def emit(self):
        nc = self.nc
        engmap = {"pe": "tensor", "act": "scalar", "dve": "vector", "pool": "gpsimd", "sp": "sync"}
        sems = self.sems
        streams = self.streams
        with nc.Block() as block:
            def mk(ename):
                def body(e):
                    for item in streams[ename]:
                        if item[0] == "w":
                            e.wait_ge(sems[item[1]], item[2])
                        else:
                            item[1](e).then_inc(sems[item[2]], item[3])
                return body
            for ename in ALLENG:
                if streams[ename]:
                    getattr(block, engmap[ename])(mk(ename))
        self.ninstr = getattr(self, "ninstr", 0) + sum(len(v) for v in streams.values())
        self.streams = {e: [] for e in ALLENG}


C_AQ, C_AK, C_AV, C_BQ, C_BK, C_BV, C_CQ, C_CI, C_FF, C_FB, C_CG, C_GT = (
    0, 512, 640, 768, 1280, 1792, 2304, 2816, 3328, 3840, 4352, 4864)
IN_W = 7936
P_AQ, P_AK, P_BQ, P_BK = 0, 512, 640, 1152
ROPE_W = 1664


def build(nc, S_, L_, dbg=(), nlayers=2):
    T = S_ + L_
    NT = T // 128
    NCT = L_ // 128
    NLT = S_ // 128
    tok_blocks = [(0, L_)] + [(L_ + 512 * i, 512) for i in range(S_ // 512)]
    NSLOT = NE * CAP

    def din(name, shape, dt=F32):
        return nc.dram_tensor(name, list(shape), dt, kind="ExternalInput").ap()

    def dscr(name, shape, dt):
        kind = "ExternalOutput" if name in dbg else "Internal"
        return nc.dram_tensor(name, list(shape), dt, kind=kind).ap()

    xin = din("xin", [S_, D]); ctxin = din("ctxin", [L_, D])
    c_col = din("c_col", [128, 8]); cc_col = din("cc_col", [128, 8])
    w_mod = din("w_mod", [2, D, 6 * D]); b_mod = din("b_mod", [2, 6 * D]); norm_g = din("norm_g", [2, 4 * D])
    w_in = din("w_in", [2, D, IN_W]); w_inp = din("w_inp", [2, D, ROPE_W])
    sink_in = din("sink", [2, 8]); dlam = din("dlam", [2, 256]); dnw = din("dnw", [2, 128])
    lbl = din("lbl", [2, 1024]); lbl_fm = din("lbl_fm", [2, 128, 8]); hnw = din("hnw", [2, 128])
    w_br = din("w_br", [2, 3, 512, D]); w_out = din("w_out", [2, D, D])
    w_router = din("w_router", [2, D, NE]); b_router = din("b_router", [2, NE])
    w_gu = din("w_gu", [2, NE, D, 2 * D]); b_gu_t = din("b_gu_t", [2, NE, 128, 16])
    w_dn = din("w_dn", [2, NE, D, D]); b_dn = din("b_dn", [2, NE, D])
    ropc = din("ropc", [128, T]); rops = din("rops", [128, T])
    cst_f = din("cst_f", [128, 8, 128])
    cst_b = din("cst_b", [128, 12, 128], BF16)
    rowmask_in = din("rowmask", [128, 4])
    SQ = S_ // 4
    NQT = SQ // 128
    idxq_in = din("idxq", [128, NQT + 1], I32)
    out = nc.dram_tensor("out", [SQ, D], F32, kind="ExternalOutput").ap()
    XQ = dscr("XQ", [SQ, D], F32)

    XR = dscr("XR", [T, D], F32)
    HT = dscr("HT", [8, 128, T], BF16)
    QA = dscr("QA", [8, 64, T], BF16); QAN = dscr("QAN", [8, T], F32)
    KA = dscr("KA", [2, 64, T], BF16); KAN = dscr("KAN", [2, T], F32)
    VA = dscr("VA", [T, 128], BF16)
    QB = dscr("QB", [8, 64, T], BF16); QBN = dscr("QBN", [8, T], F32)
    KB = dscr("KB", [8, 64, T], BF16); KBN = dscr("KBN", [8, T], F32)
    VB = dscr("VB", [T, 512], BF16)
    KMX = dscr("KMX", [10, 1], F32)
    HQ = dscr("HQ", [4, 128, T], BF16); HK = dscr("HK", [8, 128, T], BF16)
    HKt = dscr("HKt", [T, 1024], BF16); HG = dscr("HG", [T, 1024], F32)
    HV = dscr("HV", [T, 512], BF16); HGT = dscr("HGT", [T, 512], BF16)
    GT = dscr("GT", [T, 3072], BF16)
    OA = dscr("OA", [T, 512], BF16); OB = dscr("OB", [T, 512], BF16); OC = dscr("OC", [T, 512], BF16)
    OF = dscr("OF", [T, 512], F32)
    XBUF = dscr("XBUF", [NSLOT, D], BF16); YBUF = dscr("YBUF", [NSLOT, D], BF16)

    with contextlib.ExitStack() as st:
        S = Sched(nc, st)
        DS = {}
        uniq = [0]

        def SB(name, shape, dt, stack=None):
            uniq[0] += 1
            t = (stack or st).enter_context(nc.sbuf_tensor("%s_%d" % (name, uniq[0]), list(shape), dt))
            ds = DS.get(name)
            if ds is None:
                ds = DS[name] = DSem(name)
            return t, Buf(name, ds)

        class Pool:
            def __init__(self, name, shape, dt, n, stack=None):
                self.items = [SB("%s%d" % (name, i), shape, dt, stack) for i in range(n)]
                self.i = 0

            def next(self):
                it = self.items[self.i % len(self.items)]
                self.i += 1
                return it

        dB = {n: Buf(n) for n in ["XR", "HT", "QA", "QAN", "KA", "KAN", "VA", "QB", "QBN", "KB", "KBN", "VB", "KMX",
                                  "HQ", "HK", "HKt", "HG", "HV", "HGT", "GT", "OA", "OB", "OC", "OF", "XBUF", "YBUF", "out", "XQ"]}

        def ld(eng, dst, src, dbuf, rd=()):
            S.dma(eng, lambda e: e.dma_start(out=dst, in_=src), dbuf.ds, reads=rd, writes=[dbuf])

        def stq(eng, dst, src, sbuf, dram):
            S.dma("act", lambda e: e.dma_start(out=dst, in_=src), sbuf.ds, reads=[sbuf], partial=[dram])

        def mm(o, lhsT, rhs, first, last, rd, pb):
            S.op("pe", lambda e: e.matmul(o, lhsT=lhsT, rhs=rhs, start=first, stop=last), reads=rd,
                 writes=[pb] if first else (), partial=() if first else [pb])

        def mmp(o, lhsT, rhs, first, last, rd, pb):
            S.op("pe", lambda e: e.matmul(o, lhsT=lhsT, rhs=rhs, start=first, stop=last), reads=rd, partial=[pb])

        def tr(o, i_, idt, rd, pb, first):
            S.op("pe", lambda e: e.transpose(out=o, in_=i_, identity=idt), reads=rd,
                 writes=[pb] if first else (), partial=() if first else [pb])

        def act(o, i_, func, rd, wr, eng="act", **kw):
            S.op(eng, lambda e: e.activation(out=o, in_=i_, func=func, **kw), reads=rd, writes=wr)

        def tt(eng, o, a, b, op, rd, wr, pr=()):
            S.op(eng, lambda e: e.tensor_tensor(out=o, in0=a, in1=b, op=op), reads=rd, writes=wr, partial=pr)

        def ts(eng, o, a, s1, s2, op0, op1, rd, wr, pr=(), **kw):
            if s2 is None:
                S.op(eng, lambda e: e.tensor_scalar(out=o, in0=a, scalar1=s1, scalar2=None, op0=op0, **kw), reads=rd, writes=wr, partial=pr)
            else:
                S.op(eng, lambda e: e.tensor_scalar(out=o, in0=a, scalar1=s1, scalar2=s2, op0=op0, op1=op1, **kw), reads=rd, writes=wr, partial=pr)

        def stt(o, a, s, b, op0, op1, rd, wr, pr=()):
            S.op("dve", lambda e: e.scalar_tensor_tensor(out=o, in0=a, scalar=s, in1=b, op0=op0, op1=op1), reads=rd, writes=wr, partial=pr)

        def cp(eng, o, i_, rd, wr, pr=()):
            if eng == "act":
                S.op("act", lambda e: e.copy(out=o, in_=i_), reads=rd, writes=wr, partial=pr)
            else:
                S.op(eng, lambda e: e.tensor_copy(out=o, in_=i_), reads=rd, writes=wr, partial=pr)

        def memset(eng, o, v, wr):
            S.op(eng, lambda e: e.memset(o, v), writes=wr)

        CF, bCF = SB("CF", [128, 8, 128], F32)
        CB, bCB = SB("CB", [128, 12, 128], BF16)
        RM, bRM = SB("RM", [128, 4], F32)
        ld("sp", CF[:], cst_f, bCF); ld("sp", CB[:], cst_b, bCB); ld("sp", RM[:], rowmask_in, bRM)
        IDXQ, bIDXQ = SB("IDXQ", [128, NQT + 1], I32)
        ld("sp", IDXQ[:], idxq_in, bIDXQ)
        UF_f, UB_f, DF_f, DB_f, UTS_f, ONES_f, ID_f, EBASE = [CF[:, i, :] for i in range(8)]
        ID_b, MPREV, MNEXT, UF_b, UB_b = [CB[:, i, :] for i in range(5)]
        BDC = CB[:, 5:9, :]
        BONES = CB[:, 9, 0:2]

        PS = [(st.enter_context(nc.psum_tensor("ps%d" % i, [128, 512], F32)), Buf("ps%d" % i)) for i in range(8)]

        MODL, bMODL = SB("MODL", [128, 6, D], F32)
        MODC, bMODC = SB("MODC", [128, 6, D], F32)
        eps_t, beps = SB("eps_t", [128, 1], F32)
        memset("pool", eps_t[:], RMS_EPS, [beps])

        WK4 = Pool("wk4", [128, D], F32, 4)
        WKB = Pool("wkb", [128, D], BF16, 4)
        SM = Pool("sm", [128, 16], F32, 8)
        phase = [None]

        def new_phase():
            if phase[0] is not None:
                S.barrier(list(DS.values()))
                S.emit()
                phase[0].close()
            phase[0] = contextlib.ExitStack()
            return phase[0]

        def rstd_from_ss(ss_ap, bss, n):
            t, b = SM.next()
            S.op("act", lambda e: e.activation(out=t[:, 0:1], in_=ss_ap, func=AF.Sqrt, bias=eps_t[:, 0:1], scale=1.0 / n),
                 reads=[bss, beps], writes=[b])
            t2, b2 = SM.next()
            S.op("dve", lambda e: e.reciprocal(out=t2[:, 0:1], in_=t[:, 0:1]), reads=[b], writes=[b2])
            return t2[:, 0:1], b2

        def norm_mod_T(xt, bx, ms, bms, gs, shs, t0, keep=None):
            junk, bj = WK4.next()
            ss, bss = SM.next()
            act(junk[:], xt[:], AF.Square, [bx], [bj, bss], accum_out=ss[:, 0:1])
            rs, brs = rstd_from_ss(ss[:, 0:1], bss, D)
            tmp, btmp = WK4.next()
            stt(tmp[:], xt[:], rs, ms[:, gs, :], ALU.mult, ALU.mult, [bx, brs, bms], [btmp])
            hb, bhb = WKB.next()
            tt("pool", hb[:], tmp[:], ms[:, shs, :], ALU.add, [btmp, bms], [bhb])
            if keep is not None:
                tt("dve", keep[0][:], tmp[:], ms[:, shs, :], ALU.add, [btmp, bms], [keep[1]])
            pt, bpt = PS[7]
            ptb = pt[:].bitcast(BF16)
            for kc in range(8):
                tr(ptb[:, kc * 128:(kc + 1) * 128], hb[:, kc * 128:(kc + 1) * 128], ID_b, [bhb, bCB], bpt, kc == 0)
            hT, bhT = WKB.next()
            cp("act", hT[:], ptb, [bpt], [bhT])
            stq("sp", HT[:, :, t0:t0 + 128].rearrange("k p t -> p k t"), hT[:].rearrange("p (k t) -> p k t", k=8), bhT, dB["HT"])
            return hb, bhb

        for l in range(nlayers):
            lam_init = 0.8 - 0.6 * float(np.exp(-0.3 * l))
            last = (l == nlayers - 1)
            CAPl = CAP1 if last else CAP
            NSL = NE * CAPl
            EB = CF[:, 7, NE:2 * NE] if last else CF[:, 7, 0:NE]
            p6_tiles = list(range(NQT)) if last else list(range(NT))
            if phase[0] is not None:
                S.barrier(list(DS.values())); S.emit(); phase[0].close(); phase[0] = None
            lay = contextlib.ExitStack()
            LBT, bLBT = SB("LBT", [128, 1024], F32, lay)
            OMT, bOMT = SB("OMT", [128, 1024], F32, lay)
            LBF, bLBF = SB("LBF", [128, 8], F32, lay)
            OMF, bOMF = SB("OMF", [128, 8], F32, lay)
            DL, bDL = SB("DL", [128, 256], F32, lay)
            LV, bLV = SB("LV", [128, 8], F32, lay)
            DNW, bDNW = SB("DNW", [128, 128], F32, lay)
            HNW, bHNW = SB("HNW", [128, 128], F32, lay)
            SK, bSK = SB("SK", [128, 8], F32, lay)
            KMB, bKMB = SB("KMB", [128, 10], F32, lay)
            NKM, bNKM = SB("NKM", [128, 10], F32, lay)
            NKM8, bNKM8 = SB("NKM8", [128, 10], F32, lay)
            RS_, bRS = SB("RS", [128, NE], F32, lay)
            DEST, bDEST = SB("DEST", [128, NT + 1, 4], I32, lay)
            G4, bG4 = SB("G4", [128, NT, 4], F32, lay)
            ph = new_phase()
            W16a, bW16a = SB("W16a", [128, 8, 512], F32, ph)
            W16b, bW16b = SB("W16b", [128, 8, 512], F32, ph)
            cs, bcs = SB("cs", [128, 16], F32, ph)
            ld("sp", cs[:, 0:8], c_col, bcs); ld("sp", cs[:, 8:16], cc_col, bcs)
            act(cs[:], cs[:], AF.Silu, [bcs], [bcs])
            LH, bLH = SB("LH", [128, 16, 128], F32, ph)
            for j in range(16):
                cp("pool", LH[:, j, :], cs[:, j:j + 1].to_broadcast([128, 128]), [bcs], (), pr=[bLH])
            NG, bNG = W16b, bW16b
            for cb in range(12):
                ld("sp", W16a[:], w_mod[l, :, cb * 512:(cb + 1) * 512].rearrange("(k p) n -> p k n", p=128), bW16a)
                bm, bbm = WK4.next()
                ld("sp", bm[:, 0:512], b_mod[l, cb * 512:(cb + 1) * 512].partition_broadcast(128), bbm)
                for si, (ms, bms) in enumerate(((MODL, bMODL), (MODC, bMODC))):
                    pp, bpp = PS[si]
                    for kc in range(8):
                        mm(pp[:], LH[:, si * 8 + kc, :], W16a[:, kc, :], kc == 0, kc == 7, [bLH, bW16a], bpp)
                    tt("dve", ms[:, cb // 2, (cb % 2) * 512:(cb % 2) * 512 + 512], pp[:], bm[:, 0:512], ALU.add, [bpp, bbm], (), pr=[bms])
            ld("sp", NG[:].rearrange("p k n -> p (k n)"), norm_g[l].partition_broadcast(128), bNG)
            NGv = NG[:].rearrange("p k n -> p (k n)")
            for ms, bms in ((MODL, bMODL), (MODC, bMODC)):
                stt(ms[:, 1, :], ms[:, 1, :], 1.0, NGv[:, 0:D], ALU.add, ALU.mult, [bms, bNG], [bms])
                tt("dve", ms[:, 2, :], ms[:, 2, :], NGv[:, D:2 * D], ALU.mult, [bms, bNG], [bms])
                stt(ms[:, 4, :], ms[:, 4, :], 1.0, NGv[:, 2 * D:3 * D], ALU.add, ALU.mult, [bms, bNG], [bms])
                tt("dve", ms[:, 5, :], ms[:, 5, :], NGv[:, 3 * D:4 * D], ALU.mult, [bms, bNG], [bms])

            if l == 0:
                memset("pool", LBT[:], 0.0, [bLBT]); memset("pool", OMT[:], 1.0, [bOMT])
                memset("pool", LBF[:], 0.0, [bLBF]); memset("pool", OMF[:], 1.0, [bOMF])
            else:
                t1, b1 = WK4.next()
                ld("sp", LBT[:], lbl[1].partition_broadcast(128), bLBT)
                ld("sp", t1[:], lbl[0].partition_broadcast(128), b1)
                tt("dve", LBT[:], LBT[:], t1[:], ALU.subtract, [bLBT, b1], [bLBT])
                act(LBT[:], LBT[:], AF.Sigmoid, [bLBT], [bLBT])
                ts("dve", OMT[:], LBT[:], -1.0, 1.0, ALU.mult, ALU.add, [bLBT], [bOMT])
                t2, b2 = SM.next()
                ld("sp", LBF[:], lbl_fm[1], bLBF); ld("sp", t2[:, 0:8], lbl_fm[0], b2)
                tt("dve", LBF[:], LBF[:], t2[:, 0:8], ALU.subtract, [bLBF, b2], [bLBF])
                act(LBF[:], LBF[:], AF.Sigmoid, [bLBF], [bLBF])
                ts("dve", OMF[:], LBF[:], -1.0, 1.0, ALU.mult, ALU.add, [bLBF], [bOMF])
            ld("sp", DL[:], dlam[l].partition_broadcast(128), bDL)
            tt("dve", DL[:, 0:64], DL[:, 0:64], DL[:, 64:128], ALU.mult, [bDL], [bDL])
            tt("dve", DL[:, 128:192], DL[:, 128:192], DL[:, 192:256], ALU.mult, [bDL], [bDL])
            S.op("dve", lambda e: e.reduce_sum(out=LV[:, 0:1], in_=DL[:, 0:64], axis=AX.X), reads=[bDL], writes=[bLV])
            S.op("dve", lambda e: e.reduce_sum(out=LV[:, 1:2], in_=DL[:, 128:192], axis=AX.X), reads=[bDL], partial=[bLV])
            act(LV[:, 0:2], LV[:, 0:2], AF.Exp, [bLV], [bLV])
            tt("dve", LV[:, 2:3], LV[:, 1:2], LV[:, 0:1], ALU.subtract, [bLV], [bLV])
            ts("dve", LV[:, 3:4], LV[:, 2:3], -lam_init, None, ALU.add, None, [bLV], [bLV])
            NEGLAM = LV[:, 3:4]
            ld("sp", DNW[:], dnw[l].partition_broadcast(128), bDNW)
            ts("dve", DNW[:], DNW[:], 1.0 - lam_init, None, ALU.mult, None, [bDNW], [bDNW])
            ld("sp", HNW[:], hnw[l].partition_broadcast(128), bHNW)
            ld("sp", SK[:], sink_in[l].partition_broadcast(128), bSK)

            ph = new_phase()
            if l == 0:
                S.dma("sp", lambda e: e.dma_start(out=XR[0:L_, :], in_=ctxin), bCF.ds, partial=[dB["XR"]])
                for r0 in range(0, S_, 512):
                    S.dma("sp", lambda e, r0=r0: e.dma_start(out=XR[L_ + r0:L_ + r0 + 512, :], in_=xin[r0:r0 + 512, :]), bCB.ds, partial=[dB["XR"]])
            for i in range(NT):
                t0 = 128 * i
                ms, bms = (MODC, bMODC) if i < NCT else (MODL, bMODL)
                xt, bx = WK4.next()
                ld("sp", xt[:], XR[t0:t0 + 128, :], bx, [dB["XR"]])
                norm_mod_T(xt, bx, ms, bms, 1, 0, t0)

            ph = new_phase()
            WB8 = Pool("wb8", [128, 8, 512], BF16, 2, ph)
            HB = Pool("hb", [128, 8, 512], BF16, 3, ph)
            fm_jobs = []
            for i in range(4):
                fm_jobs.append((C_AQ + 128 * i, P_AQ + 128 * i, "rope", (QA, QAN, "QA", "QAN", 2 * i)))
            fm_jobs.append((C_AK, P_AK, "rope", (KA, KAN, "KA", "KAN", 0)))
            for i in range(4):
                fm_jobs.append((C_BQ + 128 * i, P_BQ + 128 * i, "rope", (QB, QBN, "QB", "QBN", 2 * i)))
            for i in range(4):
                fm_jobs.append((C_BK + 128 * i, P_BK + 128 * i, "rope", (KB, KBN, "KB", "KBN", 2 * i)))
            for i in range(4):
                fm_jobs.append((C_CQ + 128 * i, None, "silu", i))
            for i in range(4):
                fm_jobs.append((C_FF + 128 * i, None, "kfm", i))
            for i in range(4):
                fm_jobs.append((C_FB + 128 * i, None, "kfm", 4 + i))
            for (c0, p0, kind, info) in fm_jobs:
                wt, bwt = WB8.next()
                ld("pool", wt[:, :, 0:128], w_in[l, :, c0:c0 + 128].rearrange("(k p) n -> p k n", p=128), bwt)
                if kind == "rope":
                    ld("pool", wt[:, :, 128:256], w_inp[l, :, p0:p0 + 128].rearrange("(k p) n -> p k n", p=128), bwt)
                for (t0, n) in tok_blocks:
                    hb, bhb = HB.next()
                    ld("sp", hb[:, :, 0:n], HT[:, :, t0:t0 + n].rearrange("k p t -> p k t"), bhb, [dB["HT"]])
                    pa, bpa = PS[0]
                    for kc in range(8):
                        mm(pa[:, 0:n], wt[:, kc, 0:128], hb[:, kc, 0:n], kc == 0, kc == 7, [bwt, bhb], bpa)
                    if kind == "rope":
                        Qd, Nd, qn, nn, h0 = info
                        pb_, bpb = PS[1]
                        for kc in range(8):
                            mm(pb_[:, 0:n], wt[:, kc, 128:256], hb[:, kc, 0:n], kc == 0, kc == 7, [bwt, bhb], bpb)
                        rc, brc = WK4.next()
                        ld("sp", rc[:, 0:n], ropc[:, t0:t0 + n], brc); ld("sp", rc[:, 512:512 + n], rops[:, t0:t0 + n], brc)
                        t1, b1 = WK4.next()
                        tt("dve", t1[:, 0:n], pa[:, 0:n], rc[:, 0:n], ALU.mult, [bpa, brc], [b1])
                        tt("dve", t1[:, 512:512 + n], pb_[:, 0:n], rc[:, 512:512 + n], ALU.mult, [bpb, brc], (), pr=[b1])
                        qt, bqt = WKB.next()
                        tt("pool", qt[:, 0:n], t1[:, 0:n], t1[:, 512:512 + n], ALU.add, [b1], [bqt])
                        sq, bsq = WKB.next()
                        act(sq[:, 0:n], qt[:, 0:n], AF.Square, [bqt], [bsq])
                        pn, bpn = PS[2]
                        mm(pn[0:2, 0:n], BONES, sq[:, 0:n], True, True, [bCB, bsq], bpn)
                        nr, bnr = WK4.next()
                        act(nr[0:2, 0:n], pn[0:2, 0:n], AF.Sqrt, [bpn], [bnr])
                        stq("sp", Nd[h0:h0 + 2, t0:t0 + n], nr[0:2, 0:n], bnr, dB[nn])
                        stq("sp", Qd[h0, :, t0:t0 + n], qt[0:64, 0:n], bqt, dB[qn])
                        stq("sp", Qd[h0 + 1, :, t0:t0 + n], qt[64:128, 0:n], bqt, dB[qn])
                    elif kind == "silu":
                        qt, bqt = WKB.next()
                        act(qt[:, 0:n], pa[:, 0:n], AF.Silu, [bpa], [bqt])
                        stq("sp", HQ[info, :, t0:t0 + n], qt[:, 0:n], bqt, dB["HQ"])
                    else:
                        t1, b1 = WK4.next()
                        act(t1[:, 0:n], pa[:, 0:n], AF.Sigmoid, [bpa], [b1], scale=-1.0)
                        qt, bqt = WKB.next()
                        ts("dve", qt[:, 0:n], t1[:, 0:n], OMF[:, info:info + 1], None, ALU.mult, None, [b1, bOMF], [bqt])
                        stq("sp", HK[info, :, t0:t0 + n], qt[:, 0:n], bqt, dB["HK"])

            tm_jobs = [(C_AV, 128, "copy", (VA, "VA", 0)), (C_BV, 512, "copy", (VB, "VB", 0)), (C_CI, 512, "copy", (HV, "HV", 0)),
                       (C_CG, 512, "silu", (HGT, "HGT", 0)), (C_FF, 512, "logf", 0), (C_FB, 512, "logf", 1)]
            for j in range(6):
                tm_jobs.append((C_GT + 512 * j, 512, "sigm", (GT, "GT", 512 * j)))
            for (c0, w, kind, info) in tm_jobs:
                wt, bwt = WB8.next()
                ld("pool", wt[:, :, 0:w], w_in[l, :, c0:c0 + w].rearrange("(k p) n -> p k n", p=128), bwt)
                for (t0, n) in tok_blocks:
                    hb, bhb = HB.next()
                    ld("sp", hb[:, :, 0:n], HT[:, :, t0:t0 + n].rearrange("k p t -> p k t"), bhb, [dB["HT"]])
                    for s in range(n // 128):
                        tt0 = t0 + 128 * s
                        pa, bpa = PS[s % 2]
                        for kc in range(8):
                            mm(pa[:, 0:w], hb[:, kc, 128 * s:128 * s + 128], wt[:, kc, 0:w], kc == 0, kc == 7, [bwt, bhb], bpa)
                        if kind in ("copy", "silu", "sigm"):
                            dst, dn, co = info
                            ot, bot = WKB.next()
                            fn = {"copy": AF.Copy, "silu": AF.Silu, "sigm": AF.Sigmoid}[kind]
                            act(ot[:, 0:w], pa[:, 0:w], fn, [bpa], [bot])
                            stq("sp", dst[tt0:tt0 + 128, co:co + w], ot[:, 0:w], bot, dB[dn])
                        else:
                            d_ = info
                            t1, b1 = WK4.next()
                            act(t1[:, 0:512], pa[:, 0:512], AF.Sigmoid, [bpa], [b1])
                            tt("dve", t1[:, 0:512], t1[:, 0:512], OMT[:, d_ * 512:(d_ + 1) * 512], ALU.mult, [b1, bOMT], [b1])
                            tt("dve", t1[:, 0:512], t1[:, 0:512], LBT[:, d_ * 512:(d_ + 1) * 512], ALU.add, [b1, bLBT], [b1])
                            kt, bkt = WKB.next()
                            ts("dve", kt[:, 0:512], t1[:, 0:512], -1.0, 1.0, ALU.mult, ALU.add, [b1], [bkt])
                            stq("sp", HKt[tt0:tt0 + 128, d_ * 512:(d_ + 1) * 512], kt[:, 0:512], bkt, dB["HKt"])
                            act(t1[:, 512:1024], t1[:, 0:512], AF.Ln, [b1], [b1])
                            stq("sp", HG[tt0:tt0 + 128, d_ * 512:(d_ + 1) * 512], t1[:, 512:1024], b1, dB["HG"])

            ph = new_phase()
            kmrow, bkmrow = SB("kmrow", [1, T], F32, ph)
            kmv, bkmv = SB("kmv", [1, 16], F32, ph)
            for ku in range(10):
                src = KAN[ku:ku + 1, :] if ku < 2 else KBN[ku - 2:ku - 1, :]
                ld("sp", kmrow[:], src, bkmrow, [dB["KAN"], dB["KBN"]])
                S.op("dve", lambda e, ku=ku: e.reduce_max(out=kmv[:, ku:ku + 1], in_=kmrow[:], axis=AX.X), reads=[bkmrow], partial=[bkmv])
            stq("sp", KMX.rearrange("a b -> b a"), kmv[:, 0:10], bkmv, dB["KMX"])
            ld("sp", KMB[:], KMX.rearrange("a b -> (a b)").partition_broadcast(128), bKMB, [dB["KMX"]])
            ts("dve", NKM[:], KMB[:], -1.0, None, ALU.mult, None, [bKMB], [bNKM])
            ts("dve", NKM8[:], KMB[:], -0.125, None, ALU.mult, None, [bKMB], [bNKM8])

            ph = new_phase()
            KAUG = [SB("kaug%d" % i, [65, T], BF16, ph) for i in range(2)]
            VS, bVS = SB("vs", [128, NT, 132], BF16, ph)
            memset("pool", VS[:], 1.0, [bVS])
            QAUG = Pool("qaug", [65, 512], BF16, 2, ph)
            QNR = Pool("qnr", [65, 512], F32, 2, ph)
            PT = Pool("pt", [128, 512], BF16, 4, ph)
            OT = Pool("ot", [128, 512], F32, 2, ph)
            OT2 = Pool("ot2", [128, 512], F32, 2, ph)
            for i in range(2):
                memset("pool", KAUG[i][0][64:65, :], 1.0, [KAUG[i][1]])

            def load_k(slot, Kd, kname, ku):
                kt_, bk_ = KAUG[slot]
                S.dma("sp", lambda e: e.dma_start(out=kt_[0:64, :], in_=Kd[ku]), bk_.ds, reads=[dB[kname]], writes=[bk_])

            def load_v(Vd, vname, v0, dv):
                for j0 in range(0, NT, 11):
                    j1 = min(NT, j0 + 11)
                    S.dma("sp", lambda e, j0=j0, j1=j1: e.dma_start(out=VS[:, j0:j1, 0:dv], in_=Vd[128 * j0:128 * j1, v0:v0 + dv].rearrange("(j p) d -> p j d", p=128)),
                          bVS.ds, reads=[dB[vname]], writes=[bVS] if j0 == 0 else (), partial=() if j0 == 0 else [bVS])
                memset("pool", VS[:, :, dv:dv + 1], 1.0, [bVS])

            def attn_unit(Qd, Nd, qname, nname, u, kslot, kmcol, t0, n, keys, dv, accs):
                kt_, bk_ = KAUG[kslot]
                qa, bqa = QAUG.next()
                ld("sp", qa[0:64, 0:n], Qd[u, :, t0:t0 + n], bqa, [dB[qname]])
                qn, bqn = QNR.next()
                ld("sp", qn[64:65, 0:n], Nd[u:u + 1, t0:t0 + n], bqn, [dB[nname]])
                S.op("act", lambda e: e.activation(out=qa[64:65, 0:n], in_=qn[64:65, 0:n], func=AF.Copy, scale=NKM[64:65, kmcol:kmcol + 1]),
                     reads=[bqn, bNKM], partial=[bqa])
                nk = len(keys)

                def issue_st(ki):
                    pS, bpS = PS[ki % 2]
                    j = keys[ki][0]
                    mm(pS[:, 0:n], kt_[:, 128 * j:128 * j + 128], qa[:, 0:n], True, True, [bk_, bqa], bpS)
                    return pS, bpS

                cur_st = issue_st(0)
                for ki, (j, mask) in enumerate(keys):
                    pS, bpS = cur_st
                    if ki + 1 < nk:
                        cur_st = issue_st(ki + 1)
                    pt_, bpt_ = PT.next()
                    act(pt_[:, 0:n], pS[:, 0:n], AF.Exp, [bpS], [bpt_], scale=0.125)
                    if mask is not None:
                        tt("pool", pt_[:, 0:n], pt_[:, 0:n], mask, ALU.mult, [bpt_, bCB], [bpt_])
                    for s in range(n // 128):
                        ao, ab = accs[s]
                        mmp(ao, pt_[:, 128 * s:128 * s + 128], VS[:, j, 0:dv + 1], ki == 0, ki == nk - 1, [bpt_, bVS], ab)

            def acc_views(dv, banks):
                res = []
                for s in range(4):
                    p, b = PS[banks[s // 2]]
                    res.append((p[:, (s % 2) * 256:(s % 2) * 256 + dv + 1], b))
                return res

            for ku in range(2):
                load_k(0, KA, "KA", ku)
                load_v(VA, "VA", 64 * ku, 64)
                for g in range(4):
                    hq = 4 * ku + g
                    for i in range(NT):
                        t0 = 128 * i
                        if i < NCT:
                            keys = [(j, None) for j in range(NCT)]
                        else:
                            keys = [(j, None) for j in range(NCT)]
                            if i > NCT:
                                keys.append((i - 1, MPREV))
                            keys.append((i, None))
                            if i < NT - 1:
                                keys.append((i + 1, MNEXT))
                        accs = acc_views(64, (2 + (i % 2) * 2, 3 + (i % 2) * 2))
                        pass
                        attn_unit(QA, QAN, "QA", "QAN", hq, 0, ku, t0, 128, keys, 64, accs)
                        qn, bqn = SM.next()
                        S.dma("sp", lambda e, qn=qn, t0=t0, hq=hq: e.dma_start(out=qn[:, 0:1], in_=QAN[hq, t0:t0 + 128].rearrange("(p o) -> p o", o=1)),
                              bqn.ds, reads=[dB["QAN"]], writes=[bqn])
                        es, bes = SM.next()
                        S.op("act", lambda e, es=es, qn=qn, ku=ku, hq=hq: e.activation(out=es[:, 0:1], in_=qn[:, 0:1], func=AF.Exp,
                                                                                        scale=NKM8[:, ku:ku + 1], bias=SK[:, hq:hq + 1]),
                             reads=[bqn, bNKM8, bSK], writes=[bes])
                        ao, ab = accs[0]
                        tt("dve", es[:, 1:2], es[:, 0:1], ao[:, 64:65], ALU.add, [bes, ab], [bes])
                        S.op("dve", lambda e, es=es: e.reciprocal(out=es[:, 2:3], in_=es[:, 1:2]), reads=[bes], writes=[bes])
                        o_, bo_ = WKB.next()
                        ts("dve", o_[:, 0:64], ao[:, 0:64], es[:, 2:3], None, ALU.mult, None, [ab, bes], [bo_])
                        stq("sp", OA[t0:t0 + 128, 64 * hq:64 * hq + 64], o_[:, 0:64], bo_, dB["OA"])

            for h in range(4):
                load_k(0, KB, "KB", 2 * h); load_k(1, KB, "KB", 2 * h + 1)
                load_v(VB, "VB", 128 * h, 128)
                for bi, (t0, n) in enumerate(tok_blocks):
                    keys = [(j, None) for j in range(NCT)] if t0 < L_ else [(j, None) for j in range(NT)]
                    O0, bO0 = OT.next()
                    for c in range(2):
                        accs = [(PS[2 + s_][0][:, 0:129], PS[2 + s_][1]) for s_ in range(4)]
                        attn_unit(QB, QBN, "QB", "QBN", 2 * h + c, c, 2 + 2 * h + c, t0, n, keys, 128, accs)
                        for s in range(n // 128):
                            a_, b_ = accs[s]
                            r, br = SM.next()
                            S.op("dve", lambda e, r=r, a_=a_: e.reciprocal(out=r[:, 0:1], in_=a_[:, 128:129]), reads=[b_], writes=[br])
                            if c == 0:
                                ts("dve", O0[:, 128 * s:128 * s + 128], a_[:, 0:128], r[:, 0:1], None, ALU.mult, None, [b_, br], (), pr=[bO0])
                            else:
                                tt("dve", r[:, 1:2], r[:, 0:1], NEGLAM, ALU.mult, [br, bLV], [br])
                                o0, bo0 = OT2.next()
                                stt(o0[:, 128:256], a_[:, 0:128], r[:, 1:2], O0[:, 128 * s:128 * s + 128], ALU.mult, ALU.add, [b_, br, bO0], [bo0])
                                ss, bss = SM.next()
                                act(o0[:, 256:384], o0[:, 128:256], AF.Square, [bo0], [bo0, bss], accum_out=ss[:, 0:1])
                                rs, brs = rstd_from_ss(ss[:, 0:1], bss, 128)
                                ob_, bob = WKB.next()
                                stt(ob_[:, 0:128], o0[:, 128:256], rs, DNW[:], ALU.mult, ALU.mult, [bo0, brs, bDNW], [bob])
                                tt0 = t0 + 128 * s
                                stq("sp", OB[tt0:tt0 + 128, 128 * h:128 * h + 128], ob_[:, 0:128], bob, dB["OB"])

            ph = new_phase()
            SF = [SB("sf%d" % h, [128, 128], F32, ph) for h in range(4)]
            SBF = [Pool("sbf%d_" % h, [128, 128], BF16, 2, ph) for h in range(4)]
            GP = Pool("gp", [128, 512], F32, 2, ph)
            QTp = Pool("qtp", [128, 512], BF16, 2, ph)
            KTp = Pool("ktp", [128, 512], BF16, 2, ph)
            KKp = Pool("kkp", [128, 512], BF16, 2, ph)
            VVp = Pool("vvp", [128, 512], BF16, 2, ph)
            E1p = Pool("e1p", [128, 512], F32, 2, ph)
            E2p = Pool("e2p", [128, 512], F32, 2, ph)
            QSp = Pool("qsp", [128, 512], BF16, 2, ph)
            KSp = Pool("ksp", [128, 512], BF16, 2, ph)
            KHp = Pool("khp", [128, 512], BF16, 2, ph)
            KHCp = Pool("khcp", [128, 4, 512], BF16, 2, ph)
            QSCp = Pool("qscp", [128, 4, 128], BF16, 8, ph)
            ATp = Pool("atp", [128, 128], BF16, 4, ph)
            OOp = Pool("oop", [128, 512], F32, 2, ph)
            for d_ in range(2):
                Um_f = UF_f if d_ == 0 else UB_f
                Dm_f = DF_f if d_ == 0 else DB_f
                Um_b = UF_b if d_ == 0 else UB_b
                if d_ == 0:
                    order = list(range(NT))
                else:
                    order = list(range(NCT - 1, -1, -1)) + list(range(NT - 1, NCT - 1, -1))
                corder = [0, 1, 2, 3] if d_ == 0 else [3, 2, 1, 0]
                cur = []
                for h in range(4):
                    memset("pool", SF[h][0][:], 0.0, [SF[h][1]])
                    sb_, bsb_ = SBF[h].next()
                    memset("pool", sb_[:], 0.0, [bsb_])
                    cur.append((sb_, bsb_))
                for i in order:
                    t0 = 128 * i
                    g, bg = GP.next(); ld("sp", g[:], HG[t0:t0 + 128, 512 * d_:512 * d_ + 512], bg, [dB["HG"]])
                    qT, bqT = QTp.next(); ld("sp", qT[:].rearrange("p (h t) -> p h t", h=4), HQ[:, :, t0:t0 + 128].rearrange("h p t -> p h t"), bqT, [dB["HQ"]])
                    kT, bkT = KTp.next(); ld("sp", kT[:].rearrange("p (h t) -> p h t", h=4), HK[4 * d_:4 * d_ + 4, :, t0:t0 + 128].rearrange("h p t -> p h t"), bkT, [dB["HK"]])
                    kk, bkk = KKp.next(); ld("sp", kk[:], HKt[t0:t0 + 128, 512 * d_:512 * d_ + 512], bkk, [dB["HKt"]])
                    vv, bvv = VVp.next(); ld("sp", vv[:], HV[t0:t0 + 128, :], bvv, [dB["HV"]])
                    pB, bpB = PS[0]
                    for h in range(4):
                        mmp(pB[:, 128 * h:128 * h + 128], g[:, 128 * h:128 * h + 128], Um_f, True, True, [bg, bCF], bpB) if h else \
                            mm(pB[:, 0:128], g[:, 0:128], Um_f, True, True, [bg, bCF], bpB)
                    e1, be1 = E1p.next(); act(e1[:], pB[:], AF.Exp, [bpB], [be1])
                    e2, be2 = E2p.next(); act(e2[:], pB[:], AF.Exp, [bpB], [be2], scale=-1.0)
                    qs, bqs = QSp.next(); tt("dve", qs[:], qT[:], e1[:], ALU.mult, [bqT, be1], [bqs])
                    ks, bks = KSp.next(); tt("dve", ks[:], kT[:], e2[:], ALU.mult, [bkT, be2], [bks])
                    pD, bpD = PS[1]
                    mm(pD[:], Dm_f, g[:], True, True, [bCF, bg], bpD)
                    e3, be3 = E2p.next(); act(e3[:], pD[:], AF.Exp, [bpD], [be3])
                    kh, bkh = KHp.next(); tt("dve", kh[:], kk[:], e3[:], ALU.mult, [bkk, be3], [bkh])
                    khc, bkhc = KHCp.next()
                    for c in range(4):
                        ts("dve", khc[:, c, :], kh[:], RM[:, c:c + 1], None, ALU.mult, None, [bkh, bRM], (), pr=[bkhc])
                    pOs = [PS[4 + h] for h in range(4)]
                    qscs = []
                    for h in range(4):
                        hs = slice(128 * h, 128 * h + 128)
                        qsc, bqsc = QSCp.next()
                        for c in range(4):
                            tt("pool", qsc[:, c, :], qs[:, hs], BDC[:, c, :], ALU.mult, [bqs, bCB], (), pr=[bqsc])
                        pA, bpA = PS[2 + (h % 2)]
                        mm(pA[:, 0:128], ks[:, hs], qs[:, hs], True, True, [bks, bqs], bpA)
                        at_, bat = ATp.next()
                        tt("dve", at_[:], pA[:, 0:128], Um_b, ALU.mult, [bpA, bCB], [bat])
                        pO, bpO = pOs[h]
                        mm(pO[:, 0:128], at_[:], vv[:, hs], True, False, [bat, bvv], bpO)
                        qscs.append((qsc, bqsc))
                    for ci, c in enumerate(corder):
                        tl = 32 * c + 31 if d_ == 0 else 32 * c
                        for h in range(4):
                            hs = slice(128 * h, 128 * h + 128)
                            pO, bpO = pOs[h]
                            sf, bsf = SF[h]
                            sb_, bsb_ = cur[h]
                            qsc, bqsc = qscs[h]
                            mmp(pO[:, 0:128], qsc[:, c, :], sb_[:], False, ci == 3, [bqsc, bsb_], bpO)
                            pU, bpU = PS[2 + (h % 2)]
                            mm(pU[:, 0:128], khc[:, c, hs], vv[:, hs], True, True, [bkhc, bvv], bpU)
                            stt(sf[:], sf[:], e1[:, 128 * h + tl:128 * h + tl + 1], pU[:, 0:128], ALU.mult, ALU.add, [bsf, be1, bpU], [bsf])
                            nb_, bnb_ = SBF[h].next()
                            cp("act", nb_[:], sf[:], [bsf], [bnb_])
                            cur[h] = (nb_, bnb_)
                    oo, boo = OOp.next()
                    if d_ == 0:
                        for h in range(4):
                            hs = slice(128 * h, 128 * h + 128)
                            cp("act", oo[:, hs], pOs[h][0][:, 0:128], [pOs[h][1]], (), pr=[boo])
                        stq("sp", OF[t0:t0 + 128, :], oo[:], boo, dB["OF"])
                    else:
                        of_, bof = GP.next()
                        ld("sp", of_[:], OF[t0:t0 + 128, :], bof, [dB["OF"]])
                        for h in range(4):
                            hs = slice(128 * h, 128 * h + 128)
                            tt("dve", oo[:, hs], pOs[h][0][:, 0:128], of_[:, hs], ALU.add, [pOs[h][1], bof], (), pr=[boo])
                        gt_, bgt = WKB.next()
                        ld("sp", gt_[:, 0:512], HGT[t0:t0 + 128, :], bgt, [dB["HGT"]])
                        ss, bss = SM.next()
                        junk, bj = WK4.next()
                        for h in range(4):
                            hs = slice(128 * h, 128 * h + 128)
                            S.op("act", lambda e, junk=junk, oo=oo, ss=ss, hs=hs, h=h: e.activation(out=junk[:, hs], in_=oo[:, hs], func=AF.Square, accum_out=ss[:, h:h + 1]),
                                 reads=[boo], partial=[bj, bss])
                        t, b = SM.next()
                        S.op("act", lambda e, t=t, ss=ss: e.activation(out=t[:, 0:4], in_=ss[:, 0:4], func=AF.Sqrt, bias=eps_t[:, 0:1], scale=1.0 / 128),
                             reads=[bss, beps], writes=[b])
                        S.op("dve", lambda e, t=t: e.reciprocal(out=t[:, 4:8], in_=t[:, 0:4]), reads=[b], writes=[b])
                        oc, boc = WKB.next()
                        for h in range(4):
                            hs = slice(128 * h, 128 * h + 128)
                            stt(junk[:, hs], oo[:, hs], t[:, 4 + h:5 + h], HNW[:], ALU.mult, ALU.mult, [boo, b, bHNW], (), pr=[bj])
                        tt("dve", oc[:, 0:512], junk[:, 0:512], gt_[:, 0:512], ALU.mult, [bj, bgt], [boc])
                        stq("sp", OC[t0:t0 + 128, :], oc[:, 0:512], boc, dB["OC"])

            ph = new_phase()
            WBR, bWBR = SB("WBR", [128, 12, D], BF16, ph)
            ld("pool", WBR[:], w_br[l].rearrange("i (k p) n -> p (i k) n", p=128), bWBR)
            WO, bWO = SB("WO", [128, 8, D], BF16, ph)
            ld("pool", WO[:], w_out[l].rearrange("(k p) n -> p k n", p=128), bWO)
            WR, bWR = SB("WR", [128, 8, NE], F32, ph)
            ld("sp", WR[:], w_router[l].rearrange("(k p) n -> p k n", p=128), bWR)
            BR, bBR = SB("BR", [128, NE], F32, ph)
            ld("sp", BR[:], b_router[l].partition_broadcast(128), bBR)
            memset("pool", RS_[:], 0.0, [bRS])
            OTp = Pool("otp", [128, 12, 128], BF16, 2, ph)
            GTp = Pool("gtp", [128, 3072], BF16, 2, ph)
            H2Tp = Pool("h2t", [128, 8, 128], F32, 2, ph)
            SMR = Pool("smr", [128, 4, NE], F32, 3, ph)
            zt, bzt = WKB.next()
            memset("pool", zt[:], 0.0, [bzt])
            for r0 in range(0, NSL, 128):
                stq("sp", XBUF[r0:r0 + 128, :], zt[:], bzt, dB["XBUF"])

            def gath(dst, src, i, dbuf, srcname, first=True):
                S.dma("pool", lambda e: e.indirect_dma_start(out=dst, out_offset=None, in_=src,
                                                             in_offset=bass.IndirectOffsetOnAxis(ap=IDXQ[:, i:i + 1], axis=0)),
                      dbuf.ds, reads=[dB[srcname], bIDXQ], writes=[dbuf] if first else (), partial=() if first else [dbuf])

            for i in p6_tiles:
                t0 = 128 * i
                ms, bms = (MODL, bMODL) if (last or i >= NCT) else (MODC, bMODC)
                ob3, bob3 = WKB.next(), None
                oin, boin = ob3
                oin2, boin2 = WKB.next()
                gts, bgts = GTp.next()
                if last:
                    gath(oin[:, 0:512], OA, i, boin, "OA")
                    gath(oin[:, 512:1024], OB, i, boin, "OB", first=False)
                    gath(oin2[:, 0:512], OC, i, boin2, "OC")
                    gath(gts[:], GT, i, bgts, "GT")
                else:
                    ld("sp", oin[:, 0:512], OA[t0:t0 + 128, :], boin, [dB["OA"]])
                    ld("sp", oin[:, 512:1024], OB[t0:t0 + 128, :], boin, [dB["OB"]])
                    ld("sp", oin2[:, 0:512], OC[t0:t0 + 128, :], boin2, [dB["OC"]])
                    ld("sp", gts[:], GT[t0:t0 + 128, :], bgts, [dB["GT"]])
                pt, bpt = PS[7]
                ptb = pt[:].bitcast(BF16)
                oT, boT = OTp.next()
                for kc in range(8):
                    tr(ptb[:, kc * 128:(kc + 1) * 128], oin[:, kc * 128:(kc + 1) * 128], ID_b, [boin, bCB], bpt, kc == 0)
                cp("act", oT[:, 0:8, :], ptb.rearrange("p (k t) -> p k t", k=8), [bpt], [boT])
                for kc in range(4):
                    tr(ptb[:, kc * 128:(kc + 1) * 128], oin2[:, kc * 128:(kc + 1) * 128], ID_b, [boin2, bCB], bpt, kc == 0)
                cp("act", oT[:, 8:12, :], ptb[:, 0:512].rearrange("p (k t) -> p k t", k=4), [bpt], (), pr=[boT])
                y, by = WK4.next()
                tmpy, btmpy = WK4.next()
                for br_ in range(3):
                    for cb in range(2):
                        pp, bpp = PS[cb]
                        for kc in range(4):
                            mm(pp[:], oT[:, 4 * br_ + kc, :], WBR[:, 4 * br_ + kc, 512 * cb:512 * cb + 512], kc == 0, kc == 3, [boT, bWBR], bpp)
                        gsl = gts[:, D * br_ + 512 * cb:D * br_ + 512 * cb + 512]
                        if br_ == 0:
                            tt("dve", y[:, 512 * cb:512 * cb + 512], pp[:], gsl, ALU.mult, [bpp, bgts], (), pr=[by])
                        else:
                            tt("dve", tmpy[:, 512 * cb:512 * cb + 512], pp[:], gsl, ALU.mult, [bpp, bgts], (), pr=[btmpy])
                            tt("pool", y[:, 512 * cb:512 * cb + 512], y[:, 512 * cb:512 * cb + 512], tmpy[:, 512 * cb:512 * cb + 512], ALU.add, [btmpy, by], [by])
                yb, byb = WKB.next()
                cp("act", yb[:], y[:], [by], [byb])
                for kc in range(8):
                    tr(ptb[:, kc * 128:(kc + 1) * 128], yb[:, kc * 128:(kc + 1) * 128], ID_b, [byb, bCB], bpt, kc == 0)
                yT, byT = WKB.next()
                cp("act", yT[:], ptb, [bpt], [byT])
                ss, bss = SM.next()
                junk, bj = WK4.next()
                pps = []
                for cb in range(2):
                    pp, bpp = PS[2 + cb]
                    for kc in range(8):
                        mm(pp[:], yT[:, kc * 128:(kc + 1) * 128], WO[:, kc, 512 * cb:512 * cb + 512], kc == 0, kc == 7, [byT, bWO], bpp)
                    S.op("act", lambda e, junk=junk, pp=pp, ss=ss, cb=cb: e.activation(out=junk[:, 512 * cb:512 * cb + 512], in_=pp[:], func=AF.Square, accum_out=ss[:, cb:cb + 1]),
                         reads=[bpp], partial=[bj, bss])
                    pps.append((pp, bpp))
                tt("dve", ss[:, 2:3], ss[:, 0:1], ss[:, 1:2], ALU.add, [bss], [bss])
                rs, brs = rstd_from_ss(ss[:, 2:3], bss, D)
                xt, bx = WK4.next()
                if last:
                    gath(xt[:], XR, i, bx, "XR")
                else:
                    ld("sp", xt[:], XR[t0:t0 + 128, :], bx, [dB["XR"]])
                for cb in range(2):
                    pp, bpp = pps[cb]
                    stt(junk[:, 512 * cb:512 * cb + 512], pp[:], rs, ms[:, 2, 512 * cb:512 * cb + 512], ALU.mult, ALU.mult, [bpp, brs, bms], (), pr=[bj])
                tt("dve", xt[:], xt[:], junk[:], ALU.add, [bx, bj], [bx])
                if last:
                    stq("sp", XQ[t0:t0 + 128, :], xt[:], bx, dB["XQ"])
                else:
                    stq("sp", XR[t0:t0 + 128, :], xt[:], bx, dB["XR"])
                h2f, bh2f = WK4.next()
                hb2, bhb2 = norm_mod_T(xt, bx, ms, bms, 4, 3, t0, keep=(h2f, bh2f))
                h2T, bh2T = H2Tp.next()
                for half in range(2):
                    pr0, bpr0 = PS[4 + half]
                    for kc in range(4):
                        k2 = 4 * half + kc
                        tr(pr0[:, kc * 128:(kc + 1) * 128], h2f[:, k2 * 128:(k2 + 1) * 128], ID_f, [bh2f, bCF], bpr0, kc == 0)
                    cp("act", h2T[:, 4 * half:4 * half + 4, :], pr0[:].rearrange("p (k t) -> p k t", k=4), [bpr0], (), pr=[bh2T])
                pl, bpl = PS[6]
                for kc in range(8):
                    mm(pl[:, 0:NE], h2T[:, kc, :], WR[:, kc, :], kc == 0, kc == 7, [bh2T, bWR], bpl)
                sm, bsm = SMR.next()
                Lg = sm[:, 0, :]; Mk = sm[:, 1, :]; Pb = sm[:, 2, :]; Oh = sm[:, 3, :]
                tt("dve", Lg, pl[:, 0:NE], BR[:], ALU.add, [bpl, bBR], [bsm])
                t8, bt8 = SM.next()
                S.op("dve", lambda e, t8=t8, Lg=Lg: e.max(out=t8[:, 0:8], in_=Lg), reads=[bsm], writes=[bt8])
                ts("dve", t8[:, 8:9], t8[:, 0:1], -1.0, None, ALU.mult, None, [bt8], [bt8])
                S.op("act", lambda e, t8=t8: e.activation(out=t8[:, 9:13], in_=t8[:, 0:4], func=AF.Exp, bias=t8[:, 8:9], accum_out=t8[:, 13:14]),
                     reads=[bt8], writes=[bt8])
                S.op("dve", lambda e, t8=t8: e.reciprocal(out=t8[:, 14:15], in_=t8[:, 13:14]), reads=[bt8], writes=[bt8])
                ts("dve", G4[:, i, :], t8[:, 9:13], t8[:, 14:15], None, ALU.mult, None, [bt8], (), pr=[bG4])
                ts("dve", Mk, Lg, t8[:, 3:4], None, ALU.is_ge, None, [bsm, bt8], [bsm])
                pq, bpq = PS[6]
                mm(pq[:, 64:64 + NE], UTS_f, Mk, True, False, [bCF, bsm], bpq)
                mm(pq[:, 64:64 + NE], ONES_f, RS_[:], False, True, [bCF, bRS], bpq)
                tt("dve", Pb, pq[:, 64:64 + NE], EB, ALU.add, [bpq, bCF], [bsm])
                tt("pool", RS_[:], RS_[:], Mk, ALU.add, [bRS, bsm], [bRS])
                df, bdf = SM.next()
                for k in range(4):
                    ts("dve", Oh, Lg, t8[:, k:k + 1], None, ALU.is_equal, None, [bsm, bt8], [bsm])
                    tt("dve", Oh, Oh, Pb, ALU.mult, [bsm], [bsm])
                    S.op("dve", lambda e, df=df, Oh=Oh, k=k: e.reduce_sum(out=df[:, k:k + 1], in_=Oh, axis=AX.X), reads=[bsm], partial=[bdf])
                ts("dve", df[:, 0:4], df[:, 0:4], float(NSL - 1), None, ALU.min, None, [bdf], [bdf])
                cp("dve", DEST[:, i, :], df[:, 0:4], [bdf], (), pr=[bDEST])
                for k in range(4):
                    S.dma("pool", lambda e, i=i, k=k, hb2=hb2: e.indirect_dma_start(
                        out=XBUF, out_offset=bass.IndirectOffsetOnAxis(ap=DEST[:, i, k:k + 1], axis=0),
                        in_=hb2[:, :], in_offset=None),
                        bhb2.ds, reads=[bhb2, bDEST], writes=[dB["XBUF"]] if (i == p6_tiles[0] and k == 0) else (), partial=() if (i == p6_tiles[0] and k == 0) else [dB["XBUF"]])

            ph = new_phase()
            WGU = Pool("wgu", [128, 8, 2 * D], BF16, 1, ph)
            WDN = Pool("wdn", [128, 8, D], BF16, 1, ph)
            BGU = Pool("bgu", [128, 16], F32, 2, ph)
            BDN = Pool("bdn", [128, D], F32, 1, ph)
            XSp = Pool("xsp", [128, D], BF16, 3, ph)
            XTp = Pool("xtp", [128, 8, 512], BF16, 2, ph)
            ATT = Pool("att", [128, 8, 512], BF16, 1, ph)
            GCp = Pool("gcp", [128, 512], F32, 2, ph)
            SGp = Pool("sgp", [128, 512], F32, 2, ph)
            UCp = Pool("ucp", [128, 512], F32, 2, ph)
            YTp = Pool("ytp", [128, D], BF16, 2, ph)
            for e_ in range(NE):
                wg, bwg = WGU.next(); wd, bwd = WDN.next(); bg_, bbg = BGU.next(); bd_, bbd = BDN.next()
                for kc in range(8):
                    S.dma("pool", lambda e, wg=wg, kc=kc, e_=e_: e.dma_start(out=wg[:, kc, :], in_=w_gu[l, e_, kc * 128:(kc + 1) * 128, :]),
                          bwg.ds, writes=[bwg] if kc == 0 else (), partial=() if kc == 0 else [bwg])
                ld("pool", wd[:], w_dn[l, e_].rearrange("(k p) n -> p k n", p=128), bwd)
                ld("sp", bg_[:], b_gu_t[l, e_], bbg)
                ld("sp", bd_[:], b_dn[l, e_].partition_broadcast(128), bbd)
                for blk in range(CAPl // 512):
                    r0 = e_ * CAPl + 512 * blk
                    xT, bxT = XTp.next()
                    for s in range(4):
                        xs, bxs = XSp.next()
                        ld("sp", xs[:], XBUF[r0 + 128 * s:r0 + 128 * s + 128, :], bxs, [dB["XBUF"]])
                        pt, bpt = PS[6 + (s % 2)]
                        ptb = pt[:].bitcast(BF16)
                        for kc in range(8):
                            tr(ptb[:, kc * 128:(kc + 1) * 128], xs[:, kc * 128:(kc + 1) * 128], ID_b, [bxs, bCB], bpt, kc == 0)
                        cp("act", xT[:, :, 128 * s:128 * s + 128], ptb.rearrange("p (k t) -> p k t", k=8), [bpt], (), pr=[bxT])
                    aT, baT = ATT.next()
                    for f in range(8):
                        pg, bpg = PS[0 + (f % 2) * 2]
                        pu, bpu = PS[1 + (f % 2) * 2]
                        for kc in range(8):
                            mm(pg[:], wg[:, kc, 128 * f:128 * f + 128], xT[:, kc, :], kc == 0, kc == 7, [bwg, bxT], bpg)
                        for kc in range(8):
                            mm(pu[:], wg[:, kc, D + 128 * f:D + 128 * f + 128], xT[:, kc, :], kc == 0, kc == 7, [bwg, bxT], bpu)
                        gc, bgc = GCp.next(); sg, bsg = SGp.next(); uc, buc = UCp.next()
                        ts("dve", gc[:], pg[:], bg_[:, f:f + 1], 7.0, ALU.add, ALU.min, [bpg, bbg], [bgc])
                        act(sg[:], gc[:], AF.Silu, [bgc], [bsg], scale=1.702)
                        ts("dve", uc[:], pu[:], bg_[:, 8 + f:9 + f], 7.0, ALU.add, ALU.min, [bpu, bbg], [buc])
                        ts("dve", uc[:], uc[:], -7.0, 1.0, ALU.max, ALU.add, [buc], [buc])
                        stt(aT[:, f, :], uc[:], 1.0 / 1.702, sg[:], ALU.mult, ALU.mult, [buc, bsg], (), pr=[baT])
                    for s in range(4):
                        yt, byt = YTp.next()
                        for cb in range(2):
                            pp, bpp = PS[4 + cb]
                            for f in range(8):
                                mm(pp[:], aT[:, f, 128 * s:128 * s + 128], wd[:, f, 512 * cb:512 * cb + 512], f == 0, f == 7, [baT, bwd], bpp)
                            tt("dve", yt[:, 512 * cb:512 * cb + 512], pp[:], bd_[:, 512 * cb:512 * cb + 512], ALU.add, [bpp, bbd], (), pr=[byt])
                        stq("sp", YBUF[r0 + 128 * s:r0 + 128 * s + 128, :], yt[:], byt, dB["YBUF"])

            ph = new_phase()
            YG = Pool("yg", [128, 4, D], BF16, 2, ph)
            for i in p6_tiles:
                t0 = 128 * i
                ms, bms = (MODL, bMODL) if (last or i >= NCT) else (MODC, bMODC)
                yg, byg = YG.next()
                for k in range(4):
                    S.dma("pool", lambda e, yg=yg, i=i, k=k: e.indirect_dma_start(
                        out=yg[:, k, :], out_offset=None, in_=YBUF,
                        in_offset=bass.IndirectOffsetOnAxis(ap=DEST[:, i, k:k + 1], axis=0)),
                        byg.ds, reads=[dB["YBUF"], bDEST], writes=[byg] if k == 0 else (), partial=() if k == 0 else [byg])
                f_, bf_ = WK4.next()
                ts("dve", f_[:], yg[:, 0, :], G4[:, i, 0:1], None, ALU.mult, None, [byg, bG4], [bf_])
                for k in range(1, 4):
                    stt(f_[:], yg[:, k, :], G4[:, i, k:k + 1], f_[:], ALU.mult, ALU.add, [byg, bG4, bf_], [bf_])
                junk, bj = WK4.next()
                ss, bss = SM.next()
                act(junk[:], f_[:], AF.Square, [bf_], [bj, bss], accum_out=ss[:, 0:1])
                rs, brs = rstd_from_ss(ss[:, 0:1], bss, D)
                xt, bx = WK4.next()
                if last:
                    ld("sp", xt[:], XQ[t0:t0 + 128, :], bx, [dB["XQ"]])
                else:
                    ld("sp", xt[:], XR[t0:t0 + 128, :], bx, [dB["XR"]])
                stt(junk[:], f_[:], rs, ms[:, 5, :], ALU.mult, ALU.mult, [bf_, brs, bms], [bj])
                tt("dve", xt[:], xt[:], junk[:], ALU.add, [bx, bj], [bx])
                if last:
                    stq("sp", out[t0:t0 + 128, :], xt[:], bx, dB["out"])
                else:
                    stq("sp", XR[t0:t0 + 128, :], xt[:], bx, dB["XR"])

            S.barrier(list(DS.values())); S.emit(); phase[0].close(); phase[0] = None
            lay.close()
        S.wait_all("sp", list(dB.values()))
        S.emit()
        print("sems used:", S.nsem, "instr:", S.ninstr)
    return nc


def make_consts(S_, L_):
    T = S_ + L_
    GRID_W = 64
    rows = S_ // GRID_W
    row = np.repeat(np.arange(rows, dtype=np.float32), GRID_W)
    col = np.tile(np.arange(GRID_W, dtype=np.float32), rows)
    inv = (10000.0 ** (-np.arange(16, dtype=np.float32) / 16)).astype(np.float32)
    ang_r = row[:, None] * inv
    ang_c = col[:, None] * inv
    cos64 = np.ones((64, T), np.float32)
    sin64 = np.zeros((64, T), np.float32)
    for d in range(64):
        half, j = d // 32, d % 32
        n = j % 16
        ang = (ang_r if half == 0 else ang_c)[:, n]
        cos64[d, L_:] = np.cos(ang)
        sin64[d, L_:] = (-np.sin(ang)) if j < 16 else np.sin(ang)
    ropc = np.concatenate([cos64, cos64], 0)
    rops = np.concatenate([sin64, sin64], 0)
    s = np.arange(128)[:, None]
    t = np.arange(128)[None, :]
    same = (s // 32) == (t // 32)
    UF = (same & (s <= t)).astype(np.float32)
    UB = (same & (s >= t)).astype(np.float32)
    BD = same.astype(np.float32)
    cst_f = np.zeros((128, 8, 128), np.float32)
    cst_f[:, 0] = UF; cst_f[:, 1] = UB; cst_f[:, 2] = BD - UF; cst_f[:, 3] = BD - UB
    cst_f[:, 4] = (s < t).astype(np.float32)
    cst_f[:, 5] = 1.0
    cst_f[:, 6] = np.eye(128, dtype=np.float32)
    cst_f[:, 7, 0:NE] = (np.arange(NE) * CAP).astype(np.float32)[None, :]
    cst_f[:, 7, NE:2 * NE] = (np.arange(NE) * CAP1).astype(np.float32)[None, :]
    cst_b = np.zeros((128, 12, 128), np.float32)
    cst_b[:, 0] = np.eye(128)
    cst_b[:, 1] = (s >= t)
    cst_b[:, 2] = (s <= t)
    cst_b[:, 3] = UF; cst_b[:, 4] = UB
    for c in range(4):
        cst_b[:, 5 + c] = ((t // 32) == c).astype(np.float32) * np.ones((128, 1), np.float32)
    cst_b[0:64, 9, 0] = 1.0
    cst_b[64:128, 9, 1] = 1.0
    rowmask = np.zeros((128, 4), np.float32)
    for c in range(4):
        rowmask[32 * c:32 * c + 32, c] = 1.0
    return dict(ropc=ropc, rops=rops, cst_f=cst_f, cst_b=cst_b.astype(ml_dtypes.bfloat16), rowmask=rowmask)


def rope_perm_cols():
    cols = []
    for (c0, w) in ((C_AQ, 512), (C_AK, 128), (C_BQ, 512), (C_BK, 512)):
        for m in range(w):
            blk, j = m // 32, m % 32
            cols.append(c0 + blk * 32 + (j + 16) % 32)
    return np.array(cols, dtype=np.int64)


def make_in_maps(inputs, S_, L_, n_cores=8):
    f = lambda a: np.ascontiguousarray(np.asarray(a, dtype=np.float32))
    x = f(inputs["x"]); c = f(inputs["c"]); ctx = f(inputs["ctx"]); c_ctx = f(inputs["c_ctx"])
    w_in = f(inputs["w_in"])
    consts = make_consts(S_, L_)
    shared = dict(
        cc_col=np.ascontiguousarray(c_ctx.reshape(8, 128).T),
        w_mod=f(inputs["w_mod"]), b_mod=f(inputs["b_mod"]), norm_g=f(inputs["norm_g"]).reshape(2, 4 * D),
        w_in=w_in, w_inp=np.ascontiguousarray(w_in[:, :, rope_perm_cols()]),
        sink=f(inputs["attn_sink"]), dlam=f(inputs["diff_lambda"]).reshape(2, 256), dnw=f(inputs["diff_norm_w"]),
        lbl=f(inputs["hgrn_lb_logits"]).reshape(2, 1024),
        lbl_fm=np.ascontiguousarray(f(inputs["hgrn_lb_logits"]).reshape(2, 8, 128).transpose(0, 2, 1)),
        hnw=f(inputs["hgrn_norm_w"]), w_br=f(inputs["w_branch"]), w_out=f(inputs["w_out"]),
        w_router=f(inputs["w_router"]), b_router=f(inputs["b_router"]),
        w_gu=f(inputs["w_gate_up"]),
        b_gu_t=np.ascontiguousarray(f(inputs["b_gate_up"]).reshape(2, NE, 16, 128).transpose(0, 1, 3, 2)),
        w_dn=f(inputs["w_down"]), b_dn=f(inputs["b_down"]),
    )
    shared.update(consts)
    maps = []
    B = x.shape[0]
    for core in range(n_cores):
        b = core * B // n_cores
        m = dict(shared)
        m["xin"] = x[b]
        m["ctxin"] = ctx[b]
        m["c_col"] = np.ascontiguousarray(c[b].reshape(8, 128).T)
        cpb = n_cores // B
        SQ = S_ // cpb
        NQT = SQ // 128
        j = core % cpb
        idx = np.zeros((128, NQT + 1), np.int32)
        idx[:, :NQT] = (L_ + j * SQ + 128 * np.arange(NQT)[None, :] + np.arange(128)[:, None]).astype(np.int32)
        m["idxq"] = idx
        maps.append(m)
    return maps


def kernel(**inputs):
    x = np.asarray(inputs["x"])
    B, S_, _ = x.shape
    L_ = np.asarray(inputs["ctx"]).shape[1]
    nc = bass.Bass("TRN2", target_bir_lowering=False)
    build(nc, S_, L_)
    maps = make_in_maps(inputs, S_, L_)
    res = run_bass_kernel_spmd(nc, maps, core_ids=list(range(8)))
    cpb = 8 // B
    outs = [np.concatenate([np.asarray(res.results[b * cpb + j]["out"]) for j in range(cpb)], axis=0) for b in range(B)]
    return np.stack(outs, 0).astype(np.float32)
```

```python
import contextlib
import numpy as np
import ml_dtypes
import concourse.bass as bass
import concourse.mybir as mybir
from concourse.bass_utils import run_bass_kernel_spmd

F32 = mybir.dt.float32
BF16 = mybir.dt.bfloat16
I32 = mybir.dt.int32
AF = mybir.ActivationFunctionType
ALU = mybir.AluOpType
AX = mybir.AxisListType

EPOCH = 30000
DEPOCH = 1800
COMPUTE = ("pe", "act", "dve", "pool")
ALLENG = ("pe", "act", "dve", "pool", "sp")

D = 1024
NE = 32
CAP = 3072
CAP1 = 1024
RMS_EPS = 1e-6


class DSem:
    __slots__ = ("name", "ep", "cnt")

    def __init__(self, name):
        self.name = name
        self.ep = 0
        self.cnt = 0


class Buf:
    __slots__ = ("name", "w", "r", "ds")

    def __init__(self, name, ds=None):
        self.name = name
        self.w = {}
        self.r = {}
        self.ds = ds


class Sched:
    def __init__(self, nc, stack):
        self.nc = nc
        self.stack = stack
        self.streams = {e: [] for e in ALLENG}
        self.cnt = {e: 0 for e in COMPUTE}
        self.waited = {e: {} for e in ALLENG}
        self.sems = {}
        self.nsem = 0

    def sem(self, key):
        s = self.sems.get(key)
        if s is None:
            s = self.stack.enter_context(self.nc.semaphore("s%d" % self.nsem))
            self.nsem += 1
            self.sems[key] = s
        return s

    def _wait(self, eng, key, val):
        if val <= 0:
            return
        if eng == "pe" and key[0] == "pe":
            return
        w = self.waited[eng]
        if w.get(key, 0) >= val:
            return
        w[key] = val
        self.streams[eng].append(("w", key, val))

    def _deps(self, eng, reads, writes, partial):
        for b in reads:
            for k, v in b.w.items():
                self._wait(eng, k, v)
        for b in writes:
            for k, v in b.w.items():
                self._wait(eng, k, v)
            for k, v in b.r.items():
                self._wait(eng, k, v)
        for b in partial:
            for k, v in b.r.items():
                self._wait(eng, k, v)

    def _record(self, tok, reads, writes, partial):
        k, v = tok
        for b in reads:
            if b.r.get(k, 0) < v:
                b.r[k] = v
        for b in writes:
            b.w = {k: v}
            b.r = {}
        for b in partial:
            if b.w.get(k, 0) < v:
                b.w[k] = v

    def op(self, eng, fn, reads=(), writes=(), partial=()):
        self._deps(eng, reads, writes, partial)
        n = self.cnt[eng]
        self.cnt[eng] = n + 1
        key = (eng, n // EPOCH)
        val = n % EPOCH + 1
        self.sem(key)
        self.streams[eng].append(("o", fn, key, 1))
        self._record((key, val), reads, writes, partial)

    def dma(self, eng, fn, ds, reads=(), writes=(), partial=()):
        self._deps(eng, reads, writes, partial)
        key = ("d", ds.name, ds.ep)
        self._wait(eng, key, ds.cnt)
        if ds.cnt >= 16 * DEPOCH:
            ds.ep += 1
            ds.cnt = 0
            key = ("d", ds.name, ds.ep)
        self.sem(key)
        ds.cnt += 16
        self.streams[eng].append(("o", fn, key, 16))
        self._record((key, ds.cnt), reads, writes, partial)

    def barrier(self, dsems):
        toks = []
        for eng in COMPUTE:
            c = self.cnt[eng]
            if c > 0:
                toks.append(((eng, (c - 1) // EPOCH), (c - 1) % EPOCH + 1))
        for ds in dsems:
            if ds.cnt > 0:
                toks.append((("d", ds.name, ds.ep), ds.cnt))
        for e in ALLENG:
            for k, v in toks:
                self._wait(e, k, v)

    def wait_all(self, eng, bufs):
        for b in bufs:
            for k, v in b.w.items():
                self._wait(eng, k, v)
            for k, v in b.r.items():
                self._wait(eng, k, v)

    def emit(self):
        nc = self.nc
        engmap = {"pe": "tensor", "act": "scalar", "dve": "vector", "pool": "gpsimd", "sp": "sync"}
        sems = self.sems
        streams = self.streams
        with nc.Block() as block:
            def mk(ename):
                def body(e):
                    for item in streams[ename]:
                        if item[0] == "w":
                            e.wait_ge(sems[item[1]], item[2])
                        else:
                            item[1](e).then_inc(sems[item[2]], item[3])
                return body
            for ename in ALLENG:
                if streams[ename]:
                    getattr(block, engmap[ename])(mk(ename))
        self.ninstr = getattr(self, "ninstr", 0) + sum(len(v) for v in streams.values())
        self.streams = {e: [] for e in ALLENG}


C_AQ, C_AK, C_AV, C_BQ, C_BK, C_BV, C_CQ, C_CI, C_FF, C_FB, C_CG, C_GT = (
    0, 512, 640, 768, 1280, 1792, 2304, 2816, 3328, 3840, 4352, 4864)
IN_W = 7936
P_AQ, P_AK, P_BQ, P_BK = 0, 512, 640, 1152
ROPE_W = 1664


def build(nc, S_, L_, dbg=(), nlayers=2):
    T = S_ + L_
    NT = T // 128
    NCT = L_ // 128
    NLT = S_ // 128
    tok_blocks = [(0, L_)] + [(L_ + 512 * i, 512) for i in range(S_ // 512)]
    NSLOT = NE * CAP

    def din(name, shape, dt=F32):
        return nc.dram_tensor(name, list(shape), dt, kind="ExternalInput").ap()

    def dscr(name, shape, dt):
        kind = "ExternalOutput" if name in dbg else "Internal"
        return nc.dram_tensor(name, list(shape), dt, kind=kind).ap()

    xin = din("xin", [S_, D]); ctxin = din("ctxin", [L_, D])
    c_col = din("c_col", [128, 8]); cc_col = din("cc_col", [128, 8])
    w_mod = din("w_mod", [2, D, 6 * D]); b_mod = din("b_mod", [2, 6 * D]); norm_g = din("norm_g", [2, 4 * D])
    w_in = din("w_in", [2, D, IN_W]); w_inp = din("w_inp", [2, D, ROPE_W])
    sink_in = din("sink", [2, 8]); dlam = din("dlam", [2, 256]); dnw = din("dnw", [2, 128])
    lbl = din("lbl", [2, 1024]); lbl_fm = din("lbl_fm", [2, 128, 8]); hnw = din("hnw", [2, 128])
    w_br = din("w_br", [2, 3, 512, D]); w_out = din("w_out", [2, D, D])
    w_router = din("w_router", [2, D, NE]); b_router = din("b_router", [2, NE])
    w_gu = din("w_gu", [2, NE, D, 2 * D]); b_gu_t = din("b_gu_t", [2, NE, 128, 16])
    w_dn = din("w_dn", [2, NE, D, D]); b_dn = din("b_dn", [2, NE, D])
    ropc = din("ropc", [128, T]); rops = din("rops", [128, T])
    cst_f = din("cst_f", [128, 8, 128])
    cst_b = din("cst_b", [128, 12, 128], BF16)
    rowmask_in = din("rowmask", [128, 4])
    SQ = S_ // 4
    NQT = SQ // 128
    idxq_in = din("idxq", [128, NQT + 1], I32)
    out = nc.dram_tensor("out", [SQ, D], F32, kind="ExternalOutput").ap()
    XQ = dscr("XQ", [SQ, D], F32)

    XR = dscr("XR", [T, D], F32)
    HT = dscr("HT", [8, 128, T], BF16)
    QA = dscr("QA", [8, 64, T], BF16); QAN = dscr("QAN", [8, T], F32)
    KA = dscr("KA", [2, 64, T], BF16); KAN = dscr("KAN", [2, T], F32)
    VA = dscr("VA", [T, 128], BF16)
    QB = dscr("QB", [8, 64, T], BF16); QBN = dscr("QBN", [8, T], F32)
    KB = dscr("KB", [8, 64, T], BF16); KBN = dscr("KBN", [8, T], F32)
    VB = dscr("VB", [T, 512], BF16)
    KMX = dscr("KMX", [10, 1], F32)
    HQ = dscr("HQ", [4, 128, T], BF16); HK = dscr("HK", [8, 128, T], BF16)
    HKt = dscr("HKt", [T, 1024], BF16); HG = dscr("HG", [T, 1024], F32)
    HV = dscr("HV", [T, 512], BF16); HGT = dscr("HGT", [T, 512], BF16)
    GT = dscr("GT", [T, 3072], BF16)
    OA = dscr("OA", [T, 512], BF16); OB = dscr("OB", [T, 512], BF16); OC = dscr("OC", [T, 512], BF16)
    OF = dscr("OF", [T, 512], F32)
    XBUF = dscr("XBUF", [NSLOT, D], BF16); YBUF = dscr("YBUF", [NSLOT, D], BF16)

    with contextlib.ExitStack() as st:
        S = Sched(nc, st)
        DS = {}
        uniq = [0]

        def SB(name, shape, dt, stack=None):
            uniq[0] += 1
            t = (stack or st).enter_context(nc.sbuf_tensor("%s_%d" % (name, uniq[0]), list(shape), dt))
            ds = DS.get(name)
            if ds is None:
                ds = DS[name] = DSem(name)
            return t, Buf(name, ds)

        class Pool:
            def __init__(self, name, shape, dt, n, stack=None):
                self.items = [SB("%s%d" % (name, i), shape, dt, stack) for i in range(n)]
                self.i = 0

            def next(self):
                it = self.items[self.i % len(self.items)]
                self.i += 1
                return it

        dB = {n: Buf(n) for n in ["XR", "HT", "QA", "QAN", "KA", "KAN", "VA", "QB", "QBN", "KB", "KBN", "VB", "KMX",
                                  "HQ", "HK", "HKt", "HG", "HV", "HGT", "GT", "OA", "OB", "OC", "OF", "XBUF", "YBUF", "out", "XQ"]}

        def ld(eng, dst, src, dbuf, rd=()):
            S.dma(eng, lambda e: e.dma_start(out=dst, in_=src), dbuf.ds, reads=rd, writes=[dbuf])

        def stq(eng, dst, src, sbuf, dram):
            S.dma("act", lambda e: e.dma_start(out=dst, in_=src), sbuf.ds, reads=[sbuf], partial=[dram])

        def mm(o, lhsT, rhs, first, last, rd, pb):
            S.op("pe", lambda e: e.matmul(o, lhsT=lhsT, rhs=rhs, start=first, stop=last), reads=rd,
                 writes=[pb] if first else (), partial=() if first else [pb])

        def mmp(o, lhsT, rhs, first, last, rd, pb):
            S.op("pe", lambda e: e.matmul(o, lhsT=lhsT, rhs=rhs, start=first, stop=last), reads=rd, partial=[pb])

        def tr(o, i_, idt, rd, pb, first):
            S.op("pe", lambda e: e.transpose(out=o, in_=i_, identity=idt), reads=rd,
                 writes=[pb] if first else (), partial=() if first else [pb])

        def act(o, i_, func, rd, wr, eng="act", **kw):
            S.op(eng, lambda e: e.activation(out=o, in_=i_, func=func, **kw), reads=rd, writes=wr)

        def tt(eng, o, a, b, op, rd, wr, pr=()):
            S.op(eng, lambda e: e.tensor_tensor(out=o, in0=a, in1=b, op=op), reads=rd, writes=wr, partial=pr)

        def ts(eng, o, a, s1, s2, op0, op1, rd, wr, pr=(), **kw):
            if s2 is None:
                S.op(eng, lambda e: e.tensor_scalar(out=o, in0=a, scalar1=s1, scalar2=None, op0=op0, **kw), reads=rd, writes=wr, partial=pr)
            else:
                S.op(eng, lambda e: e.tensor_scalar(out=o, in0=a, scalar1=s1, scalar2=s2, op0=op0, op1=op1, **kw), reads=rd, writes=wr, partial=pr)

        def stt(o, a, s, b, op0, op1, rd, wr, pr=()):
            S.op("dve", lambda e: e.scalar_tensor_tensor(out=o, in0=a, scalar=s, in1=b, op0=op0, op1=op1), reads=rd, writes=wr, partial=pr)

        def cp(eng, o, i_, rd, wr, pr=()):
            if eng == "act":
                S.op("act", lambda e: e.copy(out=o, in_=i_), reads=rd, writes=wr, partial=pr)
            else:
                S.op(eng, lambda e: e.tensor_copy(out=o, in_=i_), reads=rd, writes=wr, partial=pr)

        def memset(eng, o, v, wr):
            S.op(eng, lambda e: e.memset(o, v), writes=wr)

        CF, bCF = SB("CF", [128, 8, 128], F32)
        CB, bCB = SB("CB", [128, 12, 128], BF16)
        RM, bRM = SB("RM", [128, 4], F32)
        ld("sp", CF[:], cst_f, bCF); ld("sp", CB[:], cst_b, bCB); ld("sp", RM[:], rowmask_in, bRM)
        IDXQ, bIDXQ = SB("IDXQ", [128, NQT + 1], I32)
        ld("sp", IDXQ[:], idxq_in, bIDXQ)
        UF_f, UB_f, DF_f, DB_f, UTS_f, ONES_f, ID_f, EBASE = [CF[:, i, :] for i in range(8)]
        ID_b, MPREV, MNEXT, UF_b, UB_b = [CB[:, i, :] for i in range(5)]
        BDC = CB[:, 5:9, :]
        BONES = CB[:, 9, 0:2]

        PS = [(st.enter_context(nc.psum_tensor("ps%d" % i, [128, 512], F32)), Buf("ps%d" % i)) for i in range(8)]

        MODL, bMODL = SB("MODL", [128, 6, D], F32)
        MODC, bMODC = SB("MODC", [128, 6, D], F32)
        eps_t, beps = SB("eps_t", [128, 1], F32)
        memset("pool", eps_t[:], RMS_EPS, [beps])

        WK4 = Pool("wk4", [128, D], F32, 4)
        WKB = Pool("wkb", [128, D], BF16, 4)
        SM = Pool("sm", [128, 16], F32, 8)
        phase = [None]

        def new_phase():
            if phase[0] is not None:
                S.barrier(list(DS.values()))
                S.emit()
                phase[0].close()
            phase[0] = contextlib.ExitStack()
            return phase[0]

        def rstd_from_ss(ss_ap, bss, n):
            t, b = SM.next()
            S.op("act", lambda e: e.activation(out=t[:, 0:1], in_=ss_ap, func=AF.Sqrt, bias=eps_t[:, 0:1], scale=1.0 / n),
                 reads=[bss, beps], writes=[b])
            t2, b2 = SM.next()
            S.op("dve", lambda e: e.reciprocal(out=t2[:, 0:1], in_=t[:, 0:1]), reads=[b], writes=[b2])
            return t2[:, 0:1], b2

        def norm_mod_T(xt, bx, ms, bms, gs, shs, t0, keep=None, do_T=True):
            junk, bj = WK4.next()
            ss, bss = SM.next()
            act(junk[:], xt[:], AF.Square, [bx], [bj, bss], accum_out=ss[:, 0:1])
            rs, brs = rstd_from_ss(ss[:, 0:1], bss, D)
            tmp, btmp = WK4.next()
            stt(tmp[:], xt[:], rs, ms[:, gs, :], ALU.mult, ALU.mult, [bx, brs, bms], [btmp])
            hb, bhb = WKB.next()
            tt("pool", hb[:], tmp[:], ms[:, shs, :], ALU.add, [btmp, bms], [bhb])
            if keep is not None:
                tt("dve", keep[0][:], tmp[:], ms[:, shs, :], ALU.add, [btmp, bms], [keep[1]])
            if not do_T:
                return hb, bhb
            pt, bpt = PS[7]
            ptb = pt[:].bitcast(BF16)
            for kc in range(8):
                tr(ptb[:, kc * 128:(kc + 1) * 128], hb[:, kc * 128:(kc + 1) * 128], ID_b, [bhb, bCB], bpt, kc == 0)
            hT, bhT = WKB.next()
            cp("act", hT[:], ptb, [bpt], [bhT])
            stq("sp", HT[:, :, t0:t0 + 128].rearrange("k p t -> p k t"), hT[:].rearrange("p (k t) -> p k t", k=8), bhT, dB["HT"])
            return hb, bhb

        for l in range(nlayers):
            lam_init = 0.8 - 0.6 * float(np.exp(-0.3 * l))
            last = (l == nlayers - 1)
            CAPl = CAP1 if last else CAP
            NSL = NE * CAPl
            EB = CF[:, 7, NE:2 * NE] if last else CF[:, 7, 0:NE]
            p6_tiles = list(range(NQT)) if last else list(range(NT))
            if phase[0] is not None:
                S.barrier(list(DS.values())); S.emit(); phase[0].close(); phase[0] = None
            lay = contextlib.ExitStack()
            LBT, bLBT = SB("LBT", [128, 1024], F32, lay)
            OMT, bOMT = SB("OMT", [128, 1024], F32, lay)
            LBF, bLBF = SB("LBF", [128, 8], F32, lay)
            OMF, bOMF = SB("OMF", [128, 8], F32, lay)
            DL, bDL = SB("DL", [128, 256], F32, lay)
            LV, bLV = SB("LV", [128, 8], F32, lay)
            DNW, bDNW = SB("DNW", [128, 128], F32, lay)
            HNW, bHNW = SB("HNW", [128, 128], F32, lay)
            SK, bSK = SB("SK", [128, 8], F32, lay)
            KMB, bKMB = SB("KMB", [128, 10], F32, lay)
            NKM, bNKM = SB("NKM", [128, 10], F32, lay)
            NKM8, bNKM8 = SB("NKM8", [128, 10], F32, lay)
            RS_, bRS = SB("RS", [128, NE], F32, lay)
            DEST, bDEST = SB("DEST", [128, NT + 1, 4], I32, lay)
            G4, bG4 = SB("G4", [128, NT, 4], F32, lay)
            ph = new_phase()
            W16a, bW16a = SB("W16a", [128, 8, 512], F32, ph)
            W16b, bW16b = SB("W16b", [128, 8, 512], F32, ph)
            cs, bcs = SB("cs", [128, 16], F32, ph)
            ld("sp", cs[:, 0:8], c_col, bcs); ld("sp", cs[:, 8:16], cc_col, bcs)
            act(cs[:], cs[:], AF.Silu, [bcs], [bcs])
            LH, bLH = SB("LH", [128, 16, 128], F32, ph)
            for j in range(16):
                cp("pool", LH[:, j, :], cs[:, j:j + 1].to_broadcast([128, 128]), [bcs], (), pr=[bLH])
            NG, bNG = W16b, bW16b
            for cb in range(12):
                ld("sp", W16a[:], w_mod[l, :, cb * 512:(cb + 1) * 512].rearrange("(k p) n -> p k n", p=128), bW16a)
                bm, bbm = WK4.next()
                ld("sp", bm[:, 0:512], b_mod[l, cb * 512:(cb + 1) * 512].partition_broadcast(128), bbm)
                for si, (ms, bms) in enumerate(((MODL, bMODL), (MODC, bMODC))):
                    pp, bpp = PS[si]
                    for kc in range(8):
                        mm(pp[:], LH[:, si * 8 + kc, :], W16a[:, kc, :], kc == 0, kc == 7, [bLH, bW16a], bpp)
                    tt("dve", ms[:, cb // 2, (cb % 2) * 512:(cb % 2) * 512 + 512], pp[:], bm[:, 0:512], ALU.add, [bpp, bbm], (), pr=[bms])
            ld("sp", NG[:].rearrange("p k n -> p (k n)"), norm_g[l].partition_broadcast(128), bNG)
            NGv = NG[:].rearrange("p k n -> p (k n)")
            for ms, bms in ((MODL, bMODL), (MODC, bMODC)):
                stt(ms[:, 1, :], ms[:, 1, :], 1.0, NGv[:, 0:D], ALU.add, ALU.mult, [bms, bNG], [bms])
                tt("dve", ms[:, 2, :], ms[:, 2, :], NGv[:, D:2 * D], ALU.mult, [bms, bNG], [bms])
                stt(ms[:, 4, :], ms[:, 4, :], 1.0, NGv[:, 2 * D:3 * D], ALU.add, ALU.mult, [bms, bNG], [bms])
                tt("dve", ms[:, 5, :], ms[:, 5, :], NGv[:, 3 * D:4 * D], ALU.mult, [bms, bNG], [bms])

            if l == 0:
                memset("pool", LBT[:], 0.0, [bLBT]); memset("pool", OMT[:], 1.0, [bOMT])
                memset("pool", LBF[:], 0.0, [bLBF]); memset("pool", OMF[:], 1.0, [bOMF])
            else:
                t1, b1 = WK4.next()
                ld("sp", LBT[:], lbl[1].partition_broadcast(128), bLBT)
                ld("sp", t1[:], lbl[0].partition_broadcast(128), b1)
                tt("dve", LBT[:], LBT[:], t1[:], ALU.subtract, [bLBT, b1], [bLBT])
                act(LBT[:], LBT[:], AF.Sigmoid, [bLBT], [bLBT])
                ts("dve", OMT[:], LBT[:], -1.0, 1.0, ALU.mult, ALU.add, [bLBT], [bOMT])
                t2, b2 = SM.next()
                ld("sp", LBF[:], lbl_fm[1], bLBF); ld("sp", t2[:, 0:8], lbl_fm[0], b2)
                tt("dve", LBF[:], LBF[:], t2[:, 0:8], ALU.subtract, [bLBF, b2], [bLBF])
                act(LBF[:], LBF[:], AF.Sigmoid, [bLBF], [bLBF])
                ts("dve", OMF[:], LBF[:], -1.0, 1.0, ALU.mult, ALU.add, [bLBF], [bOMF])
            ld("sp", DL[:], dlam[l].partition_broadcast(128), bDL)
            tt("dve", DL[:, 0:64], DL[:, 0:64], DL[:, 64:128], ALU.mult, [bDL], [bDL])
            tt("dve", DL[:, 128:192], DL[:, 128:192], DL[:, 192:256], ALU.mult, [bDL], [bDL])
            S.op("dve", lambda e: e.reduce_sum(out=LV[:, 0:1], in_=DL[:, 0:64], axis=AX.X), reads=[bDL], writes=[bLV])
            S.op("dve", lambda e: e.reduce_sum(out=LV[:, 1:2], in_=DL[:, 128:192], axis=AX.X), reads=[bDL], partial=[bLV])
            act(LV[:, 0:2], LV[:, 0:2], AF.Exp, [bLV], [bLV])
            tt("dve", LV[:, 2:3], LV[:, 1:2], LV[:, 0:1], ALU.subtract, [bLV], [bLV])
            ts("dve", LV[:, 3:4], LV[:, 2:3], -lam_init, None, ALU.add, None, [bLV], [bLV])
            NEGLAM = LV[:, 3:4]
            ld("sp", DNW[:], dnw[l].partition_broadcast(128), bDNW)
            ts("dve", DNW[:], DNW[:], 1.0 - lam_init, None, ALU.mult, None, [bDNW], [bDNW])
            ld("sp", HNW[:], hnw[l].partition_broadcast(128), bHNW)
            ld("sp", SK[:], sink_in[l].partition_broadcast(128), bSK)

            ph = new_phase()
            if l == 0:
                S.dma("sp", lambda e: e.dma_start(out=XR[0:L_, :], in_=ctxin), bCF.ds, partial=[dB["XR"]])
                for r0 in range(0, S_, 512):
                    S.dma("sp", lambda e, r0=r0: e.dma_start(out=XR[L_ + r0:L_ + r0 + 512, :], in_=xin[r0:r0 + 512, :]), bCB.ds, partial=[dB["XR"]])
            for i in range(NT):
                t0 = 128 * i
                ms, bms = (MODC, bMODC) if i < NCT else (MODL, bMODL)
                xt, bx = WK4.next()
                ld("sp", xt[:], XR[t0:t0 + 128, :], bx, [dB["XR"]])
                norm_mod_T(xt, bx, ms, bms, 1, 0, t0)

            ph = new_phase()
            WB8 = Pool("wb8", [128, 8, 512], BF16, 2, ph)
            HB = Pool("hb", [128, 8, 512], BF16, 3, ph)
            fm_jobs = []
            for i in range(4):
                fm_jobs.append((C_AQ + 128 * i, P_AQ + 128 * i, "rope", (QA, QAN, "QA", "QAN", 2 * i)))
            fm_jobs.append((C_AK, P_AK, "rope", (KA, KAN, "KA", "KAN", 0)))
            for i in range(4):
                fm_jobs.append((C_BQ + 128 * i, P_BQ + 128 * i, "rope", (QB, QBN, "QB", "QBN", 2 * i)))
            for i in range(4):
                fm_jobs.append((C_BK + 128 * i, P_BK + 128 * i, "rope", (KB, KBN, "KB", "KBN", 2 * i)))
            for i in range(4):
                fm_jobs.append((C_CQ + 128 * i, None, "silu", i))
            for i in range(4):
                fm_jobs.append((C_FF + 128 * i, None, "kfm", i))
            for i in range(4):
                fm_jobs.append((C_FB + 128 * i, None, "kfm", 4 + i))
            for (c0, p0, kind, info) in fm_jobs:
                wt, bwt = WB8.next()
                ld("pool", wt[:, :, 0:128], w_in[l, :, c0:c0 + 128].rearrange("(k p) n -> p k n", p=128), bwt)
                if kind == "rope":
                    ld("pool", wt[:, :, 128:256], w_inp[l, :, p0:p0 + 128].rearrange("(k p) n -> p k n", p=128), bwt)
                for (t0, n) in tok_blocks:
                    hb, bhb = HB.next()
                    ld("sp", hb[:, :, 0:n], HT[:, :, t0:t0 + n].rearrange("k p t -> p k t"), bhb, [dB["HT"]])
                    pa, bpa = PS[0]
                    for kc in range(8):
                        mm(pa[:, 0:n], wt[:, kc, 0:128], hb[:, kc, 0:n], kc == 0, kc == 7, [bwt, bhb], bpa)
                    if kind == "rope":
                        Qd, Nd, qn, nn, h0 = info
                        pb_, bpb = PS[1]
                        for kc in range(8):
                            mm(pb_[:, 0:n], wt[:, kc, 128:256], hb[:, kc, 0:n], kc == 0, kc == 7, [bwt, bhb], bpb)
                        rc, brc = WK4.next()
                        ld("sp", rc[:, 0:n], ropc[:, t0:t0 + n], brc); ld("sp", rc[:, 512:512 + n], rops[:, t0:t0 + n], brc)
                        t1, b1 = WK4.next()
                        tt("dve", t1[:, 0:n], pa[:, 0:n], rc[:, 0:n], ALU.mult, [bpa, brc], [b1])
                        tt("dve", t1[:, 512:512 + n], pb_[:, 0:n], rc[:, 512:512 + n], ALU.mult, [bpb, brc], (), pr=[b1])
                        qt, bqt = WKB.next()
                        tt("pool", qt[:, 0:n], t1[:, 0:n], t1[:, 512:512 + n], ALU.add, [b1], [bqt])
                        sq, bsq = WKB.next()
                        act(sq[:, 0:n], qt[:, 0:n], AF.Square, [bqt], [bsq])
                        pn, bpn = PS[2]
                        mm(pn[0:2, 0:n], BONES, sq[:, 0:n], True, True, [bCB, bsq], bpn)
                        nr, bnr = WK4.next()
                        act(nr[0:2, 0:n], pn[0:2, 0:n], AF.Sqrt, [bpn], [bnr])
                        stq("sp", Nd[h0:h0 + 2, t0:t0 + n], nr[0:2, 0:n], bnr, dB[nn])
                        stq("sp", Qd[h0, :, t0:t0 + n], qt[0:64, 0:n], bqt, dB[qn])
                        stq("sp", Qd[h0 + 1, :, t0:t0 + n], qt[64:128, 0:n], bqt, dB[qn])
                    elif kind == "silu":
                        qt, bqt = WKB.next()
                        act(qt[:, 0:n], pa[:, 0:n], AF.Silu, [bpa], [bqt])
                        stq("sp", HQ[info, :, t0:t0 + n], qt[:, 0:n], bqt, dB["HQ"])
                    else:
                        t1, b1 = WK4.next()
                        act(t1[:, 0:n], pa[:, 0:n], AF.Sigmoid, [bpa], [b1], scale=-1.0)
                        qt, bqt = WKB.next()
                        ts("dve", qt[:, 0:n], t1[:, 0:n], OMF[:, info:info + 1], None, ALU.mult, None, [b1, bOMF], [bqt])
                        stq("sp", HK[info, :, t0:t0 + n], qt[:, 0:n], bqt, dB["HK"])

            tm_jobs = [(C_AV, 128, "copy", (VA, "VA", 0)), (C_BV, 512, "copy", (VB, "VB", 0)), (C_CI, 512, "copy", (HV, "HV", 0)),
                       (C_CG, 512, "silu", (HGT, "HGT", 0)), (C_FF, 512, "logf", 0), (C_FB, 512, "logf", 1)]
            for j in range(6):
                tm_jobs.append((C_GT + 512 * j, 512, "sigm", (GT, "GT", 512 * j)))
            for (c0, w, kind, info) in tm_jobs:
                wt, bwt = WB8.next()
                ld("pool", wt[:, :, 0:w], w_in[l, :, c0:c0 + w].rearrange("(k p) n -> p k n", p=128), bwt)
                for (t0, n) in tok_blocks:
                    hb, bhb = HB.next()
                    ld("sp", hb[:, :, 0:n], HT[:, :, t0:t0 + n].rearrange("k p t -> p k t"), bhb, [dB["HT"]])
                    for s in range(n // 128):
                        tt0 = t0 + 128 * s
                        pa, bpa = PS[s % 2]
                        for kc in range(8):
                            mm(pa[:, 0:w], hb[:, kc, 128 * s:128 * s + 128], wt[:, kc, 0:w], kc == 0, kc == 7, [bwt, bhb], bpa)
                        if kind in ("copy", "silu", "sigm"):
                            dst, dn, co = info
                            ot, bot = WKB.next()
                            fn = {"copy": AF.Copy, "silu": AF.Silu, "sigm": AF.Sigmoid}[kind]
                            act(ot[:, 0:w], pa[:, 0:w], fn, [bpa], [bot])
                            stq("sp", dst[tt0:tt0 + 128, co:co + w], ot[:, 0:w], bot, dB[dn])
                        else:
                            d_ = info
                            t1, b1 = WK4.next()
                            act(t1[:, 0:512], pa[:, 0:512], AF.Sigmoid, [bpa], [b1])
                            tt("dve", t1[:, 0:512], t1[:, 0:512], OMT[:, d_ * 512:(d_ + 1) * 512], ALU.mult, [b1, bOMT], [b1])
                            tt("dve", t1[:, 0:512], t1[:, 0:512], LBT[:, d_ * 512:(d_ + 1) * 512], ALU.add, [b1, bLBT], [b1])
                            kt, bkt = WKB.next()
                            ts("dve", kt[:, 0:512], t1[:, 0:512], -1.0, 1.0, ALU.mult, ALU.add, [b1], [bkt])
                            stq("sp", HKt[tt0:tt0 + 128, d_ * 512:(d_ + 1) * 512], kt[:, 0:512], bkt, dB["HKt"])
                            act(t1[:, 512:1024], t1[:, 0:512], AF.Ln, [b1], [b1])
                            stq("sp", HG[tt0:tt0 + 128, d_ * 512:(d_ + 1) * 512], t1[:, 512:1024], b1, dB["HG"])

            ph = new_phase()
            kmrow, bkmrow = SB("kmrow", [1, T], F32, ph)
            kmv, bkmv = SB("kmv", [1, 16], F32, ph)
            for ku in range(10):
                src = KAN[ku:ku + 1, :] if ku < 2 else KBN[ku - 2:ku - 1, :]
                ld("sp", kmrow[:], src, bkmrow, [dB["KAN"], dB["KBN"]])
                S.op("dve", lambda e, ku=ku: e.reduce_max(out=kmv[:, ku:ku + 1], in_=kmrow[:], axis=AX.X), reads=[bkmrow], partial=[bkmv])
            stq("sp", KMX.rearrange("a b -> b a"), kmv[:, 0:10], bkmv, dB["KMX"])
            ld("sp", KMB[:], KMX.rearrange("a b -> (a b)").partition_broadcast(128), bKMB, [dB["KMX"]])
            ts("dve", NKM[:], KMB[:], -1.0, None, ALU.mult, None, [bKMB], [bNKM])
            ts("dve", NKM8[:], KMB[:], -0.125, None, ALU.mult, None, [bKMB], [bNKM8])

            ph = new_phase()
            KAUG = [SB("kaug%d" % i, [65, T], BF16, ph) for i in range(2)]
            VS, bVS = SB("vs", [128, NT, 132], BF16, ph)
            memset("pool", VS[:], 1.0, [bVS])
            QAUG = Pool("qaug", [65, 512], BF16, 2, ph)
            QNR = Pool("qnr", [65, 512], F32, 2, ph)
            PT = Pool("pt", [128, 512], BF16, 4, ph)
            OT = Pool("ot", [128, 512], F32, 2, ph)
            OT2 = Pool("ot2", [128, 512], F32, 2, ph)
            for i in range(2):
                memset("pool", KAUG[i][0][64:65, :], 1.0, [KAUG[i][1]])

            def load_k(slot, Kd, kname, ku):
                kt_, bk_ = KAUG[slot]
                S.dma("sp", lambda e: e.dma_start(out=kt_[0:64, :], in_=Kd[ku]), bk_.ds, reads=[dB[kname]], writes=[bk_])

            def load_v(Vd, vname, v0, dv):
                for j0 in range(0, NT, 11):
                    j1 = min(NT, j0 + 11)
                    S.dma("sp", lambda e, j0=j0, j1=j1: e.dma_start(out=VS[:, j0:j1, 0:dv], in_=Vd[128 * j0:128 * j1, v0:v0 + dv].rearrange("(j p) d -> p j d", p=128)),
                          bVS.ds, reads=[dB[vname]], writes=[bVS] if j0 == 0 else (), partial=() if j0 == 0 else [bVS])
                memset("pool", VS[:, :, dv:dv + 1], 1.0, [bVS])

            def attn_unit(Qd, Nd, qname, nname, u, kslot, kmcol, t0, n, keys, dv, accs):
                kt_, bk_ = KAUG[kslot]
                qa, bqa = QAUG.next()
                ld("sp", qa[0:64, 0:n], Qd[u, :, t0:t0 + n], bqa, [dB[qname]])
                qn, bqn = QNR.next()
                ld("sp", qn[64:65, 0:n], Nd[u:u + 1, t0:t0 + n], bqn, [dB[nname]])
                S.op("act", lambda e: e.activation(out=qa[64:65, 0:n], in_=qn[64:65, 0:n], func=AF.Copy, scale=NKM[64:65, kmcol:kmcol + 1]),
                     reads=[bqn, bNKM], partial=[bqa])
                nk = len(keys)

                def issue_st(ki):
                    pS, bpS = PS[ki % 2]
                    j = keys[ki][0]
                    mm(pS[:, 0:n], kt_[:, 128 * j:128 * j + 128], qa[:, 0:n], True, True, [bk_, bqa], bpS)
                    return pS, bpS

                cur_st = issue_st(0)
                for ki, (j, mask) in enumerate(keys):
                    pS, bpS = cur_st
                    if ki + 1 < nk:
                        cur_st = issue_st(ki + 1)
                    pt_, bpt_ = PT.next()
                    act(pt_[:, 0:n], pS[:, 0:n], AF.Exp, [bpS], [bpt_], scale=0.125)
                    if mask is not None:
                        tt("pool", pt_[:, 0:n], pt_[:, 0:n], mask, ALU.mult, [bpt_, bCB], [bpt_])
                    for s in range(n // 128):
                        ao, ab = accs[s]
                        mmp(ao, pt_[:, 128 * s:128 * s + 128], VS[:, j, 0:dv + 1], ki == 0, ki == nk - 1, [bpt_, bVS], ab)

            def acc_views(dv, banks):
                res = []
                for s in range(4):
                    p, b = PS[banks[s // 2]]
                    res.append((p[:, (s % 2) * 256:(s % 2) * 256 + dv + 1], b))
                return res

            for ku in range(2):
                load_k(0, KA, "KA", ku)
                load_v(VA, "VA", 64 * ku, 64)
                for g in range(4):
                    hq = 4 * ku + g
                    for i in range(NT):
                        t0 = 128 * i
                        if i < NCT:
                            keys = [(j, None) for j in range(NCT)]
                        else:
                            keys = [(j, None) for j in range(NCT)]
                            if i > NCT:
                                keys.append((i - 1, MPREV))
                            keys.append((i, None))
                            if i < NT - 1:
                                keys.append((i + 1, MNEXT))
                        accs = acc_views(64, (2 + (i % 2) * 2, 3 + (i % 2) * 2))
                        pass
                        attn_unit(QA, QAN, "QA", "QAN", hq, 0, ku, t0, 128, keys, 64, accs)
                        qn, bqn = SM.next()
                        S.dma("sp", lambda e, qn=qn, t0=t0, hq=hq: e.dma_start(out=qn[:, 0:1], in_=QAN[hq, t0:t0 + 128].rearrange("(p o) -> p o", o=1)),
                              bqn.ds, reads=[dB["QAN"]], writes=[bqn])
                        es, bes = SM.next()
                        S.op("act", lambda e, es=es, qn=qn, ku=ku, hq=hq: e.activation(out=es[:, 0:1], in_=qn[:, 0:1], func=AF.Exp,
                                                                                        scale=NKM8[:, ku:ku + 1], bias=SK[:, hq:hq + 1]),
                             reads=[bqn, bNKM8, bSK], writes=[bes])
                        ao, ab = accs[0]
                        tt("dve", es[:, 1:2], es[:, 0:1], ao[:, 64:65], ALU.add, [bes, ab], [bes])
                        S.op("dve", lambda e, es=es: e.reciprocal(out=es[:, 2:3], in_=es[:, 1:2]), reads=[bes], writes=[bes])
                        o_, bo_ = WKB.next()
                        ts("dve", o_[:, 0:64], ao[:, 0:64], es[:, 2:3], None, ALU.mult, None, [ab, bes], [bo_])
                        stq("sp", OA[t0:t0 + 128, 64 * hq:64 * hq + 64], o_[:, 0:64], bo_, dB["OA"])

            for h in range(4):
                load_k(0, KB, "KB", 2 * h); load_k(1, KB, "KB", 2 * h + 1)
                load_v(VB, "VB", 128 * h, 128)
                for bi, (t0, n) in enumerate(tok_blocks):
                    keys = [(j, None) for j in range(NCT)] if t0 < L_ else [(j, None) for j in range(NT)]
                    O0, bO0 = OT.next()
                    for c in range(2):
                        accs = [(PS[2 + s_][0][:, 0:129], PS[2 + s_][1]) for s_ in range(4)]
                        attn_unit(QB, QBN, "QB", "QBN", 2 * h + c, c, 2 + 2 * h + c, t0, n, keys, 128, accs)
                        for s in range(n // 128):
                            a_, b_ = accs[s]
                            r, br = SM.next()
                            S.op("dve", lambda e, r=r, a_=a_: e.reciprocal(out=r[:, 0:1], in_=a_[:, 128:129]), reads=[b_], writes=[br])
                            if c == 0:
                                ts("dve", O0[:, 128 * s:128 * s + 128], a_[:, 0:128], r[:, 0:1], None, ALU.mult, None, [b_, br], (), pr=[bO0])
                            else:
                                tt("dve", r[:, 1:2], r[:, 0:1], NEGLAM, ALU.mult, [br, bLV], [br])
                                o0, bo0 = OT2.next()
                                stt(o0[:, 128:256], a_[:, 0:128], r[:, 1:2], O0[:, 128 * s:128 * s + 128], ALU.mult, ALU.add, [b_, br, bO0], [bo0])
                                ss, bss = SM.next()
                                act(o0[:, 256:384], o0[:, 128:256], AF.Square, [bo0], [bo0, bss], accum_out=ss[:, 0:1])
                                rs, brs = rstd_from_ss(ss[:, 0:1], bss, 128)
                                ob_, bob = WKB.next()
                                stt(ob_[:, 0:128], o0[:, 128:256], rs, DNW[:], ALU.mult, ALU.mult, [bo0, brs, bDNW], [bob])
                                tt0 = t0 + 128 * s
                                stq("sp", OB[tt0:tt0 + 128, 128 * h:128 * h + 128], ob_[:, 0:128], bob, dB["OB"])

            ph = new_phase()
            SF = [SB("sf%d" % h, [128, 128], F32, ph) for h in range(4)]
            SBF = [Pool("sbf%d_" % h, [128, 128], BF16, 2, ph) for h in range(4)]
            GP = Pool("gp", [128, 512], F32, 2, ph)
            QTp = Pool("qtp", [128, 512], BF16, 2, ph)
            KTp = Pool("ktp", [128, 512], BF16, 2, ph)
            KKp = Pool("kkp", [128, 512], BF16, 2, ph)
            VVp = Pool("vvp", [128, 512], BF16, 2, ph)
            E1p = Pool("e1p", [128, 512], F32, 2, ph)
            E2p = Pool("e2p", [128, 512], F32, 2, ph)
            QSp = Pool("qsp", [128, 512], BF16, 2, ph)
            KSp = Pool("ksp", [128, 512], BF16, 2, ph)
            KHp = Pool("khp", [128, 512], BF16, 2, ph)
            KHCp = Pool("khcp", [128, 4, 512], BF16, 2, ph)
            QSCp = Pool("qscp", [128, 4, 128], BF16, 8, ph)
            ATp = Pool("atp", [128, 128], BF16, 4, ph)
            OOp = Pool("oop", [128, 512], F32, 2, ph)
            for d_ in range(2):
                Um_f = UF_f if d_ == 0 else UB_f
                Dm_f = DF_f if d_ == 0 else DB_f
                Um_b = UF_b if d_ == 0 else UB_b
                if d_ == 0:
                    order = list(range(NT))
                else:
                    order = list(range(NCT - 1, -1, -1)) + list(range(NT - 1, NCT - 1, -1))
                corder = [0, 1, 2, 3] if d_ == 0 else [3, 2, 1, 0]
                cur = []
                for h in range(4):
                    memset("pool", SF[h][0][:], 0.0, [SF[h][1]])
                    sb_, bsb_ = SBF[h].next()
                    memset("pool", sb_[:], 0.0, [bsb_])
                    cur.append((sb_, bsb_))
                for i in order:
                    t0 = 128 * i
                    g, bg = GP.next(); ld("sp", g[:], HG[t0:t0 + 128, 512 * d_:512 * d_ + 512], bg, [dB["HG"]])
                    qT, bqT = QTp.next(); ld("sp", qT[:].rearrange("p (h t) -> p h t", h=4), HQ[:, :, t0:t0 + 128].rearrange("h p t -> p h t"), bqT, [dB["HQ"]])
                    kT, bkT = KTp.next(); ld("sp", kT[:].rearrange("p (h t) -> p h t", h=4), HK[4 * d_:4 * d_ + 4, :, t0:t0 + 128].rearrange("h p t -> p h t"), bkT, [dB["HK"]])
                    kk, bkk = KKp.next(); ld("sp", kk[:], HKt[t0:t0 + 128, 512 * d_:512 * d_ + 512], bkk, [dB["HKt"]])
                    vv, bvv = VVp.next(); ld("sp", vv[:], HV[t0:t0 + 128, :], bvv, [dB["HV"]])
                    pB, bpB = PS[0]
                    for h in range(4):
                        mmp(pB[:, 128 * h:128 * h + 128], g[:, 128 * h:128 * h + 128], Um_f, True, True, [bg, bCF], bpB) if h else \
                            mm(pB[:, 0:128], g[:, 0:128], Um_f, True, True, [bg, bCF], bpB)
                    e1, be1 = E1p.next(); act(e1[:], pB[:], AF.Exp, [bpB], [be1])
                    e2, be2 = E2p.next(); act(e2[:], pB[:], AF.Exp, [bpB], [be2], scale=-1.0)
                    qs, bqs = QSp.next(); tt("dve", qs[:], qT[:], e1[:], ALU.mult, [bqT, be1], [bqs])
                    ks, bks = KSp.next(); tt("dve", ks[:], kT[:], e2[:], ALU.mult, [bkT, be2], [bks])
                    pD, bpD = PS[1]
                    mm(pD[:], Dm_f, g[:], True, True, [bCF, bg], bpD)
                    e3, be3 = E2p.next(); act(e3[:], pD[:], AF.Exp, [bpD], [be3])
                    kh, bkh = KHp.next(); tt("dve", kh[:], kk[:], e3[:], ALU.mult, [bkk, be3], [bkh])
                    khc, bkhc = KHCp.next()
                    for c in range(4):
                        ts("dve", khc[:, c, :], kh[:], RM[:, c:c + 1], None, ALU.mult, None, [bkh, bRM], (), pr=[bkhc])
                    pOs = [PS[4 + h] for h in range(4)]
                    qscs = []
                    for h in range(4):
                        hs = slice(128 * h, 128 * h + 128)
                        qsc, bqsc = QSCp.next()
                        for c in range(4):
                            tt("pool", qsc[:, c, :], qs[:, hs], BDC[:, c, :], ALU.mult, [bqs, bCB], (), pr=[bqsc])
                        pA, bpA = PS[2 + (h % 2)]
                        mm(pA[:, 0:128], ks[:, hs], qs[:, hs], True, True, [bks, bqs], bpA)
                        at_, bat = ATp.next()
                        tt("dve", at_[:], pA[:, 0:128], Um_b, ALU.mult, [bpA, bCB], [bat])
                        pO, bpO = pOs[h]
                        mm(pO[:, 0:128], at_[:], vv[:, hs], True, False, [bat, bvv], bpO)
                        qscs.append((qsc, bqsc))
                    for ci, c in enumerate(corder):
                        tl = 32 * c + 31 if d_ == 0 else 32 * c
                        for h in range(4):
                            hs = slice(128 * h, 128 * h + 128)
                            pO, bpO = pOs[h]
                            sf, bsf = SF[h]
                            sb_, bsb_ = cur[h]
                            qsc, bqsc = qscs[h]
                            mmp(pO[:, 0:128], qsc[:, c, :], sb_[:], False, ci == 3, [bqsc, bsb_], bpO)
                            pU, bpU = PS[2 + (h % 2)]
                            mm(pU[:, 0:128], khc[:, c, hs], vv[:, hs], True, True, [bkhc, bvv], bpU)
                            stt(sf[:], sf[:], e1[:, 128 * h + tl:128 * h + tl + 1], pU[:, 0:128], ALU.mult, ALU.add, [bsf, be1, bpU], [bsf])
                            nb_, bnb_ = SBF[h].next()
                            cp("act", nb_[:], sf[:], [bsf], [bnb_])
                            cur[h] = (nb_, bnb_)
                    oo, boo = OOp.next()
                    if d_ == 0:
                        for h in range(4):
                            hs = slice(128 * h, 128 * h + 128)
                            cp("act", oo[:, hs], pOs[h][0][:, 0:128], [pOs[h][1]], (), pr=[boo])
                        stq("sp", OF[t0:t0 + 128, :], oo[:], boo, dB["OF"])
                    else:
                        of_, bof = GP.next()
                        ld("sp", of_[:], OF[t0:t0 + 128, :], bof, [dB["OF"]])
                        for h in range(4):
                            hs = slice(128 * h, 128 * h + 128)
                            tt("dve", oo[:, hs], pOs[h][0][:, 0:128], of_[:, hs], ALU.add, [pOs[h][1], bof], (), pr=[boo])
                        gt_, bgt = WKB.next()
                        ld("sp", gt_[:, 0:512], HGT[t0:t0 + 128, :], bgt, [dB["HGT"]])
                        ss, bss = SM.next()
                        junk, bj = WK4.next()
                        for h in range(4):
                            hs = slice(128 * h, 128 * h + 128)
                            S.op("act", lambda e, junk=junk, oo=oo, ss=ss, hs=hs, h=h: e.activation(out=junk[:, hs], in_=oo[:, hs], func=AF.Square, accum_out=ss[:, h:h + 1]),
                                 reads=[boo], partial=[bj, bss])
                        t, b = SM.next()
                        S.op("act", lambda e, t=t, ss=ss: e.activation(out=t[:, 0:4], in_=ss[:, 0:4], func=AF.Sqrt, bias=eps_t[:, 0:1], scale=1.0 / 128),
                             reads=[bss, beps], writes=[b])
                        S.op("dve", lambda e, t=t: e.reciprocal(out=t[:, 4:8], in_=t[:, 0:4]), reads=[b], writes=[b])
                        oc, boc = WKB.next()
                        for h in range(4):
                            hs = slice(128 * h, 128 * h + 128)
                            stt(junk[:, hs], oo[:, hs], t[:, 4 + h:5 + h], HNW[:], ALU.mult, ALU.mult, [boo, b, bHNW], (), pr=[bj])
                        tt("dve", oc[:, 0:512], junk[:, 0:512], gt_[:, 0:512], ALU.mult, [bj, bgt], [boc])
                        stq("sp", OC[t0:t0 + 128, :], oc[:, 0:512], boc, dB["OC"])

            ph = new_phase()
            WBR, bWBR = SB("WBR", [128, 12, D], BF16, ph)
            ld("pool", WBR[:], w_br[l].rearrange("i (k p) n -> p (i k) n", p=128), bWBR)
            WO, bWO = SB("WO", [128, 8, D], BF16, ph)
            ld("pool", WO[:], w_out[l].rearrange("(k p) n -> p k n", p=128), bWO)
            WR, bWR = SB("WR", [128, 8, NE], F32, ph)
            ld("sp", WR[:], w_router[l].rearrange("(k p) n -> p k n", p=128), bWR)
            BR, bBR = SB("BR", [128, NE], F32, ph)
            ld("sp", BR[:], b_router[l].partition_broadcast(128), bBR)
            memset("pool", RS_[:], 0.0, [bRS])
            OTp = Pool("otp", [128, 12, 128], BF16, 2, ph)
            GTp = Pool("gtp", [128, 3072], BF16, 2, ph)
            H2Tp = Pool("h2t", [128, 8, 128], F32, 2, ph)
            SMR = Pool("smr", [128, 4, NE], F32, 3, ph)
            zt, bzt = WKB.next()
            memset("pool", zt[:], 0.0, [bzt])
            for r0 in range(0, NSL, 128):
                stq("sp", XBUF[r0:r0 + 128, :], zt[:], bzt, dB["XBUF"])

            def gath(dst, src, i, dbuf, srcname, first=True):
                S.dma("pool", lambda e: e.indirect_dma_start(out=dst, out_offset=None, in_=src,
                                                             in_offset=bass.IndirectOffsetOnAxis(ap=IDXQ[:, i:i + 1], axis=0)),
                      dbuf.ds, reads=[dB[srcname], bIDXQ], writes=[dbuf] if first else (), partial=() if first else [dbuf])

            for i in p6_tiles:
                t0 = 128 * i
                ms, bms = (MODL, bMODL) if (last or i >= NCT) else (MODC, bMODC)
                ob3, bob3 = WKB.next(), None
                oin, boin = ob3
                oin2, boin2 = WKB.next()
                gts, bgts = GTp.next()
                if last:
                    gath(oin[:, 0:512], OA, i, boin, "OA")
                    gath(oin[:, 512:1024], OB, i, boin, "OB", first=False)
                    gath(oin2[:, 0:512], OC, i, boin2, "OC")
                    gath(gts[:], GT, i, bgts, "GT")
                else:
                    ld("sp", oin[:, 0:512], OA[t0:t0 + 128, :], boin, [dB["OA"]])
                    ld("sp", oin[:, 512:1024], OB[t0:t0 + 128, :], boin, [dB["OB"]])
                    ld("sp", oin2[:, 0:512], OC[t0:t0 + 128, :], boin2, [dB["OC"]])
                    ld("sp", gts[:], GT[t0:t0 + 128, :], bgts, [dB["GT"]])
                pt, bpt = PS[7]
                ptb = pt[:].bitcast(BF16)
                oT, boT = OTp.next()
                for kc in range(8):
                    tr(ptb[:, kc * 128:(kc + 1) * 128], oin[:, kc * 128:(kc + 1) * 128], ID_b, [boin, bCB], bpt, kc == 0)
                cp("act", oT[:, 0:8, :], ptb.rearrange("p (k t) -> p k t", k=8), [bpt], [boT])
                for kc in range(4):
                    tr(ptb[:, kc * 128:(kc + 1) * 128], oin2[:, kc * 128:(kc + 1) * 128], ID_b, [boin2, bCB], bpt, kc == 0)
                cp("act", oT[:, 8:12, :], ptb[:, 0:512].rearrange("p (k t) -> p k t", k=4), [bpt], (), pr=[boT])
                y, by = WK4.next()
                tmpy, btmpy = WK4.next()
                for br_ in range(3):
                    for cb in range(2):
                        pp, bpp = PS[cb]
                        for kc in range(4):
                            mm(pp[:], oT[:, 4 * br_ + kc, :], WBR[:, 4 * br_ + kc, 512 * cb:512 * cb + 512], kc == 0, kc == 3, [boT, bWBR], bpp)
                        gsl = gts[:, D * br_ + 512 * cb:D * br_ + 512 * cb + 512]
                        if br_ == 0:
                            tt("dve", y[:, 512 * cb:512 * cb + 512], pp[:], gsl, ALU.mult, [bpp, bgts], (), pr=[by])
                        else:
                            tt("dve", tmpy[:, 512 * cb:512 * cb + 512], pp[:], gsl, ALU.mult, [bpp, bgts], (), pr=[btmpy])
                            tt("pool", y[:, 512 * cb:512 * cb + 512], y[:, 512 * cb:512 * cb + 512], tmpy[:, 512 * cb:512 * cb + 512], ALU.add, [btmpy, by], [by])
                yb, byb = WKB.next()
                cp("act", yb[:], y[:], [by], [byb])
                for kc in range(8):
                    tr(ptb[:, kc * 128:(kc + 1) * 128], yb[:, kc * 128:(kc + 1) * 128], ID_b, [byb, bCB], bpt, kc == 0)
                yT, byT = WKB.next()
                cp("act", yT[:], ptb, [bpt], [byT])
                ss, bss = SM.next()
                junk, bj = WK4.next()
                pps = []
                for cb in range(2):
                    pp, bpp = PS[2 + cb]
                    for kc in range(8):
                        mm(pp[:], yT[:, kc * 128:(kc + 1) * 128], WO[:, kc, 512 * cb:512 * cb + 512], kc == 0, kc == 7, [byT, bWO], bpp)
                    S.op("act", lambda e, junk=junk, pp=pp, ss=ss, cb=cb: e.activation(out=junk[:, 512 * cb:512 * cb + 512], in_=pp[:], func=AF.Square, accum_out=ss[:, cb:cb + 1]),
                         reads=[bpp], partial=[bj, bss])
                    pps.append((pp, bpp))
                tt("dve", ss[:, 2:3], ss[:, 0:1], ss[:, 1:2], ALU.add, [bss], [bss])
                rs, brs = rstd_from_ss(ss[:, 2:3], bss, D)
                xt, bx = WK4.next()
                if last:
                    gath(xt[:], XR, i, bx, "XR")
                else:
                    ld("sp", xt[:], XR[t0:t0 + 128, :], bx, [dB["XR"]])
                for cb in range(2):
                    pp, bpp = pps[cb]
                    stt(junk[:, 512 * cb:512 * cb + 512], pp[:], rs, ms[:, 2, 512 * cb:512 * cb + 512], ALU.mult, ALU.mult, [bpp, brs, bms], (), pr=[bj])
                tt("dve", xt[:], xt[:], junk[:], ALU.add, [bx, bj], [bx])
                if last:
                    stq("sp", XQ[t0:t0 + 128, :], xt[:], bx, dB["XQ"])
                else:
                    stq("sp", XR[t0:t0 + 128, :], xt[:], bx, dB["XR"])
                h2f, bh2f = WK4.next()
                hb2, bhb2 = norm_mod_T(xt, bx, ms, bms, 4, 3, t0, keep=(h2f, bh2f), do_T=False)
                h2T, bh2T = H2Tp.next()
                for half in range(2):
                    pr0, bpr0 = PS[4 + half]
                    for kc in range(4):
                        k2 = 4 * half + kc
                        tr(pr0[:, kc * 128:(kc + 1) * 128], h2f[:, k2 * 128:(k2 + 1) * 128], ID_f, [bh2f, bCF], bpr0, kc == 0)
                    cp("act", h2T[:, 4 * half:4 * half + 4, :], pr0[:].rearrange("p (k t) -> p k t", k=4), [bpr0], (), pr=[bh2T])
                pl, bpl = PS[6]
                for kc in range(8):
                    mm(pl[:, 0:NE], h2T[:, kc, :], WR[:, kc, :], kc == 0, kc == 7, [bh2T, bWR], bpl)
                sm, bsm = SMR.next()
                Lg = sm[:, 0, :]; Mk = sm[:, 1, :]; Pb = sm[:, 2, :]; Oh = sm[:, 3, :]
                tt("dve", Lg, pl[:, 0:NE], BR[:], ALU.add, [bpl, bBR], [bsm])
                t8, bt8 = SM.next()
                S.op("dve", lambda e, t8=t8, Lg=Lg: e.max(out=t8[:, 0:8], in_=Lg), reads=[bsm], writes=[bt8])
                ts("dve", t8[:, 8:9], t8[:, 0:1], -1.0, None, ALU.mult, None, [bt8], [bt8])
                S.op("act", lambda e, t8=t8: e.activation(out=t8[:, 9:13], in_=t8[:, 0:4], func=AF.Exp, bias=t8[:, 8:9], accum_out=t8[:, 13:14]),
                     reads=[bt8], writes=[bt8])
                S.op("dve", lambda e, t8=t8: e.reciprocal(out=t8[:, 14:15], in_=t8[:, 13:14]), reads=[bt8], writes=[bt8])
                ts("dve", G4[:, i, :], t8[:, 9:13], t8[:, 14:15], None, ALU.mult, None, [bt8], (), pr=[bG4])
                ts("dve", Mk, Lg, t8[:, 3:4], None, ALU.is_ge, None, [bsm, bt8], [bsm])
                pq, bpq = PS[6]
                mm(pq[:, 64:64 + NE], UTS_f, Mk, True, False, [bCF, bsm], bpq)
                mm(pq[:, 64:64 + NE], ONES_f, RS_[:], False, True, [bCF, bRS], bpq)
                tt("dve", Pb, pq[:, 64:64 + NE], EB, ALU.add, [bpq, bCF], [bsm])
                tt("pool", RS_[:], RS_[:], Mk, ALU.add, [bRS, bsm], [bRS])
                df, bdf = SM.next()
                for k in range(4):
                    ts("dve", Oh, Lg, t8[:, k:k + 1], None, ALU.is_equal, None, [bsm, bt8], [bsm])
                    tt("dve", Oh, Oh, Pb, ALU.mult, [bsm], [bsm])
                    S.op("dve", lambda e, df=df, Oh=Oh, k=k: e.reduce_sum(out=df[:, k:k + 1], in_=Oh, axis=AX.X), reads=[bsm], partial=[bdf])
                ts("dve", df[:, 0:4], df[:, 0:4], float(NSL - 1), None, ALU.min, None, [bdf], [bdf])
                cp("dve", DEST[:, i, :], df[:, 0:4], [bdf], (), pr=[bDEST])
                for k in range(4):
                    S.dma("pool", lambda e, i=i, k=k, hb2=hb2: e.indirect_dma_start(
                        out=XBUF, out_offset=bass.IndirectOffsetOnAxis(ap=DEST[:, i, k:k + 1], axis=0),
                        in_=hb2[:, :], in_offset=None),
                        bhb2.ds, reads=[bhb2, bDEST], writes=[dB["XBUF"]] if (i == p6_tiles[0] and k == 0) else (), partial=() if (i == p6_tiles[0] and k == 0) else [dB["XBUF"]])

            ph = new_phase()
            WGU = Pool("wgu", [128, 8, 2 * D], BF16, 1, ph)
            WDN = Pool("wdn", [128, 8, D], BF16, 1, ph)
            BGU = Pool("bgu", [128, 16], F32, 2, ph)
            BDN = Pool("bdn", [128, D], F32, 1, ph)
            XSp = Pool("xsp", [128, D], BF16, 3, ph)
            XTp = Pool("xtp", [128, 8, 512], BF16, 2, ph)
            ATT = Pool("att", [128, 8, 512], BF16, 1, ph)
            GCp = Pool("gcp", [128, 512], F32, 2, ph)
            SGp = Pool("sgp", [128, 512], F32, 2, ph)
            UCp = Pool("ucp", [128, 512], F32, 2, ph)
            YTp = Pool("ytp", [128, D], BF16, 2, ph)
            for e_ in range(NE):
                wg, bwg = WGU.next(); wd, bwd = WDN.next(); bg_, bbg = BGU.next(); bd_, bbd = BDN.next()
                for kc in range(8):
                    S.dma("pool", lambda e, wg=wg, kc=kc, e_=e_: e.dma_start(out=wg[:, kc, :], in_=w_gu[l, e_, kc * 128:(kc + 1) * 128, :]),
                          bwg.ds, writes=[bwg] if kc == 0 else (), partial=() if kc == 0 else [bwg])
                ld("pool", wd[:], w_dn[l, e_].rearrange("(k p) n -> p k n", p=128), bwd)
                ld("sp", bg_[:], b_gu_t[l, e_], bbg)
                ld("sp", bd_[:], b_dn[l, e_].partition_broadcast(128), bbd)
                for blk in range(CAPl // 512):
                    r0 = e_ * CAPl + 512 * blk
                    xT, bxT = XTp.next()
                    for s in range(4):
                        xs, bxs = XSp.next()
                        ld("sp", xs[:], XBUF[r0 + 128 * s:r0 + 128 * s + 128, :], bxs, [dB["XBUF"]])
                        pt, bpt = PS[6 + (s % 2)]
                        ptb = pt[:].bitcast(BF16)
                        for kc in range(8):
                            tr(ptb[:, kc * 128:(kc + 1) * 128], xs[:, kc * 128:(kc + 1) * 128], ID_b, [bxs, bCB], bpt, kc == 0)
                        cp("act", xT[:, :, 128 * s:128 * s + 128], ptb.rearrange("p (k t) -> p k t", k=8), [bpt], (), pr=[bxT])
                    aT, baT = ATT.next()
                    for f in range(8):
                        pg, bpg = PS[0 + (f % 2) * 2]
                        pu, bpu = PS[1 + (f % 2) * 2]
                        for kc in range(8):
                            mm(pg[:], wg[:, kc, 128 * f:128 * f + 128], xT[:, kc, :], kc == 0, kc == 7, [bwg, bxT], bpg)
                        for kc in range(8):
                            mm(pu[:], wg[:, kc, D + 128 * f:D + 128 * f + 128], xT[:, kc, :], kc == 0, kc == 7, [bwg, bxT], bpu)
                        gc, bgc = GCp.next(); sg, bsg = SGp.next(); uc, buc = UCp.next()
                        ts("dve", gc[:], pg[:], bg_[:, f:f + 1], 7.0, ALU.add, ALU.min, [bpg, bbg], [bgc])
                        act(sg[:], gc[:], AF.Silu, [bgc], [bsg], scale=1.702)
                        ts("dve", uc[:], pu[:], bg_[:, 8 + f:9 + f], 7.0, ALU.add, ALU.min, [bpu, bbg], [buc])
                        ts("dve", uc[:], uc[:], -7.0, 1.0, ALU.max, ALU.add, [buc], [buc])
                        stt(aT[:, f, :], uc[:], 1.0 / 1.702, sg[:], ALU.mult, ALU.mult, [buc, bsg], (), pr=[baT])
                    for s in range(4):
                        yt, byt = YTp.next()
                        for cb in range(2):
                            pp, bpp = PS[4 + cb]
                            for f in range(8):
                                mm(pp[:], aT[:, f, 128 * s:128 * s + 128], wd[:, f, 512 * cb:512 * cb + 512], f == 0, f == 7, [baT, bwd], bpp)
                            tt("dve", yt[:, 512 * cb:512 * cb + 512], pp[:], bd_[:, 512 * cb:512 * cb + 512], ALU.add, [bpp, bbd], (), pr=[byt])
                        stq("sp", YBUF[r0 + 128 * s:r0 + 128 * s + 128, :], yt[:], byt, dB["YBUF"])

            ph = new_phase()
            YG = Pool("yg", [128, 4, D], BF16, 2, ph)
            for i in p6_tiles:
                t0 = 128 * i
                ms, bms = (MODL, bMODL) if (last or i >= NCT) else (MODC, bMODC)
                yg, byg = YG.next()
                for k in range(4):
                    S.dma("pool", lambda e, yg=yg, i=i, k=k: e.indirect_dma_start(
                        out=yg[:, k, :], out_offset=None, in_=YBUF,
                        in_offset=bass.IndirectOffsetOnAxis(ap=DEST[:, i, k:k + 1], axis=0)),
                        byg.ds, reads=[dB["YBUF"], bDEST], writes=[byg] if k == 0 else (), partial=() if k == 0 else [byg])
                f_, bf_ = WK4.next()
                ts("dve", f_[:], yg[:, 0, :], G4[:, i, 0:1], None, ALU.mult, None, [byg, bG4], [bf_])
                for k in range(1, 4):
                    stt(f_[:], yg[:, k, :], G4[:, i, k:k + 1], f_[:], ALU.mult, ALU.add, [byg, bG4, bf_], [bf_])
                junk, bj = WK4.next()
                ss, bss = SM.next()
                act(junk[:], f_[:], AF.Square, [bf_], [bj, bss], accum_out=ss[:, 0:1])
                rs, brs = rstd_from_ss(ss[:, 0:1], bss, D)
                xt, bx = WK4.next()
                if last:
                    ld("sp", xt[:], XQ[t0:t0 + 128, :], bx, [dB["XQ"]])
                else:
                    ld("sp", xt[:], XR[t0:t0 + 128, :], bx, [dB["XR"]])
                stt(junk[:], f_[:], rs, ms[:, 5, :], ALU.mult, ALU.mult, [bf_, brs, bms], [bj])
                tt("dve", xt[:], xt[:], junk[:], ALU.add, [bx, bj], [bx])
                if last:
                    stq("sp", out[t0:t0 + 128, :], xt[:], bx, dB["out"])
                else:
                    stq("sp", XR[t0:t0 + 128, :], xt[:], bx, dB["XR"])

            S.barrier(list(DS.values())); S.emit(); phase[0].close(); phase[0] = None
            lay.close()
        S.wait_all("sp", list(dB.values()))
        S.emit()
        print("sems used:", S.nsem, "instr:", S.ninstr)
    return nc


def make_consts(S_, L_):
    T = S_ + L_
    GRID_W = 64
    rows = S_ // GRID_W
    row = np.repeat(np.arange(rows, dtype=np.float32), GRID_W)
    col = np.tile(np.arange(GRID_W, dtype=np.float32), rows)
    inv = (10000.0 ** (-np.arange(16, dtype=np.float32) / 16)).astype(np.float32)
    ang_r = row[:, None] * inv
    ang_c = col[:, None] * inv
    cos64 = np.ones((64, T), np.float32)
    sin64 = np.zeros((64, T), np.float32)
    for d in range(64):
        half, j = d // 32, d % 32
        n = j % 16
        ang = (ang_r if half == 0 else ang_c)[:, n]
        cos64[d, L_:] = np.cos(ang)
        sin64[d, L_:] = (-np.sin(ang)) if j < 16 else np.sin(ang)
    ropc = np.concatenate([cos64, cos64], 0)
    rops = np.concatenate([sin64, sin64], 0)
    s = np.arange(128)[:, None]
    t = np.arange(128)[None, :]
    same = (s // 32) == (t // 32)
    UF = (same & (s <= t)).astype(np.float32)
    UB = (same & (s >= t)).astype(np.float32)
    BD = same.astype(np.float32)
    cst_f = np.zeros((128, 8, 128), np.float32)
    cst_f[:, 0] = UF; cst_f[:, 1] = UB; cst_f[:, 2] = BD - UF; cst_f[:, 3] = BD - UB
    cst_f[:, 4] = (s < t).astype(np.float32)
    cst_f[:, 5] = 1.0
    cst_f[:, 6] = np.eye(128, dtype=np.float32)
    cst_f[:, 7, 0:NE] = (np.arange(NE) * CAP).astype(np.float32)[None, :]
    cst_f[:, 7, NE:2 * NE] = (np.arange(NE) * CAP1).astype(np.float32)[None, :]
    cst_b = np.zeros((128, 12, 128), np.float32)
    cst_b[:, 0] = np.eye(128)
    cst_b[:, 1] = (s >= t)
    cst_b[:, 2] = (s <= t)
    cst_b[:, 3] = UF; cst_b[:, 4] = UB
    for c in range(4):
        cst_b[:, 5 + c] = ((t // 32) == c).astype(np.float32) * np.ones((128, 1), np.float32)
    cst_b[0:64, 9, 0] = 1.0
    cst_b[64:128, 9, 1] = 1.0
    rowmask = np.zeros((128, 4), np.float32)
    for c in range(4):
        rowmask[32 * c:32 * c + 32, c] = 1.0
    return dict(ropc=ropc, rops=rops, cst_f=cst_f, cst_b=cst_b.astype(ml_dtypes.bfloat16), rowmask=rowmask)


def rope_perm_cols():
    cols = []
    for (c0, w) in ((C_AQ, 512), (C_AK, 128), (C_BQ, 512), (C_BK, 512)):
        for m in range(w):
            blk, j = m // 32, m % 32
            cols.append(c0 + blk * 32 + (j + 16) % 32)
    return np.array(cols, dtype=np.int64)


def make_in_maps(inputs, S_, L_, n_cores=8):
    f = lambda a: np.ascontiguousarray(np.asarray(a, dtype=np.float32))
    x = f(inputs["x"]); c = f(inputs["c"]); ctx = f(inputs["ctx"]); c_ctx = f(inputs["c_ctx"])
    w_in = f(inputs["w_in"])
    consts = make_consts(S_, L_)
    shared = dict(
        cc_col=np.ascontiguousarray(c_ctx.reshape(8, 128).T),
        w_mod=f(inputs["w_mod"]), b_mod=f(inputs["b_mod"]), norm_g=f(inputs["norm_g"]).reshape(2, 4 * D),
        w_in=w_in, w_inp=np.ascontiguousarray(w_in[:, :, rope_perm_cols()]),
        sink=f(inputs["attn_sink"]), dlam=f(inputs["diff_lambda"]).reshape(2, 256), dnw=f(inputs["diff_norm_w"]),
        lbl=f(inputs["hgrn_lb_logits"]).reshape(2, 1024),
        lbl_fm=np.ascontiguousarray(f(inputs["hgrn_lb_logits"]).reshape(2, 8, 128).transpose(0, 2, 1)),
        hnw=f(inputs["hgrn_norm_w"]), w_br=f(inputs["w_branch"]), w_out=f(inputs["w_out"]),
        w_router=f(inputs["w_router"]), b_router=f(inputs["b_router"]),
        w_gu=f(inputs["w_gate_up"]),
        b_gu_t=np.ascontiguousarray(f(inputs["b_gate_up"]).reshape(2, NE, 16, 128).transpose(0, 1, 3, 2)),
        w_dn=f(inputs["w_down"]), b_dn=f(inputs["b_down"]),
    )
    shared.update(consts)
    maps = []
    B = x.shape[0]
    for core in range(n_cores):
        b = core * B // n_cores
        m = dict(shared)
        m["xin"] = x[b]
        m["ctxin"] = ctx[b]
        m["c_col"] = np.ascontiguousarray(c[b].reshape(8, 128).T)
        cpb = n_cores // B
        SQ = S_ // cpb
        NQT = SQ // 128
        j = core % cpb
        idx = np.zeros((128, NQT + 1), np.int32)
        idx[:, :NQT] = (L_ + j * SQ + 128 * np.arange(NQT)[None, :] + np.arange(128)[:, None]).astype(np.int32)
        m["idxq"] = idx
        maps.append(m)
    return maps


def kernel(**inputs):
    x = np.asarray(inputs["x"])
    B, S_, _ = x.shape
    L_ = np.asarray(inputs["ctx"]).shape[1]
    nc = bass.Bass("TRN2", target_bir_lowering=False)
    build(nc, S_, L_)
    maps = make_in_maps(inputs, S_, L_)
    res = run_bass_kernel_spmd(nc, maps, core_ids=list(range(8)))
    cpb = 8 // B
    outs = [np.concatenate([np.asarray(res.results[b * cpb + j]["out"]) for j in range(cpb)], axis=0) for b in range(B)]
    return np.stack(outs, 0).astype(np.float32)
```
